# Optimizing a Trainium2 kernel written in Bass

```python
import jax
import jax.numpy as jnp
from jax import lax
import numpy as np

D_MODEL = 1024
BATCH = 4
SEQ = 8192
DEPTH = 2

GRID_W = 64
CTX_LEN = 256
HEAD_DIM = 64
ROPE_BASE = 10000.0
EPS = 1e-6

A_HEADS = 8
A_KV_HEADS = 2
A_GROUP = A_HEADS // A_KV_HEADS
A_WINDOW = 128
A_BLOCK = A_WINDOW
B_HEADS = 8
B_DK = 64
B_DV = 64
B_CHUNK = 64
C_HEADS = 8
C_NOPE = 64
C_ROPE = 32
C_V = 64
C_Q_LORA = 256
C_KV_LORA = 128
C_QBLOCK = 128
N_BRANCH = 3
BRANCH_W = 512
N_GROUPS = 4
EXPERTS_PER_GROUP = 8
N_EXPERTS = N_GROUPS * EXPERTS_PER_GROUP
TOP_K_IN_GROUP = 2
D_EXPERT = 256

A_Q_W = A_HEADS * HEAD_DIM
A_KV_W = A_KV_HEADS * HEAD_DIM
B_W = B_HEADS * B_DK
GATE_W = N_BRANCH * D_MODEL
IN_SPLITS = (A_Q_W, A_KV_W, A_KV_W, B_W, B_W, B_W, B_W, B_W, C_Q_LORA, C_KV_LORA, C_ROPE, GATE_W)
IN_W = A_Q_W + 2 * A_KV_W + 5 * B_W + C_Q_LORA + C_KV_LORA + C_ROPE + GATE_W

kernel_name = 'hybrid_dit_trunk_swa_hgrn2_mla_hmoe'

F32 = jnp.float32


def rmsnorm(x, g):
    xf = x.astype(F32)
    y = xf * lax.rsqrt(jnp.mean(xf * xf, axis=-1, keepdims=True) + EPS)
    return (y * g.astype(F32)).astype(x.dtype)


def modulate(x, g, shift, scale):
    return rmsnorm(x, g) * (1 + scale) + shift


def axial_rope(rows, rot_dim):
    row = jnp.repeat(jnp.arange(rows, dtype=F32), GRID_W)
    col = jnp.tile(jnp.arange(GRID_W, dtype=F32), rows)
    n_freq = rot_dim // 4
    inv = ROPE_BASE ** (-jnp.arange(n_freq, dtype=F32) / n_freq)
    ang = jnp.concatenate([row[:, None] * inv, col[:, None] * inv], axis=-1)
    return jnp.cos(ang), jnp.sin(ang)


def apply_rope(x, cos, sin):
    half = x.shape[-1] // 2
    x1, x2 = x[..., :half], x[..., half:]
    c = cos[None, :, None, :].astype(x.dtype)
    s = sin[None, :, None, :].astype(x.dtype)
    return jnp.concatenate([x1 * c - x2 * s, x1 * s + x2 * c], axis=-1)


def multi_softmax(parts, sink=None):
    m = parts[0].max(-1, keepdims=True)
    for p in parts[1:]:
        m = jnp.maximum(m, p.max(-1, keepdims=True))
    if sink is not None:
        m = jnp.maximum(m, sink)
    es = [jnp.exp(p - m) for p in parts]
    denom = es[0].sum(-1, keepdims=True)
    for e in es[1:]:
        denom = denom + e.sum(-1, keepdims=True)
    if sink is not None:
        denom = denom + jnp.exp(sink - m)
    return [e / denom for e in es]


def split_in(p):
    idx = np.cumsum(IN_SPLITS)[:-1].tolist()
    return jnp.split(p, idx, axis=-1)


def window_gqa(q, k, v, qc, kc, vc, sink, cos, sin, need_ctx):
    b, s = q.shape[:2]
    nb = s // A_BLOCK
    scale = HEAD_DIM ** -0.5
    q = apply_rope(q, cos, sin)
    k = apply_rope(k, cos, sin)
    qb = q.reshape(b, nb, A_BLOCK, A_KV_HEADS, A_GROUP, HEAD_DIM)

    def band(t):
        tp = jnp.pad(t, ((0, 0), (A_BLOCK, A_BLOCK), (0, 0), (0, 0)))
        tp = tp.reshape(b, nb + 2, A_BLOCK, A_KV_HEADS, HEAD_DIM)
        return jnp.concatenate([tp[:, :-2], tp[:, 1:-1], tp[:, 2:]], axis=2)

    kb, vb = band(k), band(v)
    blk = jnp.arange(nb)[:, None, None] * A_BLOCK
    qpos = blk + jnp.arange(A_BLOCK)[None, :, None]
    kpos = blk - A_BLOCK + jnp.arange(3 * A_BLOCK)[None, None, :]
    valid = (kpos >= 0) & (kpos < s) & (jnp.abs(qpos - kpos) <= A_WINDOW)
    s_lat = jnp.einsum('bnqhgd,bnkhd->bnhgqk', qb, kb, preferred_element_type=F32) * scale
    s_lat = jnp.where(valid[None, :, None, None], s_lat, -jnp.inf)
    s_ctx = jnp.einsum('bnqhgd,blhd->bnhgql', qb, kc, preferred_element_type=F32) * scale
    snk = sink.astype(F32).reshape(1, 1, A_KV_HEADS, A_GROUP, 1, 1)
    p_lat, p_ctx = multi_softmax([s_lat, s_ctx], snk)
    o = (jnp.einsum('bnhgqk,bnkhd->bnqhgd', p_lat.astype(v.dtype), vb)
         + jnp.einsum('bnhgql,blhd->bnqhgd', p_ctx.astype(v.dtype), vc))
    o = o.reshape(b, s, A_Q_W)
    oc = None
    if need_ctx:
        lc = qc.shape[1]
        qcg = qc.reshape(b, lc, A_KV_HEADS, A_GROUP, HEAD_DIM)
        sc = jnp.einsum('blhgd,bmhd->bhglm', qcg, kc, preferred_element_type=F32) * scale
        (pc,) = multi_softmax([sc], sink.astype(F32).reshape(1, A_KV_HEADS, A_GROUP, 1, 1))
        oc = jnp.einsum('bhglm,bmhd->blhgd', pc.astype(vc.dtype), vc).reshape(b, lc, A_Q_W)
    return o, oc


def gla_chunks(q, k, v, logf, s0):
    b, t = q.shape[:2]
    nc = t // B_CHUNK

    def chunks(a):
        return a.astype(F32).reshape(b, nc, B_CHUNK, B_HEADS, a.shape[-1]).transpose(1, 0, 3, 2, 4)

    tri = jnp.tril(jnp.ones((B_CHUNK, B_CHUNK), dtype=bool))

    def step(S, inp):
        qc, kc, vc, gc = inp
        bc = jnp.cumsum(gc, axis=2)
        inter = jnp.einsum('bhtd,bhde->bhte', qc * jnp.exp(bc), S)
        diff = jnp.where(tri[:, :, None], bc[:, :, :, None, :] - bc[:, :, None, :, :], -jnp.inf)
        att = jnp.einsum('bhtsd,bhsd->bhts', qc[:, :, :, None, :] * jnp.exp(diff), kc)
        o = inter + jnp.einsum('bhts,bhse->bhte', att, vc)
        blast = bc[:, :, -1:, :]
        S = jnp.exp(blast[:, :, 0, :, None]) * S + jnp.einsum('bhsd,bhse->bhde', kc * jnp.exp(blast - bc), vc)
        return S, o

    S, o = lax.scan(step, s0, (chunks(q), chunks(k), chunks(v), chunks(logf)))
    o = o.transpose(1, 0, 3, 2, 4).reshape(b, t, B_HEADS, B_DV)
    return o, S


def hgrn2(q, i, zf, zb, g, qc, ic, zfc, zbc, gc, lb, g_onorm, need_ctx):
    def heads(a):
        return a.reshape(a.shape[0], a.shape[1], B_HEADS, B_DK)

    def forget(z, lbd):
        z = z.astype(F32)
        logf = jnp.logaddexp(jnp.log(lbd), jnp.log1p(-lbd) + jax.nn.log_sigmoid(z))
        key = (1 - lbd) * jax.nn.sigmoid(-z)
        return heads(key), heads(logf)

    def flip(a):
        return a[:, ::-1]

    b = q.shape[0]
    s0 = jnp.zeros((b, B_HEADS, B_DK, B_DV), F32)
    qh, vh, qch, vch = heads(q), heads(i), heads(qc), heads(ic)
    kfc, lfc = forget(zfc, lb[0])
    kf, lf = forget(zf, lb[0])
    oc_f, S_f = gla_chunks(qch, kfc, vch, lfc, s0)
    o_f, _ = gla_chunks(qh, kf, vh, lf, S_f)
    kbc, lbc = forget(zbc, lb[1])
    kbw, lbw = forget(zb, lb[1])
    oc_b, S_b = gla_chunks(flip(qch), flip(kbc), flip(vch), flip(lbc), s0)
    o_b, _ = gla_chunks(flip(qh), flip(kbw), flip(vh), flip(lbw), S_b)
    gn = g_onorm.reshape(B_HEADS, B_DV)
    o = rmsnorm(o_f + flip(o_b), gn).reshape(q.shape[0], q.shape[1], B_W).astype(g.dtype) * jax.nn.silu(g)
    oc = None
    if need_ctx:
        oc = rmsnorm(oc_f + flip(oc_b), gn).reshape(qc.shape[0], qc.shape[1], B_W).astype(gc.dtype) * jax.nn.silu(gc)
    return o, oc


def mla(cq, ckv, kr, cqc, ckvc, krc, g_q, g_kv, w_uq, w_ukv, cos, sin, need_ctx):
    def expand(cq_, ckv_, kr_, rope):
        b_, n_ = cq_.shape[:2]
        qh = (rmsnorm(cq_, g_q) @ w_uq).reshape(b_, n_, C_HEADS, C_NOPE + C_ROPE)
        kvh = (rmsnorm(ckv_, g_kv) @ w_ukv).reshape(b_, n_, C_HEADS, C_NOPE + C_V)
        q_nope, q_rope = qh[..., :C_NOPE], qh[..., C_NOPE:]
        k_nope, vv = kvh[..., :C_NOPE], kvh[..., C_NOPE:]
        k_rope = kr_[:, :, None, :]
        if rope:
            q_rope = apply_rope(q_rope, cos, sin)
            k_rope = apply_rope(k_rope, cos, sin)
        qf = jnp.concatenate([q_nope, q_rope], axis=-1)
        kf = jnp.concatenate([k_nope, jnp.broadcast_to(k_rope, (b_, n_, C_HEADS, C_ROPE))], axis=-1)
        return qf, kf, vv

    q, k, v = expand(cq, ckv, kr, True)
    qc, kc, vc = expand(cqc, ckvc, krc, False)
    scale = (C_NOPE + C_ROPE) ** -0.5
    b, s = q.shape[:2]
    nb = s // C_QBLOCK
    qb = q.reshape(b, nb, C_QBLOCK, C_HEADS, C_NOPE + C_ROPE).transpose(1, 0, 2, 3, 4)

    def block(qi):
        s_lat = jnp.einsum('bqhd,bkhd->bhqk', qi, k, preferred_element_type=F32) * scale
        s_ctx = jnp.einsum('bqhd,blhd->bhql', qi, kc, preferred_element_type=F32) * scale
        p_lat, p_ctx = multi_softmax([s_lat, s_ctx])
        return (jnp.einsum('bhqk,bkhd->bqhd', p_lat.astype(v.dtype), v)
                + jnp.einsum('bhql,blhd->bqhd', p_ctx.astype(vc.dtype), vc))

    o = lax.map(block, qb).transpose(1, 0, 2, 3, 4).reshape(b, s, C_HEADS * C_V)
    oc = None
    if need_ctx:
        sc = jnp.einsum('blhd,bmhd->bhlm', qc, kc, preferred_element_type=F32) * scale
        (pc,) = multi_softmax([sc])
        oc = jnp.einsum('bhlm,bmhd->blhd', pc.astype(vc.dtype), vc).reshape(qc.shape[0], qc.shape[1], C_HEADS * C_V)
    return o, oc


def merge_branches(branches, gate_logits, w_br, w_out):
    gl = gate_logits.reshape(gate_logits.shape[0], gate_logits.shape[1], N_BRANCH, D_MODEL)
    y = jax.nn.sigmoid(gl[:, :, 0]) * (branches[0] @ w_br[0])
    for n in range(1, N_BRANCH):
        y = y + jax.nn.sigmoid(gl[:, :, n]) * (branches[n] @ w_br[n])
    return y @ w_out


def hier_moe(h, w_rg, w_re, w1, w3, w2):
    shp = h.shape
    hf = h.reshape(-1, D_MODEL)
    n = hf.shape[0]
    g_logits = jnp.dot(hf, w_rg, preferred_element_type=F32)
    g_prob = jax.nn.softmax(g_logits, axis=-1)
    g_sel = jnp.argmax(g_logits, axis=-1)
    g_w = jnp.take_along_axis(g_prob, g_sel[:, None], axis=-1)
    e_logits = jnp.dot(hf, w_re, preferred_element_type=F32).reshape(n, N_GROUPS, EXPERTS_PER_GROUP)
    e_logits = jnp.take_along_axis(e_logits, g_sel[:, None, None], axis=1)[:, 0]
    e_prob = jax.nn.softmax(e_logits, axis=-1)
    top_v, top_i = lax.top_k(e_prob, TOP_K_IN_GROUP)
    wts = g_w * top_v / top_v.sum(-1, keepdims=True)
    ids = g_sel[:, None] * EXPERTS_PER_GROUP + top_i
    combine = (jax.nn.one_hot(ids, N_EXPERTS, dtype=F32) * wts[..., None]).sum(1)

    def expert(y, inp):
        w1e, w3e, w2e, ce = inp
        a = jax.nn.silu(hf @ w1e) * (hf @ w3e)
        return y + ce[:, None].astype(hf.dtype) * (a @ w2e), None

    y, _ = lax.scan(expert, jnp.zeros_like(hf), (w1, w3, w2, combine.T))
    return y.reshape(shp)


def trunk_layer(x, xc, mod, mod_c, lb, cos_a, sin_a, cos_c, sin_c, g_n1, g_n2, w_in, sink, g_onorm,
                g_q, g_kv, w_uq, w_ukv, w_br, w_out, w_rg, w_re, w1, w3, w2, need_ctx):
    sh1, sc1, gt1, sh2, sc2, gt2 = jnp.split(mod[:, None, :], 6, axis=-1)
    sh1c, sc1c, gt1c, sh2c, sc2c, gt2c = jnp.split(mod_c, 6, axis=-1)
    h = modulate(x, g_n1, sh1, sc1)
    hc = modulate(xc, g_n1, sh1c, sc1c)
    aq, ak, av, bq, bi, bzf, bzb, bg, cq, ckv, ckr, gl = split_in(h @ w_in)
    aqc, akc, avc, bqc, bic, bzfc, bzbc, bgc, cqc, ckvc, ckrc, glc = split_in(hc @ w_in)

    def hd(t, nh):
        return t.reshape(t.shape[0], t.shape[1], nh, HEAD_DIM)

    o_a, oc_a = window_gqa(hd(aq, A_HEADS), hd(ak, A_KV_HEADS), hd(av, A_KV_HEADS),
                           hd(aqc, A_HEADS), hd(akc, A_KV_HEADS), hd(avc, A_KV_HEADS),
                           sink, cos_a, sin_a, need_ctx)
    o_b, oc_b = hgrn2(bq, bi, bzf, bzb, bg, bqc, bic, bzfc, bzbc, bgc, lb, g_onorm, need_ctx)
    o_c, oc_c = mla(cq, ckv, ckr, cqc, ckvc, ckrc, g_q, g_kv, w_uq, w_ukv, cos_c, sin_c, need_ctx)
    x = x + gt1 * merge_branches((o_a, o_b, o_c), gl, w_br, w_out)
    x = x + gt2 * hier_moe(modulate(x, g_n2, sh2, sc2), w_rg, w_re, w1, w3, w2)
    if need_ctx:
        xc = xc + gt1c * merge_branches((oc_a, oc_b, oc_c), glc, w_br, w_out)
        xc = xc + gt2c * hier_moe(modulate(xc, g_n2, sh2c, sc2c), w_rg, w_re, w1, w3, w2)
    return x, xc


def setup_inputs(seed: int = 0) -> dict:
    key = jax.random.key(seed)
    ks = jax.random.split(key, 24)

    def nrm(k, shape, s):
        return jax.random.normal(k, shape, F32) * s

    return {
        'x': nrm(ks[0], (BATCH, SEQ, D_MODEL), 1.0),
        'c': nrm(ks[1], (BATCH, D_MODEL), 1.0),
        'ctx': nrm(ks[2], (BATCH, CTX_LEN, D_MODEL), 1.0),
        'c_ctx': nrm(ks[3], (D_MODEL,), 1.0),
        'w_mod': nrm(ks[4], (DEPTH, D_MODEL, 6 * D_MODEL), 0.5 * D_MODEL ** -0.5),
        'b_mod': nrm(ks[5], (DEPTH, 6 * D_MODEL), 0.02),
        'g_norm1': 1.0 + nrm(ks[6], (DEPTH, D_MODEL), 0.02),
        'g_norm2': 1.0 + nrm(ks[7], (DEPTH, D_MODEL), 0.02),
        'w_in': nrm(ks[8], (DEPTH, D_MODEL, IN_W), D_MODEL ** -0.5),
        'a_sink': nrm(ks[9], (DEPTH, A_HEADS), 0.5),
        'b_lb_logits': nrm(ks[10], (DEPTH, 2, B_W), 0.5),
        'b_onorm': 1.0 + nrm(ks[11], (DEPTH, B_W), 0.02),
        'c_qnorm': 1.0 + nrm(ks[12], (DEPTH, C_Q_LORA), 0.02),
        'c_kvnorm': 1.0 + nrm(ks[13], (DEPTH, C_KV_LORA), 0.02),
        'w_uq': nrm(ks[14], (DEPTH, C_Q_LORA, C_HEADS * (C_NOPE + C_ROPE)), C_Q_LORA ** -0.5),
        'w_ukv': nrm(ks[15], (DEPTH, C_KV_LORA, C_HEADS * (C_NOPE + C_V)), C_KV_LORA ** -0.5),
        'w_br': nrm(ks[16], (DEPTH, N_BRANCH, BRANCH_W, D_MODEL), BRANCH_W ** -0.5),
        'w_out': nrm(ks[17], (DEPTH, D_MODEL, D_MODEL), D_MODEL ** -0.5),
        'w_rg': nrm(ks[18], (DEPTH, D_MODEL, N_GROUPS), D_MODEL ** -0.5),
        'w_re': nrm(ks[19], (DEPTH, D_MODEL, N_EXPERTS), D_MODEL ** -0.5),
        'w1': nrm(ks[20], (DEPTH, N_EXPERTS, D_MODEL, D_EXPERT), D_MODEL ** -0.5),
        'w3': nrm(ks[21], (DEPTH, N_EXPERTS, D_MODEL, D_EXPERT), D_MODEL ** -0.5),
        'w2': nrm(ks[22], (DEPTH, N_EXPERTS, D_EXPERT, D_MODEL), D_EXPERT ** -0.5),
        'g_final': 1.0 + nrm(ks[23], (D_MODEL,), 0.02),
    }


def reference(x, c, ctx, c_ctx, w_mod, b_mod, g_norm1, g_norm2, w_in, a_sink, b_lb_logits, b_onorm,
              c_qnorm, c_kvnorm, w_uq, w_ukv, w_br, w_out, w_rg, w_re, w1, w3, w2, g_final):
    rows = x.shape[1] // GRID_W
    cos_a, sin_a = axial_rope(rows, HEAD_DIM)
    cos_c, sin_c = axial_rope(rows, C_ROPE)
    lb_all = jnp.cumsum(jax.nn.softmax(b_lb_logits.astype(F32), axis=0), axis=0)
    lb_all = lb_all - lb_all[0:1]
    xc = ctx
    for l in range(DEPTH):
        mod = jax.nn.silu(c) @ w_mod[l] + b_mod[l]
        mod_c = jax.nn.silu(c_ctx) @ w_mod[l] + b_mod[l]
        x, xc = trunk_layer(x, xc, mod, mod_c, lb_all[l], cos_a, sin_a, cos_c, sin_c,
                            g_norm1[l], g_norm2[l], w_in[l], a_sink[l], b_onorm[l],
                            c_qnorm[l], c_kvnorm[l], w_uq[l], w_ukv[l], w_br[l], w_out[l],
                            w_rg[l], w_re[l], w1[l], w3[l], w2[l], l < DEPTH - 1)
    return rmsnorm(x, g_final)
```

```python
from concourse.bass_utils import run_bass_kernel_spmd
import ml_dtypes

import numpy as np
import concourse.bass as bass
import concourse.mybir as mybir

F32 = mybir.dt.float32
BF16 = mybir.dt.bfloat16
AF = mybir.ActivationFunctionType
ALU = mybir.AluOpType
AX = mybir.AxisListType

COMPUTE = ("pe", "act", "dve", "pool")
NDMA_SEMS = 24
SEM_EPOCH = 30000


class Slot:
    __slots__ = ("name", "writers", "readers", "excl")

    def __init__(self, name):
        self.name = name
        self.excl = False
        self.writers = {}
        self.readers = {}


class Op:
    __slots__ = ("id", "eng", "fn", "deps", "is_dma", "idx", "signal", "queue", "dma_no")

    def __init__(self, id, eng, fn, is_dma, queue):
        self.id = id
        self.eng = eng
        self.fn = fn
        self.deps = []
        self.is_dma = is_dma
        self.queue = queue
        self.signal = False
        self.idx = -1
        self.dma_no = -1


class Prog:
    def __init__(self, nc):
        self.nc = nc
        self.ops = []
        self.queues = {k: [] for k in ("pe", "act", "dve", "pool", "sync")}
        self.nslot = 0
        self.cur_barrier = None
        self.last_barrier_pos = 0

    def barrier(self):
        op = Op(len(self.ops), "sync", lambda e: e.nop(), False, "sync")
        self.ops.append(op)
        q = self.queues["sync"]
        op.idx = len(q)
        q.append(op)
        deps = set()
        if self.cur_barrier is not None:
            deps.add(self.cur_barrier)
        for qn, qq in self.queues.items():
            nd = 0
            got_c = False
            for o in reversed(qq[:-1] if qn == "sync" else qq):
                if o.is_dma:
                    if nd < NDMA_SEMS:
                        deps.add(o.id)
                        nd += 1
                elif not got_c:
                    deps.add(o.id)
                    got_c = True
                if nd >= NDMA_SEMS and got_c:
                    break
        op.deps = sorted(deps)
        self.cur_barrier = op.id
        return op

    def slot(self, name=None):
        self.nslot += 1
        return Slot(name or f"s{self.nslot}")

    def slots(self, n, name=None):
        return [self.slot(f"{name}{i}") for i in range(n)]

    def _add(self, eng, fn, reads, writes, is_dma=False):
        op = Op(len(self.ops), eng, fn, is_dma, eng)
        self.ops.append(op)
        q = self.queues[eng]
        op.idx = len(q)
        q.append(op)
        key = ("dma", op.id) if is_dma else eng
        deps = set()
        xs_ = [s for s in reads if s.excl]
        if xs_:
            writes = list(writes) + xs_
        for s in reads:
            for k, oid in s.writers.items():
                deps.add(oid)
        for s in writes:
            for k, oid in s.writers.items():
                deps.add(oid)
            for k, oid in s.readers.items():
                deps.add(oid)
        deps.discard(op.id)
        if self.cur_barrier is not None:
            deps.add(self.cur_barrier)
        for s in reads:
            s.readers[key] = op.id
        for s in writes:
            s.writers = {key: op.id}
            s.readers = {}
        op.deps = sorted(deps)
        return op

    def pe(self, fn, reads=(), writes=()):
        return self._add("pe", fn, reads, writes)

    def act(self, fn, reads=(), writes=()):
        return self._add("act", fn, reads, writes)

    def dve(self, fn, reads=(), writes=()):
        return self._add("dve", fn, reads, writes)

    def pool(self, fn, reads=(), writes=()):
        return self._add("pool", fn, reads, writes)

    def dma(self, out, in_, reads=(), writes=(), q="sync", **kw):
        return self._add(q, lambda e: e.dma_start(out=out, in_=in_, **kw), reads, writes, is_dma=True)

    def emit(self, final_wait_ops=()):
        nc = self.nc
        ops = self.ops
        for op in ops:
            for d in op.deps:
                dop = ops[d]
                if dop.is_dma:
                    continue
                if dop.eng == "pe" and op.eng == "pe" and not op.is_dma:
                    continue
                dop.signal = True
        for o in final_wait_ops:
            if not o.is_dma:
                o.signal = True
        sems = {}
        sigval = {}
        for qn, q in self.queues.items():
            cnt = 0
            ep = 0
            for op in q:
                if op.is_dma or not op.signal:
                    continue
                if cnt >= SEM_EPOCH:
                    cnt = 0
                    ep += 1
                cnt += 1
                sigval[op.id] = (qn, ep, cnt)
                if (qn, ep) not in sems:
                    sems[(qn, ep)] = nc.alloc_semaphore(f"s_{qn}_{ep}")
        dma_sems = {}
        dma_cnt = {}
        for qn, q in self.queues.items():
            n = 0
            for op in q:
                if op.is_dma:
                    op.dma_no = n
                    n += 1
            dma_cnt[qn] = n
            if n:
                dma_sems[qn] = [nc.alloc_semaphore(f"d_{qn}_{i}") for i in range(min(n, NDMA_SEMS))]
        dma_ops = {qn: [op for op in q if op.is_dma] for qn, q in self.queues.items()}

        def dma_sem_val(op):
            return dma_sems[op.queue][op.dma_no % NDMA_SEMS], 16 * (op.dma_no // NDMA_SEMS + 1)

        snaps = [None] * len(ops)
        self.nwaits = 0

        def run_queue(qn, eng):
            q = self.queues[qn]
            clock = {}
            known_dma = set()

            def need(d):
                dop = ops[d]
                if dop.is_dma:
                    if d in known_dma:
                        return
                    s, v = dma_sem_val(dop)
                    eng.wait_ge(s, v)
                    self.nwaits += 1
                    known_dma.add(d)
                    return
                if clock.get(dop.eng, -1) >= dop.idx:
                    return
                _, ep, cnt = sigval[d]
                eng.wait_ge(sems[(dop.eng, ep)], cnt)
                self.nwaits += 1
                clock[dop.eng] = dop.idx
                sn = snaps[d]
                if sn:
                    for k, v in sn.items():
                        if clock.get(k, -1) < v:
                            clock[k] = v

            for op in q:
                for d in op.deps:
                    dop = ops[d]
                    if (not dop.is_dma) and dop.eng == "pe" and qn == "pe" and not op.is_dma:
                        continue
                    need(d)
                if op.is_dma and op.dma_no >= NDMA_SEMS:
                    need(dma_ops[qn][op.dma_no - NDMA_SEMS].id)
                snaps[op.id] = dict(clock)
                ins = op.fn(eng)
                if op.is_dma:
                    s, v = dma_sem_val(op)
                    ins.then_inc(s, 16)
                elif op.signal:
                    _, ep, cnt = sigval[op.id]
                    ins.then_inc(sems[(qn, ep)], 1)
            if qn == "sync":
                for o in final_wait_ops:
                    need(o.id)

        self._dry_snapshots(ops, sigval, snaps)

        with nc.Block() as block:
            @block.tensor
            def _(e):
                run_queue("pe", e)

            @block.scalar
            def _(e):
                run_queue("act", e)

            @block.vector
            def _(e):
                run_queue("dve", e)

            @block.gpsimd
            def _(e):
                run_queue("pool", e)

            @block.sync
            def _(e):
                run_queue("sync", e)

    def _dry_snapshots(self, ops, sigval, snaps):
        clocks = {qn: {} for qn in self.queues}
        for op in ops:
            clock = clocks[op.queue]
            for d in op.deps:
                dop = ops[d]
                if dop.is_dma:
                    continue
                if dop.eng == "pe" and op.queue == "pe" and not op.is_dma:
                    continue
                if clock.get(dop.eng, -1) >= dop.idx:
                    continue
                clock[dop.eng] = dop.idx
                sn = snaps[d]
                if sn:
                    for k, v in sn.items():
                        if clock.get(k, -1) < v:
                            clock[k] = v
            snaps[op.id] = dict(clock)


class Arena:
    def __init__(self, nc, nbytes, name="arena"):
        self.n = nbytes // 4
        self.t = nc.alloc_sbuf_tensor(name, [128, self.n], F32)
        self.off = 0
        self.peak = 0

    def reset(self, to=0):
        self.off = to

    def mark(self):
        return self.off

    def alloc(self, free_shape, dtype, parts=128):
        esz = 2 if dtype == BF16 else 4
        nel = int(np.prod(free_shape))
        nw = (nel * esz + 3) // 4
        nw = (nw + 7) // 8 * 8
        assert self.off + nw <= self.n, f"arena overflow {self.off}+{nw}>{self.n}"
        ap = self.t[0:parts, self.off:self.off + nw]
        self.off += nw
        self.peak = max(self.peak, self.off)
        if dtype != F32:
            ap = ap.bitcast(dtype)
        ap = ap[:, 0:nel]
        if len(free_shape) >= 2:
            names = [f"a{i}" for i in range(len(free_shape))]
            kw = {nm: int(v) for nm, v in zip(names[1:], free_shape[1:])}
            ap = ap.rearrange("p (" + " ".join(names) + ") -> p " + " ".join(names), **kw)
        return ap

U32 = mybir.dt.uint32
D = 1024
L = 256
EPS = 1e-6
O_AQ, O_AK, O_AV, O_BQ, O_BI, O_BZF, O_BZB, O_BG, O_CQ, O_CKV, O_CKR, O_GL = (
    0, 512, 640, 768, 1280, 1792, 2304, 2816, 3328, 3584, 3712, 3744)
IN_W = 6816
NEXP = 32


class H:
    def __init__(self, P):
        self.P = P

    def mm(self, out, lhsT, rhs, start, stop, reads, writes, tp=None):
        if tp is None:
            self.P.pe(lambda e: e.matmul(out, lhsT=lhsT, rhs=rhs, start=start, stop=stop), reads, writes)
        else:
            self.P.pe(lambda e: e.matmul(out, lhsT=lhsT, rhs=rhs, start=start, stop=stop, tile_position=tp), reads, writes)

    def tr(self, out, in_, ident, reads, writes):
        self.P.pe(lambda e: e.transpose(out=out, in_=in_, identity=ident), reads, writes)

    def act(self, out, in_, func, reads, writes, **kw):
        self.P.act(lambda e: e.activation(out=out, in_=in_, func=func, **kw), reads, writes)

    def tt(self, eng, out, in0, in1, op, reads, writes):
        self.P._add(eng, lambda e: e.tensor_tensor(out=out, in0=in0, in1=in1, op=op), reads, writes)

    def ts(self, eng, out, in0, s1, s2, op0, op1, reads, writes, **kw):
        if s2 is None:
            self.P._add(eng, lambda e: e.tensor_scalar(out=out, in0=in0, scalar1=s1, scalar2=None, op0=op0, **kw), reads, writes)
        else:
            self.P._add(eng, lambda e: e.tensor_scalar(out=out, in0=in0, scalar1=s1, scalar2=s2, op0=op0, op1=op1, **kw), reads, writes)

    def stt(self, eng, out, in0, scalar, in1, op0, op1, reads, writes):
        self.P._add(eng, lambda e: e.scalar_tensor_tensor(out=out, in0=in0, scalar=scalar, in1=in1, op0=op0, op1=op1), reads, writes)

    def cp(self, eng, out, in_, reads, writes):
        if eng == "act":
            self.P.act(lambda e: e.activation(out=out, in_=in_, func=AF.Identity), reads, writes)
        else:
            self.P._add(eng, lambda e: e.tensor_copy(out=out, in_=in_), reads, writes)

    def memset(self, eng, ap, val, writes):
        self.P._add(eng, lambda e: e.memset(ap, val), (), writes)

    def reduce(self, out, in_, op, reads, writes, axis=AX.X):
        self.P.dve(lambda e: e.tensor_reduce(out=out, in_=in_, axis=axis, op=op), reads, writes)


def build_program(S, dbg=False, layers=2, phases=None):
    T = L + S
    NT = T // 128
    nc = bass.Bass("TRN2", target_bir_lowering=False)
    P = Prog(nc)
    h = H(P)
    MM, TR, ACT, TT, TS, CP = h.mm, h.tr, h.act, h.tt, h.ts, h.cp
    LD = "sync"
    ST = "sync"

    def din(name, shape, dt=F32):
        return nc.dram_tensor(name, list(shape), dt, kind="ExternalInput").ap()

    dbg_names = []

    def dscr(name, shape, dt=BF16):
        if dbg:
            dbg_names.append(name)
            return nc.dram_tensor(name, list(shape), dt, kind="ExternalOutput").ap()
        return nc.dram_tensor(name, list(shape), dt).ap()

    xin = din("xin", [T, D])
    cvec = din("cvec", [128, 8, 2])
    w_mod = din("w_mod", [2, D, 6 * D])
    b_modT = din("b_modT", [2, 128, 48])
    b_mod = din("b_mod", [2, 6 * D])
    g1T = din("g1T", [2, 128, 8])
    g2T = din("g2T", [2, 128, 8])
    w_in = din("w_in", [2, D, IN_W])
    a_sink = din("a_sink", [2, 8])
    lb_logits = din("lb_logits", [2, 2, 512])
    b_onorm = din("b_onorm", [2, 512])
    gqT = din("gqT", [2, 128, 2])
    gkvT = din("gkvT", [2, 128, 1])
    w_uq = din("w_uq", [2, 256, 768])
    w_ukv = din("w_ukv", [2, 128, 1024])
    w_br = din("w_br", [2, 3, 512, D])
    w_out = din("w_out", [2, D, D])
    w_r = din("w_r", [2, D, 36])
    w1 = din("w1", [2, NEXP, D, 256])
    w3 = din("w3", [2, NEXP, D, 256])
    w2 = din("w2", [2, NEXP, 256, D])
    g_final = din("g_final", [D])
    ident_d = din("ident", [128, 128])
    ropeA = din("ropeA", [T, 4, 64])
    ropeC = din("ropeC", [T, 4, 32])
    amask = din("amask", [128, 2, 128])
    bmats = din("bmats", [64, 4, 64])
    bmask = din("bmask", [64, 8, 64], F32)
    brm = din("brm", [64, 2], F32)
    out = nc.dram_tensor("out", [S, D], F32, kind="ExternalOutput").ap()

    xs = dscr("xs", [T, D], F32)
    xs2 = dscr("xs2", [T, D], F32)
    wb_in = dscr("wb_in", [2, D, IN_W])
    wb_uq = dscr("wb_uq", [2, 256, 768])
    wb_ukv = dscr("wb_ukv", [2, 128, 1024])
    wb_br = dscr("wb_br", [2, 3, 512, D])
    wb_out = dscr("wb_out", [2, D, D])
    wb1 = dscr("wb1", [2, NEXP, D, 256])
    wb3 = dscr("wb3", [2, NEXP, D, 256])
    wb2 = dscr("wb2", [2, NEXP, 256, D])
    qaT = dscr("qaT", [512, T])
    kaT = dscr("kaT", [128, T])
    va = dscr("va", [T, 128])
    qbT = dscr("qbT", [512, T])
    ib = dscr("ib", [T, 512])
    zf = dscr("zf", [T, 512])
    zb = dscr("zb", [T, 512])
    sgb = dscr("sgb", [T, 512])
    qcT = dscr("qcT", [8, 96, T])
    kcT = dscr("kcT", [512, T])
    krT = dscr("krT", [32, T])
    vc = dscr("vc", [T, 512])
    gT = dscr("gT", [3072, T])
    oT = dscr("oT", [3, 512, T])
    ofb = dscr("ofb", [2, T, 512])
    h2T = dscr("h2T", [D, T])
    cmbd = dscr("cmbd", [T, 32], F32)
    gtd = dscr("gtd", [2, 2, 2, 1024], F32)

    AR = Arena(nc, 196 * 1024)
    ident = AR.alloc([128], F32)
    identb = AR.alloc([128], BF16)
    onesf = AR.alloc([128], F32)
    modA = AR.alloc([2, 2, 8, 2], F32)
    modB = AR.alloc([2, 2, 8, 2], F32)
    sexp = AR.alloc([2, 8], F32)
    s_const = P.slot("const")
    s_mod = P.slot("mod")
    persist_mark = AR.mark()

    PSB = [nc.alloc_psum_tensor(f"psb{i}", [128, 512], F32)[:, :] for i in range(8)]
    PSS = P.slots(8, "ps")
    for s__ in PSS:
        s__.excl = True
    ps_rr = [0]

    def ps():
        i = 2 + ps_rr[0] % 6
        ps_rr[0] += 1
        return PSB[i], PSS[i]

    acc_rr = [0]

    def psacc():
        i = acc_rr[0] % 2
        acc_rr[0] += 1
        return PSB[i], PSS[i]


    P.dma(ident, ident_d, writes=[s_const])
    CP("dve", identb, ident, [s_const], [s_const])
    h.memset("pool", onesf, 1.0, [s_const])
    P.dma(sexp, a_sink.rearrange("l h -> (l h)").unsqueeze(0).broadcast_to([128, 16]).rearrange("p (l h) -> p l h", h=8), writes=[s_const])
    ACT(sexp, sexp, AF.Exp, [s_const], [s_const])

    s_w = P.slot("wcast")
    if phases is None or "W" in phases:
        for l in range(layers):
            for r in range(8):
                P.dma(wb_in[l, r * 128:(r + 1) * 128, :], w_in[l, r * 128:(r + 1) * 128, :], q="pool")
            P.dma(wb_uq[l], w_uq[l], q="pool")
            P.dma(wb_ukv[l], w_ukv[l], q="pool")
            for n in range(3):
                for r in range(4):
                    P.dma(wb_br[l, n, r * 128:(r + 1) * 128, :], w_br[l, n, r * 128:(r + 1) * 128, :], q="pool")
            for r in range(8):
                P.dma(wb_out[l, r * 128:(r + 1) * 128, :], w_out[l, r * 128:(r + 1) * 128, :], q="pool")
            for e in range(NEXP):
                for r in range(0, 8, 4):
                    P.dma(wb1[l, e, r * 128:(r + 4) * 128, :], w1[l, e, r * 128:(r + 4) * 128, :], q="pool")
                    P.dma(wb3[l, e, r * 128:(r + 4) * 128, :], w3[l, e, r * 128:(r + 4) * 128, :], q="pool")
                P.dma(wb2[l, e], w2[l, e], q="pool")

    def phase_mod(l):
        AR.reset(persist_mark)
        cv = AR.alloc([8, 2], F32)
        scv = AR.alloc([8, 2], F32)
        screp = AR.alloc([8, 2, 128], F32)
        bm = AR.alloc([48], F32)
        g1 = AR.alloc([8], F32)
        g2 = AR.alloc([8], F32)
        modv = AR.alloc([48, 2], F32)
        brow = AR.alloc([2, 1024], F32)
        gst = AR.alloc([2, 1024], F32)
        s_gst = P.slots(2, "gst")
        wm = [AR.alloc([8, 1024], F32) for _ in range(2)]
        s_l = P.slot()
        s_wm = P.slots(2, "wm")
        P.dma(cv, cvec, writes=[s_l])
        P.dma(bm, b_modT[l], writes=[s_l])
        P.dma(g1, g1T[l], writes=[s_l])
        P.dma(g2, g2T[l], writes=[s_l])
        for w_i, c0 in enumerate((2048, 5120)):
            P.dma(brow[:, w_i, :], b_mod[l:l + 1, c0:c0 + 1024].broadcast_to([128, 1024]), writes=[s_l])
        ACT(scv, cv, AF.Silu, [s_l], [s_l])
        for k in range(8):
            for j in range(2):
                TS("dve", screp[:, k, j, :], onesf, scv[:, k, j:j + 1], None, ALU.mult, None, [s_l, s_const], [s_l])
        pmod, s_pmod = psacc()
        for g in range(6):
            b = g % 2
            P.dma(wm[b], w_mod[l, :, g * 1024:(g + 1) * 1024].rearrange("(k p) n -> p k n", p=128), writes=[s_wm[b]])
            for m in range(8):
                for k in range(8):
                    MM(pmod[:, (g * 8 + m) * 2:(g * 8 + m) * 2 + 2], wm[b][:, k, m * 128:(m + 1) * 128], scv[:, k, :], k == 0, k == 7,
                       [s_wm[b], s_l], [s_pmod])
            if g in (2, 5):
                w_i = 0 if g == 2 else 1
                for j in range(2):
                    for hf in range(2):
                        pg, s_pg = ps()
                        for k in range(8):
                            MM(pg, screp[:, k, j, :], wm[b][:, k, hf * 512:(hf + 1) * 512], k == 0, k == 7, [s_wm[b], s_l], [s_pg])
                        TT("dve", gst[:, j, hf * 512:(hf + 1) * 512], pg, brow[:, w_i, hf * 512:(hf + 1) * 512], ALU.add,
                           [s_pg, s_l], [s_gst[j]])
                    P.dma(gtd[l, w_i, j:j + 1, :], gst[0:1, j, :], reads=[s_gst[j]])
        TT("dve", modv, pmod[:, 0:96].rearrange("p (m j) -> p m j", j=2), bm.unsqueeze(2).broadcast_to([128, 48, 2]), ALU.add,
           [s_pmod, s_l], [s_l])
        for w_i, (sh0, sc0, g) in enumerate(((0, 8, g1), (24, 32, g2))):
            TS("dve", modA[:, l, w_i, :, :], modv[:, sc0:sc0 + 8, :], 1.0, None, ALU.add, None, [s_l], [s_mod])
            TT("dve", modA[:, l, w_i, :, :], modA[:, l, w_i, :, :], g.unsqueeze(2).broadcast_to([128, 8, 2]), ALU.mult, [s_l, s_mod], [s_mod])
            CP("dve", modB[:, l, w_i, :, :], modv[:, sh0:sh0 + 8, :], [s_l], [s_mod])

    def norm_mod_T(xt, s_x, ntile, l, which, jfun, hTb, s_hT, tmp, s_tmp, hTf=None, tok0=0):
        ms = tmp["ms"]
        for j in range(ntile):
            ACT(tmp["junk"], xt[:, j, :], AF.Square, [s_x], [s_tmp], scale=1.0 / 32.0, accum_out=ms[:, j:j + 1])
        TS("dve", ms[:, 0:ntile], ms[:, 0:ntile], EPS, None, ALU.add, None, [s_tmp], [s_tmp])
        ACT(ms[:, 0:ntile], ms[:, 0:ntile], AF.Ln, [s_tmp], [s_tmp])
        ACT(ms[:, 0:ntile], ms[:, 0:ntile], AF.Exp, [s_tmp], [s_tmp], scale=-0.5)
        for j in range(ntile):
            jj = jfun(j)
            if hTf is None:
                xn = tmp["xnb"]
                TS("dve", xn, xt[:, j, :], ms[:, j:j + 1], None, ALU.mult, None, [s_x, s_tmp], [s_tmp])
                pt, s_pt = ps()
                ptb = pt.bitcast(BF16)
                for k in range(8):
                    TR(ptb[:, k * 128:(k + 1) * 128], xn[:, k * 128:(k + 1) * 128], identb, [s_tmp, s_const], [s_pt])
                t2 = tmp["t2"]
                TT("dve", t2, ptb.rearrange("p (k t) -> p k t", t=128),
                   modA[:, l, which, :, jj:jj + 1].broadcast_to([128, 8, 128]), ALU.mult, [s_pt, s_mod], [s_tmp])
                TT("pool", hTb[:, :, tok0 + j * 128:tok0 + (j + 1) * 128], t2,
                   modB[:, l, which, :, jj:jj + 1].broadcast_to([128, 8, 128]), ALU.add, [s_tmp, s_mod], [s_hT])
            else:
                xn = tmp["xnf"]
                TS("dve", xn, xt[:, j, :], ms[:, j:j + 1], None, ALU.mult, None, [s_x, s_tmp], [s_tmp])
                for hf in range(2):
                    pt, s_pt = ps()
                    for k in range(4):
                        kk = hf * 4 + k
                        TR(pt[:, k * 128:(k + 1) * 128], xn[:, kk * 128:(kk + 1) * 128], ident, [s_tmp, s_const], [s_pt])
                    t2 = tmp["t2f"]
                    TT("dve", t2, pt.rearrange("p (k t) -> p k t", t=128),
                       modA[:, l, which, hf * 4:hf * 4 + 4, jj:jj + 1].broadcast_to([128, 4, 128]), ALU.mult, [s_pt, s_mod], [s_tmp])
                    TT("pool", hTf[:, hf * 4:hf * 4 + 4, j * 128:(j + 1) * 128], t2,
                       modB[:, l, which, hf * 4:hf * 4 + 4, jj:jj + 1].broadcast_to([128, 4, 128]), ALU.add, [s_tmp, s_mod], [s_hT])
                CP("act", hTb[:, :, tok0 + j * 128:tok0 + (j + 1) * 128], hTf[:, :, j * 128:(j + 1) * 128], [s_hT], [s_hT])

    def blocks(include_ctx=True, bs=512):
        bl = []
        if include_ctx:
            bl.append((0, L, True))
        t = L
        while t < T:
            n = min(bs, T - t)
            bl.append((t, n, False))
            t += n
        return bl


    NTM = O_GL

    def phase_A(l, xsrc):
        AR.reset(persist_mark)
        wsb = AR.alloc([8, NTM], BF16)
        wuq = AR.alloc([2, 768], BF16)
        wukv = AR.alloc([1024], BF16)
        gq = AR.alloc([2], F32)
        gkv = AR.alloc([1], F32)
        s_wsb = P.slot("wsb")
        for k in range(8):
            P.dma(wsb[:, k, :], wb_in[l, k * 128:(k + 1) * 128, 0:NTM], writes=[s_wsb])
        P.dma(wuq, wb_uq[l].rearrange("(k p) n -> p k n", p=128), writes=[s_wsb])
        P.dma(wukv, wb_ukv[l], writes=[s_wsb])
        P.dma(gq, gqT[l], writes=[s_wsb])
        P.dma(gkv, gkvT[l], writes=[s_wsb])

        def dbl(shape, dt):
            return [AR.alloc(shape, dt) for _ in range(2)]
        wg = dbl([8, 512], BF16)
        s_wg = P.slots(2, "wg")
        xt = dbl([1, 1024], F32)
        s_x = P.slots(2, "xt")
        hT = dbl([8, 512], BF16)
        s_hT = P.slots(2, "hT")
        rA = dbl([4, 64], F32)
        rC = dbl([4, 32], F32)
        s_r = P.slots(2, "rope")
        tmp = dict(ms=AR.alloc([8], F32), junk=AR.alloc([1024], F32), xnb=AR.alloc([1024], BF16), t2=AR.alloc([8, 128], BF16))
        tmp_q = AR.alloc([768], F32)
        s_tq = P.slot("tq")
        s_tmp = P.slot("tmpA")
        qa_tm = dbl([512], BF16); ka_tm = dbl([128], BF16)
        ropet = dbl([512], F32); ropeu = dbl([512], F32)
        va_s = dbl([128], BF16)
        ib_s = dbl([512], BF16); zf_s = dbl([512], BF16); zb_s = dbl([512], BF16); sg_s = dbl([512], BF16)
        cst = dbl([8], F32)
        cqn = dbl([256], BF16); ckvn = dbl([128], BF16); kr_tm = dbl([32], BF16)
        qc_tm = dbl([768], BF16)
        vc_s = dbl([512], BF16)
        qaT_s = AR.alloc([4, 512], BF16); kaT_s = AR.alloc([512], BF16); qbT_s = AR.alloc([4, 512], BF16)
        cqnT = AR.alloc([2, 512], BF16); ckvnT = AR.alloc([512], BF16); krT_s = AR.alloc([512], BF16)
        qcT_s = AR.alloc([8, 512], BF16); kcT_s = AR.alloc([4, 512], BF16)
        gT_s = dbl([4, 512], BF16)
        s_ev = {}

        def sl(name, par=0):
            key = (name, par)
            if key not in s_ev:
                s_ev[key] = P.slot(name + str(par))
            return s_ev[key]

        tix = 0
        for bi_, (t0, n, isctx) in enumerate(blocks()):
            bp = bi_ % 2
            nt = n // 128
            jj = 1 if isctx else 0
            rd = [s_hT[bp], s_wsb]
            for j in range(nt):
                par = tix % 2
                tix += 1
                tt0 = t0 + j * 128
                P.dma(xt[par][:, 0, :], xsrc[tt0:tt0 + 128, :], writes=[s_x[par]], q=LD)
                P.dma(rA[par], ropeA[tt0:tt0 + 128], writes=[s_r[par]], q=LD)
                P.dma(rC[par], ropeC[tt0:tt0 + 128], writes=[s_r[par]], q=LD)
                norm_mod_T(xt[par], s_x[par], 1, l, 0, lambda j_: jj, hT[bp], s_hT[bp], tmp, s_tmp, tok0=j * 128)

                def tm_group(c0, ncols):
                    pg, s_pg = ps()
                    for k in range(8):
                        MM(pg[:, 0:ncols], hT[bp][:, k, j * 128:(j + 1) * 128], wsb[:, k, c0:c0 + ncols], k == 0, k == 7, rd, [s_pg])
                    return pg, s_pg

                def rope(dst, src, s_src, nh, hd, tab, a0, dsl, eng2="pool"):
                    half = hd // 2
                    s3 = src.rearrange("p (h d) -> p h d", d=hd)
                    t3 = ropet[par][:, 0:nh * hd].rearrange("p (h d) -> p h d", d=hd)
                    u3 = ropeu[par][:, 0:nh * hd].rearrange("p (h d) -> p h d", d=hd)
                    s_t = sl("ropet", par)
                    TT("dve", t3, s3, tab[:, a0, :].unsqueeze(1).broadcast_to([128, nh, hd]), ALU.mult, [s_src, s_r[par]], [s_t])
                    TT("dve", u3[:, :, 0:half], s3[:, :, half:hd], tab[:, a0 + 1, 0:half].unsqueeze(1).broadcast_to([128, nh, half]),
                       ALU.mult, [s_src, s_r[par]], [s_t])
                    TT("dve", u3[:, :, half:hd], s3[:, :, 0:half], tab[:, a0 + 1, half:hd].unsqueeze(1).broadcast_to([128, nh, half]),
                       ALU.mult, [s_src, s_r[par]], [s_t])
                    TT(eng2, dst, ropet[par][:, 0:nh * hd], ropeu[par][:, 0:nh * hd], ALU.add, [s_t], [dsl])

                tsl = slice(tt0, tt0 + 128)
                import os as _os
                KA = int(_os.environ.get("KA", "9"))
                if KA < 2:
                    continue
                KB = int(_os.environ.get("KB", "15"))
                if KB & 1:
                    pg, s_pg = tm_group(O_AQ, 512)
                    rope(qa_tm[par], pg, s_pg, 8, 64, rA[par], 0, sl("qa_tm", par))
                if KB & 2:
                    pt, s_pt = ps()
                    ptb = pt.bitcast(BF16)
                    for c in range(4):
                        TR(ptb[:, c * 128:(c + 1) * 128], qa_tm[par][:, c * 128:(c + 1) * 128], identb, [sl("qa_tm", par), s_const], [s_pt])
                    CP("act", qaT_s[:, :, j * 128:(j + 1) * 128], ptb[:, 0:512].rearrange("p (c t) -> p c t", t=128), [s_pt], [sl("qaT_s")])
                if KB & 4:
                    pg, s_pg = tm_group(O_AK, 256)
                    rope(ka_tm[par], pg[:, 0:128], s_pg, 2, 64, rA[par], 2, sl("ka_tm", par))
                    KC = int(_os.environ.get("KC", "3"))
                    if KC >= 2:
                        CP("act", va_s[par], pg[:, 128:256], [s_pg], [sl("va_s", par)])
                    if KC >= 3:
                        P.dma(va[tsl, :], va_s[par], reads=[sl("va_s", par)], q=ST)
                if KB & 8:
                    pt, s_pt = ps()
                    ptb = pt.bitcast(BF16)
                    TR(ptb[:, 0:128], ka_tm[par], identb, [sl("ka_tm", par), s_const], [s_pt])
                    CP("act", kaT_s[:, j * 128:(j + 1) * 128], ptb[:, 0:128], [s_pt], [sl("kaT_s")])
                if KA < 3:
                    continue
                for c0, dst, nm, dd in ((O_BI, ib_s, "ib_s", ib), (O_BZF, zf_s, "zf_s", zf), (O_BZB, zb_s, "zb_s", zb)):
                    pg, s_pg = tm_group(c0, 512)
                    CP("dve" if nm != "zb_s" else "act", dst[par], pg, [s_pg], [sl(nm, par)])
                    P.dma(dd[tsl, :], dst[par], reads=[sl(nm, par)], q=ST)
                pg, s_pg = tm_group(O_BG, 512)
                ACT(sg_s[par], pg, AF.Silu, [s_pg], [sl("sg_s", par)])
                P.dma(sgb[tsl, :], sg_s[par], reads=[sl("sg_s", par)], q=ST)
                if KA < 4:
                    continue
                pg, s_pg = tm_group(O_CQ, 416)
                cs = cst[par]
                s_cs = sl("cst", par)
                ACT(tmp["junk"][:, 0:256], pg[:, 0:256], AF.Square, [s_pg], [s_tmp, s_cs], scale=1.0 / 16.0, accum_out=cs[:, 0:1])
                ACT(tmp["junk"][:, 0:128], pg[:, 256:384], AF.Square, [s_pg], [s_tmp, s_cs], scale=float(128 ** -0.5), accum_out=cs[:, 1:2])
                TS("dve", cs[:, 0:2], cs[:, 0:2], EPS, None, ALU.add, None, [s_cs], [s_cs])
                ACT(cs[:, 0:2], cs[:, 0:2], AF.Ln, [s_cs], [s_cs])
                ACT(cs[:, 0:2], cs[:, 0:2], AF.Exp, [s_cs], [s_cs], scale=-0.5)
                TS("dve", cqn[par], pg[:, 0:256], cs[:, 0:1], None, ALU.mult, None, [s_pg, s_cs], [sl("cqn", par)])
                TS("dve", ckvn[par], pg[:, 256:384], cs[:, 1:2], None, ALU.mult, None, [s_pg, s_cs], [sl("ckvn", par)])
                rope(kr_tm[par], pg[:, 384:416], s_pg, 1, 32, rC[par], 2, sl("kr_tm", par))
                pt, s_pt = ps()
                ptb = pt.bitcast(BF16)
                TR(ptb[:, 0:128], cqn[par][:, 0:128], identb, [sl("cqn", par), s_const], [s_pt])
                TR(ptb[:, 128:256], cqn[par][:, 128:256], identb, [sl("cqn", par), s_const], [s_pt])
                TR(ptb[:, 256:384], ckvn[par], identb, [sl("ckvn", par), s_const], [s_pt])
                TR(ptb[0:32, 384:512], kr_tm[par], identb, [sl("kr_tm", par), s_const], [s_pt])
                for c in range(2):
                    ACT(cqnT[:, c, j * 128:(j + 1) * 128], ptb[:, c * 128:(c + 1) * 128], AF.Identity, [s_pt, s_wsb], [sl("cqnT")],
                        scale=gq[:, c:c + 1])
                ACT(ckvnT[:, j * 128:(j + 1) * 128], ptb[:, 256:384], AF.Identity, [s_pt, s_wsb], [sl("ckvnT")], scale=gkv[:, 0:1])
                CP("dve", krT_s[0:32, j * 128:(j + 1) * 128], ptb[0:32, 384:512], [s_pt], [sl("krT_s")])
                if KA < 5:
                    continue
                pq0, s_pq0 = ps()
                pq1, s_pq1 = ps()
                for c in range(2):
                    MM(pq0, cqnT[:, c, j * 128:(j + 1) * 128], wuq[:, c, 0:512], c == 0, c == 1, [sl("cqnT"), s_wsb], [s_pq0])
                for c in range(2):
                    MM(pq1[:, 0:256], cqnT[:, c, j * 128:(j + 1) * 128], wuq[:, c, 512:768], c == 0, c == 1, [sl("cqnT"), s_wsb], [s_pq1])
                qscale = float(96 ** -0.5)
                q3 = qc_tm[par].rearrange("p (h d) -> p h d", d=96)
                s_qc = sl("qc_tm", par)
                CP("act", tmp_q[:, 0:512], pq0, [s_pq0], [s_tq])
                CP("act", tmp_q[:, 512:768], pq1[:, 0:256], [s_pq1], [s_tq])
                tq3 = tmp_q.rearrange("p (h d) -> p h d", d=96)
                TS("dve", q3[:, :, 0:64], tq3[:, :, 0:64], qscale, None, ALU.mult, None, [s_tq], [s_qc])
                t3 = ropet[par][:, 0:256].rearrange("p (h d) -> p h d", d=32)
                u3 = ropeu[par][:, 0:256].rearrange("p (h d) -> p h d", d=32)
                s_t = sl("ropet", par)
                TT("dve", t3, tq3[:, :, 64:96], rC[par][:, 0, :].unsqueeze(1).broadcast_to([128, 8, 32]), ALU.mult, [s_tq, s_r[par]], [s_t])
                TT("dve", u3[:, :, 0:16], tq3[:, :, 80:96], rC[par][:, 1, 0:16].unsqueeze(1).broadcast_to([128, 8, 16]), ALU.mult,
                   [s_tq, s_r[par]], [s_t])
                TT("dve", u3[:, :, 16:32], tq3[:, :, 64:80], rC[par][:, 1, 16:32].unsqueeze(1).broadcast_to([128, 8, 16]), ALU.mult,
                   [s_tq, s_r[par]], [s_t])
                TT("pool", q3[:, :, 64:96], t3, u3, ALU.add, [s_t], [s_qc])
                pt, s_pt = ps()
                ptb = pt.bitcast(BF16)
                for hh in range(8):
                    TR(ptb[0:96, hh * 128:(hh + 1) * 128], qc_tm[par][:, hh * 96:(hh + 1) * 96], identb, [s_qc, s_const], [s_pt])
                CP("act", qcT_s[0:96, :, j * 128:(j + 1) * 128], ptb[0:96, :].rearrange("p (h t) -> p h t", t=128), [s_pt], [sl("qcT_s")])
                pv, s_pv = ps()
                MM(pv, ckvnT[:, j * 128:(j + 1) * 128], wukv[:, 512:1024], True, True, [sl("ckvnT"), s_wsb], [s_pv])
                CP("dve", vc_s[par], pv, [s_pv], [sl("vc_s", par)])
                P.dma(vc[tsl, :], vc_s[par], reads=[sl("vc_s", par)], q=ST)
            bsl = slice(t0, t0 + n)
            if KA < 6:
                continue
            for c in range(4):
                pg, s_pg = ps()
                for k in range(8):
                    MM(pg[:, 0:n], wsb[:, k, O_BQ + c * 128:O_BQ + (c + 1) * 128], hT[bp][:, k, 0:n], k == 0, k == 7, rd, [s_pg])
                CP("act" if c % 2 else "dve", qbT_s[:, c, 0:n], pg[:, 0:n], [s_pg], [sl("qbT_s")])
            for c in range(4):
                pg, s_pg = ps()
                MM(pg[:, 0:n], wukv[:, c * 128:(c + 1) * 128], ckvnT[:, 0:n], True, True, [sl("ckvnT"), s_wsb], [s_pg])
                CP("dve", kcT_s[:, c, 0:n], pg[:, 0:n], [s_pg], [sl("kcT_s")])
            P.dma(qaT[:, bsl].rearrange("(c p) t -> p c t", p=128), qaT_s[:, :, 0:n], reads=[sl("qaT_s")], q=ST)
            P.dma(kaT[:, bsl], kaT_s[:, 0:n], reads=[sl("kaT_s")], q=ST)
            P.dma(qbT[:, bsl].rearrange("(c p) t -> p c t", p=128), qbT_s[:, :, 0:n], reads=[sl("qbT_s")], q=ST)
            P.dma(qcT[:, :, bsl].rearrange("h d t -> d h t"), qcT_s[0:96, :, 0:n], reads=[sl("qcT_s")], q=ST)
            P.dma(kcT[:, bsl].rearrange("(c p) t -> p c t", p=128), kcT_s[:, :, 0:n], reads=[sl("kcT_s")], q=ST)
            P.dma(krT[:, bsl], krT_s[0:32, 0:n], reads=[sl("krT_s")], q=ST)
            for g in range(6):
                gp = g % 2
                P.dma(wg[gp], wb_in[l, :, O_GL + g * 512:O_GL + (g + 1) * 512].rearrange("(k p) n -> p k n", p=128), writes=[s_wg[gp]], q=LD)
                for c in range(4):
                    pg, s_pg = ps()
                    for k in range(8):
                        MM(pg[:, 0:n], wg[gp][:, k, c * 128:(c + 1) * 128], hT[bp][:, k, 0:n], k == 0, k == 7, [s_hT[bp], s_wg[gp]], [s_pg])
                    ACT(gT_s[gp][:, c, 0:n], pg[:, 0:n], AF.Sigmoid, [s_pg], [sl("gT_s", gp)])
                P.dma(gT[g * 512:(g + 1) * 512, bsl].rearrange("(c p) t -> p c t", p=128), gT_s[gp][:, :, 0:n], reads=[sl("gT_s", gp)], q=ST)

    def att_finalize(po, s_po, n, sink_ap, dst_dram, tmpo, rdt, s_fin, eng_dma=ST, nh=1):
        if sink_ap is not None:
            TT("dve", rdt[64:65, 0:n].rearrange("p (g t) -> p g t", g=nh), po[64:65, 0:n].rearrange("p (g t) -> p g t", g=nh),
               sink_ap, ALU.add, [s_po, s_const], [s_fin])
            P.dve(lambda e: e.reciprocal(out=rdt[64:65, 0:n], in_=rdt[64:65, 0:n]), [s_fin], [s_fin])
        else:
            P.dve(lambda e: e.reciprocal(out=rdt[64:65, 0:n], in_=po[64:65, 0:n]), [s_po], [s_fin])
        pb, s_pb = ps()
        MM(pb[0:64, 0:n], onesf[64:65, 0:64], rdt[64:65, 0:n], True, True, [s_fin, s_const], [s_pb])
        CP("act", tmpo[0:64, 0:n], po[0:64, 0:n], [s_po], [s_fin])
        o16 = tmpo[0:64, 512:1024].bitcast(BF16)[:, 0:n]
        TT("dve", o16, tmpo[0:64, 0:n], pb[0:64, 0:n], ALU.mult, [s_fin, s_pb], [s_fin])
        P.dma(dst_dram, o16 if nh == 1 else o16.rearrange("p (g t) -> p g t", g=nh), reads=[s_fin], q=eng_dma)

    def phase_attA(l, with_ctx):
        AR.reset(persist_mark)
        kT = AR.alloc([2, T], BF16)
        vt = AR.alloc([NT, 2, 65], BF16)
        msk = AR.alloc([2, 128], BF16)
        mskf = AR.alloc([2, 128], F32)
        s_kv = P.slot("attA_kv")
        for kvh in range(2):
            P.dma(kT[0:64, kvh, :], kaT[kvh * 64:(kvh + 1) * 64, :], writes=[s_kv])
        h.memset("pool", vt, 1.0, [s_kv])
        for kvh in range(2):
            P.dma(vt[:, :, kvh, 0:64], va[:, kvh * 64:(kvh + 1) * 64].rearrange("(j p) d -> p j d", p=128), writes=[s_kv])
        P.dma(mskf, amask, writes=[s_kv])
        CP("dve", msk, mskf, [s_kv], [s_kv])
        qt = [AR.alloc([8, 128], BF16) for _ in range(2)]
        s_q = P.slots(2, "attA_q")
        pT = [AR.alloc([512], BF16) for _ in range(4)]
        s_pT = P.slots(4, "attA_pT")
        pcnt_ = [0]
        tmpo = [AR.alloc([1024], F32) for _ in range(2)]
        rdt = [AR.alloc([512], F32) for _ in range(2)]
        s_fin = P.slots(2, "attA_fin")
        nlat = S // 128
        qtiles = ([0, 1] if with_ctx else []) + list(range(2, NT))
        cnt = 0
        pcnt = 0
        for qi_, gi in enumerate(qtiles):
            qp = qi_ % 2
            P.dma(qt[qp][0:64], qaT[:, gi * 128:(gi + 1) * 128].rearrange("(h d) t -> d h t", d=64), writes=[s_q[qp]], q=LD)
            if gi < 2:
                keys = [(0, None), (1, None)]
            else:
                nq = gi - 2
                keys = [(0, None), (1, None)]
                if nq >= 1:
                    keys.append((gi - 1, 0))
                keys.append((gi, None))
                if nq + 1 < nlat:
                    keys.append((gi + 1, 1))
            for kvh in range(2):
                po, s_po = psacc()
                LA = 2
                ppl = {}

                def qk(ki, kvh=kvh, qp=qp, keys=keys, ppl=ppl):
                    kt, mk = keys[ki]
                    pss, s_pss = ps()
                    MM(pss, kT[0:64, kvh, kt * 128:(kt + 1) * 128], qt[qp][0:64, kvh * 4:(kvh + 1) * 4, :], True, True,
                       [s_kv, s_q[qp]], [s_pss])
                    pp = pcnt_[0] % 4
                    pcnt_[0] += 1
                    ACT(pT[pp], pss, AF.Exp, [s_pss], [s_pT[pp]])
                    if mk is not None:
                        p3 = pT[pp].rearrange("p (g t) -> p g t", g=4)
                        TT("pool", p3, p3, msk[:, mk, :].unsqueeze(1).broadcast_to([128, 4, 128]), ALU.mult, [s_pT[pp], s_kv], [s_pT[pp]])
                    ppl[ki] = pp
                for ki in range(min(LA, len(keys))):
                    qk(ki)
                for ki, (kt, mk) in enumerate(keys):
                    if ki + LA < len(keys):
                        qk(ki + LA)
                    pp = ppl.pop(ki)
                    MM(po[0:65, :], vt[:, kt, kvh, :], pT[pp], ki == 0, ki == len(keys) - 1, [s_kv, s_pT[pp]], [s_po])
                fp = cnt % 2
                cnt += 1
                att_finalize(po, s_po, 512, sexp[64:65, l, kvh * 4:(kvh + 1) * 4].unsqueeze(2).broadcast_to([1, 4, 128]),
                             oT[0, kvh * 256:(kvh + 1) * 256, gi * 128:(gi + 1) * 128].rearrange("(g d) t -> d g t", d=64),
                             tmpo[fp], rdt[fp], s_fin[fp], nh=4)

    def phase_attC(l, with_ctx):
        AR.reset(persist_mark)
        kT = [AR.alloc([T], BF16) for _ in range(2)]
        vt = [AR.alloc([NT, 65], BF16) for _ in range(2)]
        s_kv = P.slots(2, "attC_kv")
        qt = [AR.alloc([512], BF16) for _ in range(2)]
        s_q = P.slots(2, "attC_q")
        pT = [AR.alloc([512], BF16) for _ in range(4)]
        s_pT = P.slots(4, "attC_pT")
        pcnt_ = [0]
        tmpo = [AR.alloc([1024], F32) for _ in range(2)]
        rdt = [AR.alloc([512], F32) for _ in range(2)]
        s_fin = P.slots(2, "attC_fin")
        qblocks = blocks(include_ctx=with_ctx)
        cnt = 0
        pcnt = 0
        for hh in range(8):
            hp = hh % 2
            P.dma(kT[hp][0:64, :], kcT[hh * 64:(hh + 1) * 64, :], writes=[s_kv[hp]], q=LD)
            P.dma(kT[hp][64:96, :], krT, writes=[s_kv[hp]], q=LD)
            h.memset("pool", vt[hp], 1.0, [s_kv[hp]])
            P.dma(vt[hp][:, :, 0:64], vc[:, hh * 64:(hh + 1) * 64].rearrange("(j p) d -> p j d", p=128), writes=[s_kv[hp]], q=LD)
            for (t0, n, isctx) in qblocks:
                qp = cnt % 2
                cnt += 1
                P.dma(qt[qp][0:96, 0:n], qcT[hh, :, t0:t0 + n], writes=[s_q[qp]], q=LD)
                keys = [0, 1] if isctx else list(range(NT))
                po, s_po = psacc()
                LA = 2
                ppl = {}

                def qk(ki, n=n, hp=hp, qp=qp, keys=keys, ppl=ppl):
                    kt = keys[ki]
                    pss, s_pss = ps()
                    MM(pss[:, 0:n], kT[hp][0:96, kt * 128:(kt + 1) * 128], qt[qp][0:96, 0:n], True, True, [s_kv[hp], s_q[qp]], [s_pss])
                    pp = pcnt_[0] % 4
                    pcnt_[0] += 1
                    ACT(pT[pp][:, 0:n], pss[:, 0:n], AF.Exp, [s_pss], [s_pT[pp]])
                    ppl[ki] = pp
                for ki in range(min(LA, len(keys))):
                    qk(ki)
                for ki, kt in enumerate(keys):
                    if ki + LA < len(keys):
                        qk(ki + LA)
                    pp = ppl.pop(ki)
                    MM(po[0:65, 0:n], vt[hp][:, kt, :], pT[pp][:, 0:n], ki == 0, ki == len(keys) - 1, [s_kv[hp], s_pT[pp]], [s_po])
                att_finalize(po, s_po, n, None, oT[2, hh * 64:(hh + 1) * 64, t0:t0 + n], tmpo[qp], rdt[qp], s_fin[qp])

    def phase_B(l):
        AR.reset(persist_mark)
        C = 32
        R2 = 2 * C
        NCH = T // C
        GS = 4
        bm = AR.alloc([4, R2], F32)
        bmk = AR.alloc([8, R2], F32)
        rmk = AR.alloc([2], F32)
        lbt = AR.alloc([512], F32)
        c1t = AR.alloc([512], F32)
        l0 = AR.alloc([512], F32)
        s_c = P.slot("B_const")
        P.dma(bm[0:R2], bmats, writes=[s_c])
        P.dma(bmk[0:R2], bmask, writes=[s_c])
        P.dma(rmk[0:R2], brm, writes=[s_c])
        if l == 0:
            h.memset("pool", lbt, 0.0, [s_c])
            h.memset("pool", c1t, 1.0, [s_c])
        else:
            for d_ in range(2):
                P.dma(l0[d_ * C:(d_ + 1) * C, :], lb_logits[0, d_:d_ + 1, :].broadcast_to([C, 512]), writes=[s_c])
                P.dma(lbt[d_ * C:(d_ + 1) * C, :], lb_logits[1, d_:d_ + 1, :].broadcast_to([C, 512]), writes=[s_c])
            TT("dve", lbt[0:R2], lbt[0:R2], l0[0:R2], ALU.subtract, [s_c], [s_c])
            ACT(lbt[0:R2], lbt[0:R2], AF.Sigmoid, [s_c], [s_c])
            TS("dve", c1t[0:R2], lbt[0:R2], -1.0, 1.0, ALU.mult, ALU.add, [s_c], [s_c])
        Sst = AR.alloc([2, 8, 64], F32)
        Sb = AR.alloc([2, 8, 64], BF16)
        s_S = P.slot("B_S")
        s_Sb = P.slot("B_Sb")
        h.memset("pool", Sst, 0.0, [s_S])
        h.memset("pool", Sb, 0.0, [s_Sb])

        def dbl(shape, dt, n=2):
            return [AR.alloc(shape, dt) for _ in range(n)]
        z2 = dbl([GS, 512], BF16); v2 = dbl([GS, 512], BF16); q2 = dbl([8, GS, R2], BF16)
        s_z = P.slots(2, "B_z"); s_v = P.slots(2, "B_v"); s_q2 = P.slots(2, "B_q")
        sig = AR.alloc([GS, 512], F32); logf = dbl([GS, 512], F32); kk = dbl([GS, 512], F32)
        s_sig = P.slot("B_sig"); s_logf = P.slots(2, "B_logf"); s_kk = P.slots(2, "B_kk")
        ek = dbl([512], F32); e2 = dbl([512], F32); ktl = dbl([512], BF16); kh = dbl([2, 512], BF16)
        s_ek = P.slots(2, "B_ek"); s_e2 = P.slots(2, "B_e2"); s_kt = P.slots(2, "B_kt"); s_kh = P.slots(2, "B_kh")
        ktT = dbl([8, R2], BF16); s_ktT = P.slots(2, "B_ktT")
        eq = dbl([8, R2], F32); eqm = dbl([8, R2], F32); s_eq = P.slots(2, "B_eq"); s_eqm = P.slots(2, "B_eqm")
        qebf = dbl([8, R2], BF16); qebb = dbl([8, R2], BF16); qtl = dbl([8, R2], BF16)
        s_qe = P.slots(2, "B_qe"); s_qt = P.slots(2, "B_qt")
        attT = dbl([8, R2], BF16); s_att = P.slots(2, "B_att")
        o_s = dbl([GS, 512], BF16); s_os = P.slots(2, "B_os")
        for b_ in range(2):
            h.memset("pool", qebf[b_], 0.0, [s_qe[b_]])
            h.memset("pool", qebb[b_], 0.0, [s_qe[b_]])
        step = 0
        nctx = L // C
        for g in range(NCH // GS):
            gp = g % 2
            cf0 = g * GS
            gctx = nctx // GS
            cb0 = (nctx - GS * (g + 1)) if g < gctx else NCH - GS * (g - gctx + 1)
            fsl = slice(cf0 * C, (cf0 + GS) * C)
            P.dma(z2[gp][0:C], zf[fsl, :].rearrange("(s p) f -> p s f", p=C), writes=[s_z[gp]], q=LD)
            P.dma(v2[gp][0:C], ib[fsl, :].rearrange("(s p) f -> p s f", p=C), writes=[s_v[gp]], q=LD)
            for s_ in range(GS):
                cb = cb0 + GS - 1 - s_
                cf = cf0 + s_
                P.dma(z2[gp][C:R2, s_, :], zb[cb * C:(cb + 1) * C, :], writes=[s_z[gp]], q=LD)
                P.dma(v2[gp][C:R2, s_, :], ib[cb * C:(cb + 1) * C, :], writes=[s_v[gp]], q=LD)
                P.dma(q2[gp][0:64, :, s_, 0:C], qbT[:, cf * C:(cf + 1) * C].rearrange("(h d) t -> d h t", d=64), writes=[s_q2[gp]], q=LD)
                P.dma(q2[gp][0:64, :, s_, C:R2], qbT[:, cb * C:(cb + 1) * C].rearrange("(h d) t -> d h t", d=64), writes=[s_q2[gp]], q=LD)
            z2f = z2[gp][0:R2].rearrange("p s f -> p (s f)")
            sigf = sig[0:R2].rearrange("p s f -> p (s f)")
            ACT(sigf, z2f, AF.Sigmoid, [s_z[gp]], [s_sig])
            TT("dve", sig[0:R2], sig[0:R2], c1t[0:R2].unsqueeze(1).broadcast_to([R2, GS, 512]), ALU.mult, [s_sig, s_c], [s_sig])
            TT("dve", sig[0:R2], sig[0:R2], lbt[0:R2].unsqueeze(1).broadcast_to([R2, GS, 512]), ALU.add, [s_sig, s_c], [s_sig])
            ACT(logf[gp][0:R2].rearrange("p s f -> p (s f)"), sigf, AF.Ln, [s_sig], [s_logf[gp]])
            TS("pool", kk[gp][0:R2].rearrange("p s f -> p (s f)"), sigf, -1.0, 1.0, ALU.mult, ALU.add, [s_sig], [s_kk[gp]])
            for s_ in range(GS):
                sp = step % 2
                step += 1
                lf = logf[gp][0:R2, s_, :]
                pe1, s_pe1 = ps()
                MM(pe1[0:R2], bm[0:R2, 0, :], lf, True, True, [s_c, s_logf[gp]], [s_pe1])
                ACT(ek[sp][0:R2], pe1[0:R2], AF.Exp, [s_pe1], [s_ek[sp]])
                TT("dve", ktl[sp][0:R2], kk[gp][0:R2, s_, :], ek[sp][0:R2], ALU.mult, [s_kk[gp], s_ek[sp]], [s_kt[sp]])
                pe2, s_pe2 = ps()
                MM(pe2[0:R2], bm[0:R2, 1, :], lf, True, True, [s_c, s_logf[gp]], [s_pe2])
                ACT(e2[sp][0:R2], pe2[0:R2], AF.Exp, [s_pe2], [s_e2[sp]])
                TT("pool", e2[sp][0:R2], kk[gp][0:R2, s_, :], e2[sp][0:R2], ALU.mult, [s_kk[gp], s_e2[sp]], [s_e2[sp]])
                for d_ in range(2):
                    TS("pool", kh[sp][0:R2, d_, :], e2[sp][0:R2], rmk[0:R2, d_:d_ + 1], None, ALU.mult, None, [s_e2[sp], s_c], [s_kh[sp]])
                pt, s_pt = ps()
                ptb = pt.bitcast(BF16)
                for hh in range(8):
                    TR(ptb[0:64, hh * R2:(hh + 1) * R2], ktl[sp][0:R2, hh * 64:(hh + 1) * 64], identb[0:R2, 0:R2], [s_kt[sp], s_const], [s_pt])
                CP("act", ktT[sp][0:64], ptb[0:64, 0:8 * R2].rearrange("p (c t) -> p c t", t=R2), [s_pt], [s_ktT[sp]])
                pbT, s_pbT = ps()
                for hh in range(8):
                    MM(pbT[0:64, hh * R2:(hh + 1) * R2], lf[:, hh * 64:(hh + 1) * 64], bm[0:R2, 2, :], True, True, [s_c, s_logf[gp]], [s_pbT])
                ACT(eq[sp][0:64], pbT[0:64, 0:8 * R2].rearrange("p (c t) -> p c t", t=R2), AF.Exp, [s_pbT], [s_eq[sp]])
                pbm, s_pbm = ps()
                for hh in range(8):
                    MM(pbm[0:64, hh * R2:(hh + 1) * R2], lf[:, hh * 64:(hh + 1) * 64], bm[0:R2, 3, :], True, True, [s_c, s_logf[gp]], [s_pbm])
                ACT(eqm[sp][0:64], pbm[0:64, 0:8 * R2].rearrange("p (c t) -> p c t", t=R2), AF.Exp, [s_pbm], [s_eqm[sp]])
                qs = q2[gp][0:64, :, s_, :]
                TT("dve", qebf[sp][0:64, :, 0:C], qs[:, :, 0:C], eq[sp][0:64, :, 0:C], ALU.mult, [s_q2[gp], s_eq[sp]], [s_qe[sp]])
                TT("dve", qebb[sp][0:64, :, C:R2], qs[:, :, C:R2], eq[sp][0:64, :, C:R2], ALU.mult, [s_q2[gp], s_eq[sp]], [s_qe[sp]])
                TT("pool", qtl[sp][0:64], qs, eqm[sp][0:64], ALU.mult, [s_q2[gp], s_eqm[sp]], [s_qt[sp]])
                pa, s_pa = ps()
                for hh in range(8):
                    MM(pa[0:R2, hh * R2:(hh + 1) * R2], ktT[sp][0:64, hh, :], qtl[sp][0:64, hh, :], True, True, [s_ktT[sp], s_qt[sp]], [s_pa])
                TT("dve", attT[sp][0:R2], pa[0:R2, 0:8 * R2].rearrange("p (h t) -> p h t", t=R2), bmk[0:R2], ALU.mult,
                   [s_pa, s_c], [s_att[sp]])
                po, s_po = psacc()
                for hh in range(8):
                    osl = po[0:R2, hh * 64:(hh + 1) * 64]
                    MM(osl, attT[sp][0:R2, hh, :], v2[gp][0:R2, s_, hh * 64:(hh + 1) * 64], True, False, [s_att[sp], s_v[gp]], [s_po])
                    MM(osl, qebf[sp][0:64, hh, :], Sb[0:64, 0, hh, :], False, False, [s_qe[sp], s_Sb], [s_po])
                    MM(osl, qebb[sp][0:64, hh, :], Sb[0:64, 1, hh, :], False, True, [s_qe[sp], s_Sb], [s_po])
                CP("act", o_s[gp][0:R2, s_, :], po[0:R2], [s_po], [s_os[gp]])
                for d_ in range(2):
                    pS, s_pS = ps()
                    for hh in range(8):
                        MM(pS[0:64, hh * 64:(hh + 1) * 64], kh[sp][0:R2, d_, hh * 64:(hh + 1) * 64], v2[gp][0:R2, s_, hh * 64:(hh + 1) * 64], True, True,
                           [s_kh[sp], s_v[gp]], [s_pS])
                    Sv = Sst[0:64, d_]
                    TT("dve", Sv, Sv, eq[sp][0:64, :, C - 1 + d_:C + d_].broadcast_to([64, 8, 64]), ALU.mult, [s_S, s_eq[sp]], [s_S])
                    TT("dve", Sv, Sv, pS[0:64].rearrange("p (h x) -> p h x", x=64), ALU.add, [s_S, s_pS], [s_S])
                CP("pool", Sb[0:64], Sst[0:64], [s_S], [s_Sb])
            P.dma(ofb[0, fsl, :].rearrange("(s p) f -> p s f", p=C), o_s[gp][0:C], reads=[s_os[gp]], q=ST)
            for s_ in range(GS):
                cb = cb0 + GS - 1 - s_
                P.dma(ofb[1, cb * C:(cb + 1) * C, :], o_s[gp][C:R2, s_, :], reads=[s_os[gp]], q=ST)
        P.barrier()
        gon = AR.alloc([512], F32)
        s_g = P.slot("B_gon")
        P.dma(gon, b_onorm[l:l + 1, :].broadcast_to([128, 512]), writes=[s_g])
        of_ = dbl([512], BF16); ob_ = dbl([512], BF16); sg_ = dbl([512], BF16)
        s_in = P.slots(2, "Bf_in")
        osum = dbl([512], F32); osq = dbl([512], F32); st = dbl([8], F32); on_ = dbl([512], BF16); oTs = dbl([4, 128], BF16)
        s_w2 = P.slots(2, "Bf_w"); s_oTs = P.slots(2, "Bf_oT")
        for j in range(NT):
            p_ = j % 2
            tsl = slice(j * 128, (j + 1) * 128)
            P.dma(of_[p_], ofb[0, tsl, :], writes=[s_in[p_]], q=LD)
            P.dma(ob_[p_], ofb[1, tsl, :], writes=[s_in[p_]], q=LD)
            P.dma(sg_[p_], sgb[tsl, :], writes=[s_in[p_]], q=LD)
            TT("dve", osum[p_], of_[p_], ob_[p_], ALU.add, [s_in[p_]], [s_w2[p_]])
            ACT(osq[p_], osum[p_], AF.Square, [s_w2[p_]], [s_w2[p_]], scale=0.125)
            h.reduce(st[p_], osq[p_].rearrange("p (h d) -> p h d", d=64), ALU.add, [s_w2[p_]], [s_w2[p_]])
            TS("dve", st[p_], st[p_], EPS, None, ALU.add, None, [s_w2[p_]], [s_w2[p_]])
            ACT(st[p_], st[p_], AF.Ln, [s_w2[p_]], [s_w2[p_]])
            ACT(st[p_], st[p_], AF.Exp, [s_w2[p_]], [s_w2[p_]], scale=-0.5)
            o3 = osum[p_].rearrange("p (h d) -> p h d", d=64)
            TT("dve", o3, o3, st[p_].unsqueeze(2).broadcast_to([128, 8, 64]), ALU.mult, [s_w2[p_]], [s_w2[p_]])
            TT("pool", osum[p_], osum[p_], gon, ALU.mult, [s_w2[p_], s_g], [s_w2[p_]])
            TT("pool", on_[p_], osum[p_], sg_[p_], ALU.mult, [s_w2[p_], s_in[p_]], [s_w2[p_]])
            pt, s_pt = ps()
            ptb = pt.bitcast(BF16)
            for c in range(4):
                TR(ptb[:, c * 128:(c + 1) * 128], on_[p_][:, c * 128:(c + 1) * 128], identb, [s_w2[p_], s_const], [s_pt])
            CP("act", oTs[p_], ptb[:, 0:512].rearrange("p (c t) -> p c t", t=128), [s_pt], [s_oTs[p_]])
            P.dma(oT[1, :, tsl].rearrange("(c p) t -> p c t", p=128), oTs[p_], reads=[s_oTs[p_]], q=ST)

    def phase_merge(l, xsrc, with_ctx):
        AR.reset(persist_mark)
        wbr = AR.alloc([3, 4, 1024], BF16)
        wout = AR.alloc([8, 1024], BF16)
        wr = AR.alloc([8, 36], F32)
        gt1 = AR.alloc([2, 1024], F32)
        s_wm = P.slot("M_w")
        for n_ in range(3):
            P.dma(wbr[:, n_], wb_br[l, n_].rearrange("(k p) n -> p k n", p=128), writes=[s_wm])
        P.dma(wout, wb_out[l].rearrange("(k p) n -> p k n", p=128), writes=[s_wm])
        P.dma(wr, w_r[l].rearrange("(k p) n -> p k n", p=128), writes=[s_wm])
        for j_ in range(2):
            P.dma(gt1[:, j_, :], gtd[l, 0, j_:j_ + 1, :].broadcast_to([128, 1024]), writes=[s_wm])
        oT3 = [AR.alloc([3, 4, 512], BF16) for _ in range(2)]
        gT3 = [AR.alloc([24, 512], BF16) for _ in range(2)]
        s_in = P.slots(2, "M_in")
        yT = AR.alloc([8, 512], BF16)
        s_yT = P.slot("M_yT")
        acc = [AR.alloc([512], F32) for _ in range(2)]
        tm1 = [AR.alloc([512], F32) for _ in range(2)]
        s_acc = P.slots(2, "M_acc")
        xt = [AR.alloc([1, 1024], F32) for _ in range(2)]
        xnew = [AR.alloc([1, 1024], F32) for _ in range(2)]
        s_x = P.slots(2, "M_x")
        s_xn = P.slots(2, "M_xn")
        h2f = AR.alloc([8, 128], F32)
        h2b = [AR.alloc([8, 512], BF16) for _ in range(2)]
        _sh2 = P.slot("M_h2")
        s_h2 = [_sh2, _sh2]
        s_h2f = P.slot("M_h2f")
        tmp = dict(ms=AR.alloc([8], F32), junk=AR.alloc([1024], F32), xnf=AR.alloc([1024], F32), t2f=AR.alloc([4, 128], F32))
        s_tmp = P.slot("M_tmp")
        R = {k: AR.alloc([n_], F32) for k, n_ in dict(lg=36, oh=4, ge=4, esel=8, es2=8, eq1=8, eq2=8, sc=8, csel=8).items()}
        cmb = [AR.alloc([32], F32) for _ in range(2)]
        s_R = P.slot("M_R")
        s_cmb = P.slots(2, "M_cmb")
        tix = 0
        for bi_, (t0, n, isctx) in enumerate(blocks(include_ctx=with_ctx)):
            bp = bi_ % 2
            nt = n // 128
            jj = 1 if isctx else 0
            bsl = slice(t0, t0 + n)
            for br in range(3):
                P.dma(oT3[bp][:, br, :, 0:n], oT[br, :, bsl].rearrange("(k p) t -> p k t", p=128), writes=[s_in[bp]], q=LD)
            P.dma(gT3[bp][:, :, 0:n], gT[:, bsl].rearrange("(c p) t -> p c t", p=128), writes=[s_in[bp]], q=LD)
            for m in range(8):
                pbs = []
                for br in range(3):
                    pb, s_pb = ps()
                    for k in range(4):
                        MM(pb[:, 0:n], wbr[:, br, k, m * 128:(m + 1) * 128], oT3[bp][:, br, k, 0:n], k == 0, k == 3, [s_wm, s_in[bp]], [s_pb])
                    pbs.append((pb, s_pb))
                ap_ = m % 2
                TT("dve", acc[ap_][:, 0:n], pbs[0][0][:, 0:n], gT3[bp][:, m, 0:n], ALU.mult, [pbs[0][1], s_in[bp]], [s_acc[ap_]])
                TT("dve", tm1[ap_][:, 0:n], pbs[1][0][:, 0:n], gT3[bp][:, 8 + m, 0:n], ALU.mult, [pbs[1][1], s_in[bp]], [s_acc[ap_]])
                TT("pool", acc[ap_][:, 0:n], acc[ap_][:, 0:n], tm1[ap_][:, 0:n], ALU.add, [s_acc[ap_]], [s_acc[ap_]])
                TT("dve", tm1[ap_][:, 0:n], pbs[2][0][:, 0:n], gT3[bp][:, 16 + m, 0:n], ALU.mult, [pbs[2][1], s_in[bp], s_acc[ap_]], [s_acc[ap_]])
                TT("pool", yT[:, m, 0:n], acc[ap_][:, 0:n], tm1[ap_][:, 0:n], ALU.add, [s_acc[ap_]], [s_yT])
            for j in range(nt):
                par = tix % 2
                tix += 1
                tsl = slice(t0 + j * 128, t0 + (j + 1) * 128)
                P.dma(xt[par][:, 0, :], xsrc[tsl, :], writes=[s_x[par]], q=LD)
                for hf in range(2):
                    pz, s_pz = ps()
                    for k in range(8):
                        MM(pz, yT[:, k, j * 128:(j + 1) * 128], wout[:, k, hf * 512:(hf + 1) * 512], k == 0, k == 7, [s_yT, s_wm], [s_pz])
                    TT("dve", xnew[par][:, 0, hf * 512:(hf + 1) * 512], pz, gt1[:, jj, hf * 512:(hf + 1) * 512], ALU.mult, [s_pz, s_wm], [s_xn[par]])
                TT("pool", xnew[par][:, 0, :], xnew[par][:, 0, :], xt[par][:, 0, :], ALU.add, [s_xn[par], s_x[par]], [s_xn[par]])
                P.dma(xs2[tsl, :], xnew[par][:, 0, :], reads=[s_xn[par]], q=ST)
                norm_mod_T(xnew[par], s_xn[par], 1, l, 1, lambda j_: jj, h2b[bp], s_h2[bp], tmp, s_tmp, hTf=h2f, tok0=j * 128)
                pr_, s_pr = ps()
                for k in range(8):
                    MM(pr_[:, 0:36], h2f[:, k, :], wr[:, k, :], k == 0, k == 7, [s_h2[bp], s_wm], [s_pr])
                lg = R["lg"]
                CP("dve", lg, pr_[:, 0:36], [s_pr], [s_R])
                sc_ = R["sc"]
                rs = [s_R]
                h.reduce(sc_[:, 0:1], lg[:, 0:4], ALU.max, rs, rs)
                TS("dve", R["oh"], lg[:, 0:4], sc_[:, 0:1], None, ALU.is_equal, None, rs, rs)
                TS("dve", sc_[:, 1:2], sc_[:, 0:1], -1.0, None, ALU.mult, None, rs, rs)
                ACT(R["ge"], lg[:, 0:4], AF.Exp, rs, rs, bias=sc_[:, 1:2], accum_out=sc_[:, 2:3])
                P.dve(lambda e, o_=sc_[:, 2:3]: e.reciprocal(out=o_, in_=o_), rs, rs)
                el = lg[:, 4:36].rearrange("p (g e) -> p g e", e=8)
                TS("dve", R["esel"], el[:, 0, :], R["oh"][:, 0:1], None, ALU.mult, None, rs, rs)
                for g_ in range(1, 4):
                    h.stt("dve", R["esel"], el[:, g_, :], R["oh"][:, g_:g_ + 1], R["esel"], ALU.mult, ALU.add, rs, rs)
                h.reduce(sc_[:, 3:4], R["esel"], ALU.max, rs, rs)
                TS("dve", R["eq1"], R["esel"], sc_[:, 3:4], None, ALU.is_equal, None, rs, rs)
                h.stt("dve", R["es2"], R["eq1"], -1e30, R["esel"], ALU.mult, ALU.add, rs, rs)
                h.reduce(sc_[:, 4:5], R["es2"], ALU.max, rs, rs)
                TS("dve", R["eq2"], R["es2"], sc_[:, 4:5], None, ALU.is_equal, None, rs, rs)
                TS("dve", sc_[:, 5:6], sc_[:, 3:4], -1.0, None, ALU.mult, None, rs, rs)
                ACT(sc_[:, 6:7], sc_[:, 4:5], AF.Exp, rs, rs, bias=sc_[:, 5:6])
                TS("dve", sc_[:, 7:8], sc_[:, 6:7], 1.0, None, ALU.add, None, rs, rs)
                P.dve(lambda e, o_=sc_[:, 7:8]: e.reciprocal(out=o_, in_=o_), rs, rs)
                TT("dve", sc_[:, 7:8], sc_[:, 7:8], sc_[:, 2:3], ALU.mult, rs, rs)
                TT("dve", sc_[:, 6:7], sc_[:, 6:7], sc_[:, 7:8], ALU.mult, rs, rs)
                TS("dve", R["csel"], R["eq1"], sc_[:, 7:8], None, ALU.mult, None, rs, rs)
                h.stt("dve", R["csel"], R["eq2"], sc_[:, 6:7], R["csel"], ALU.mult, ALU.add, rs, rs)
                c3 = cmb[par].rearrange("p (g e) -> p g e", e=8)
                for g_ in range(4):
                    TS("dve", c3[:, g_, :], R["csel"], R["oh"][:, g_:g_ + 1], None, ALU.mult, None, rs, [s_cmb[par]])
                P.dma(cmbd[tsl, :], cmb[par], reads=[s_cmb[par]], q=ST)
            P.dma(h2T[:, bsl].rearrange("(k p) t -> p k t", p=128), h2b[bp][:, :, 0:n], reads=[s_h2[bp]], q=ST)

    def phase_moe(l, with_ctx, last):
        AR.reset(persist_mark)
        BS = 2048
        gt2 = AR.alloc([2, 1024], F32)
        gfin = AR.alloc([1024], F32)
        s_c = P.slot("E_c")
        for j_ in range(2):
            P.dma(gt2[:, j_, :], gtd[l, 1, j_:j_ + 1, :].broadcast_to([128, 1024]), writes=[s_c])
        P.dma(gfin, g_final.unsqueeze(0).broadcast_to([128, 1024]), writes=[s_c])
        hb = AR.alloc([8, BS], BF16)
        cm = AR.alloc([BS // 128, 32], F32)
        yacc = AR.alloc([BS // 128, 1024], F32)
        s_hb = P.slot("E_hb"); s_y = P.slots(BS // 128, "E_y")
        w1e = [AR.alloc([8, 256], BF16) for _ in range(2)]
        w3e = [AR.alloc([8, 256], BF16) for _ in range(2)]
        w2e = [AR.alloc([2, 1024], BF16) for _ in range(2)]
        s_we = P.slots(2, "E_w")
        su = [AR.alloc([512], F32) for _ in range(2)]
        aT = [AR.alloc([2, 512], BF16) for _ in range(2)]
        s_su = P.slots(2, "E_su"); s_aT = P.slots(2, "E_aT")
        xt = [AR.alloc([1024], F32) for _ in range(2)]
        xo = [AR.alloc([1024], F32) for _ in range(2)]
        st = [AR.alloc([2], F32) for _ in range(2)]
        junk = AR.alloc([1024], F32)
        s_x = P.slots(2, "E_x"); s_xo = P.slots(2, "E_xo")
        for (t0, n, isctx) in blocks(include_ctx=with_ctx, bs=BS):
            nt = n // 128
            jj = 1 if isctx else 0
            bsl = slice(t0, t0 + n)
            P.dma(hb[:, :, 0:n], h2T[:, bsl].rearrange("(k p) t -> p k t", p=128), writes=[s_hb], q=LD)
            P.dma(cm[:, 0:nt, :], cmbd[bsl, :].rearrange("(j p) e -> p j e", p=128), writes=[s_hb], q=LD)
            for j in range(nt):
                h.memset("pool", yacc[:, j, :], 0.0, [s_y[j]])
            items = [(e_, sb0, min(512, n - sb0)) for e_ in range(NEXP) for sb0 in range(0, n, 512)]
            loaded = set()

            def uv(ii):
                e_, sb0, nn = items[ii]
                ep = e_ % 2
                ap_ = ii % 2
                if e_ not in loaded:
                    loaded.add(e_)
                    P.dma(w1e[ep], wb1[l, e_].rearrange("(k p) n -> p k n", p=128), writes=[s_we[ep]], q=LD)
                    P.dma(w3e[ep], wb3[l, e_].rearrange("(k p) n -> p k n", p=128), writes=[s_we[ep]], q=LD)
                    P.dma(w2e[ep], wb2[l, e_].rearrange("(k p) n -> p k n", p=128), writes=[s_we[ep]], q=LD)
                for cc in range(2):
                    pu, s_pu = ps()
                    for k in range(8):
                        MM(pu[:, 0:nn], w1e[ep][:, k, cc * 128:(cc + 1) * 128], hb[:, k, sb0:sb0 + nn], k == 0, k == 7, [s_we[ep], s_hb], [s_pu])
                    pv, s_pv = ps()
                    for k in range(8):
                        MM(pv[:, 0:nn], w3e[ep][:, k, cc * 128:(cc + 1) * 128], hb[:, k, sb0:sb0 + nn], k == 0, k == 7, [s_we[ep], s_hb], [s_pv])
                    ACT(su[cc][:, 0:nn], pu[:, 0:nn], AF.Silu, [s_pu], [s_su[cc]])
                    TT("dve", aT[ap_][:, cc, 0:nn], su[cc][:, 0:nn], pv[:, 0:nn], ALU.mult, [s_su[cc], s_pv], [s_aT[ap_]])

            def yy(ii):
                e_, sb0, nn = items[ii]
                ep = e_ % 2
                ap_ = ii % 2
                for j in range(nn // 128):
                    tj = sb0 // 128 + j
                    for hf in range(2):
                        py, s_py = ps()
                        for cc in range(2):
                            MM(py, aT[ap_][:, cc, j * 128:(j + 1) * 128], w2e[ep][:, cc, hf * 512:(hf + 1) * 512], cc == 0, cc == 1,
                               [s_aT[ap_], s_we[ep]], [s_py])
                        ysl = yacc[:, tj, hf * 512:(hf + 1) * 512]
                        h.stt("dve", ysl, py, cm[:, tj, e_:e_ + 1], ysl, ALU.mult, ALU.add, [s_py, s_hb, s_y[tj]], [s_y[tj]])

            uv(0)
            for ii in range(len(items)):
                if ii + 1 < len(items):
                    uv(ii + 1)
                yy(ii)
            for j in range(nt):
                p_ = j % 2
                tsl = slice(t0 + j * 128, t0 + (j + 1) * 128)
                P.dma(xt[p_], xs2[tsl, :], writes=[s_x[p_]], q=LD)
                TT("dve", xo[p_], yacc[:, j, :], gt2[:, jj, :], ALU.mult, [s_y[j], s_c], [s_xo[p_]])
                TT("pool", xo[p_], xo[p_], xt[p_], ALU.add, [s_xo[p_], s_x[p_]], [s_xo[p_]])
                if not last:
                    P.dma(xs[tsl, :], xo[p_], reads=[s_xo[p_]], q=ST)
                elif not isctx:
                    ACT(junk, xo[p_], AF.Square, [s_xo[p_]], [s_xo[p_]], scale=1.0 / 32.0, accum_out=st[p_][:, 0:1])
                    TS("dve", st[p_][:, 0:1], st[p_][:, 0:1], EPS, None, ALU.add, None, [s_xo[p_]], [s_xo[p_]])
                    ACT(st[p_][:, 0:1], st[p_][:, 0:1], AF.Ln, [s_xo[p_]], [s_xo[p_]])
                    ACT(st[p_][:, 0:1], st[p_][:, 0:1], AF.Exp, [s_xo[p_]], [s_xo[p_]], scale=-0.5)
                    h.stt("dve", xo[p_], xo[p_], st[p_][:, 0:1], gfin, ALU.mult, ALU.mult, [s_xo[p_], s_c], [s_xo[p_]])
                    final_ops.append(P.dma(out[t0 - L + j * 128:t0 - L + (j + 1) * 128, :], xo[p_], reads=[s_xo[p_]], q=ST))

    final_ops = []
    P.barrier()

    def on(name):
        return phases is None or name in phases

    for l in range(layers):
        last = (l == layers - 1)
        if on("mod"):
            phase_mod(l)
            P.barrier()
        if on("A"):
            phase_A(l, xin if l == 0 else xs)
            P.barrier()
        if on("attA"):
            phase_attA(l, not last)
            P.barrier()
        if on("attC"):
            phase_attC(l, not last)
            P.barrier()
        if on("B"):
            phase_B(l)
            P.barrier()
        if on("merge"):
            phase_merge(l, xin if l == 0 else xs, not last)
            P.barrier()
        if on("moe"):
            phase_moe(l, not last, last)
            P.barrier()
    if not final_ops:
        final_ops = [P.barrier()]
    P.emit(final_wait_ops=final_ops)
    return nc, dbg_names


def _rope_tables(S, rot_dim, qscale):
    rows = S // 64
    row = np.repeat(np.arange(rows, dtype=np.float32), 64)
    col = np.tile(np.arange(64, dtype=np.float32), rows)
    n_freq = rot_dim // 4
    inv = (10000.0 ** (-np.arange(n_freq, dtype=np.float32) / n_freq)).astype(np.float32)
    ang = np.concatenate([row[:, None] * inv, col[:, None] * inv], axis=-1).astype(np.float32)
    c, s_ = np.cos(ang).astype(np.float32), np.sin(ang).astype(np.float32)
    T = L + S
    tab = np.zeros((T, 4, rot_dim), np.float32)
    c2 = np.concatenate([c, c], -1)
    s2 = np.concatenate([-s_, s_], -1)
    tab[L:, 0] = c2 * qscale
    tab[L:, 1] = s2 * qscale
    tab[L:, 2] = c2
    tab[L:, 3] = s2
    tab[:L, 0] = qscale
    tab[:L, 2] = 1.0
    return tab


def _consts(S):
    c = {}
    c["ident"] = np.eye(128, dtype=np.float32)
    c["ropeA"] = _rope_tables(S, 64, 64 ** -0.5)
    c["ropeC"] = _rope_tables(S, 32, 96 ** -0.5)
    j = np.arange(128)[:, None]
    i = np.arange(128)[None, :]
    am = np.zeros((128, 2, 128), np.float32)
    am[:, 0] = (j >= i)
    am[:, 1] = (j <= i)
    c["amask"] = am
    CC = 32
    mid = CC // 2 - 1
    s_ = np.arange(CC)[:, None]
    t_ = np.arange(CC)[None, :]
    bm = np.zeros((2 * CC, 4, 2 * CC), np.float32)
    cmk_f = (s_ <= mid).astype(np.float32) - (s_ <= t_)
    cmk_b = (s_ >= CC - 1 - mid).astype(np.float32) - (s_ >= t_)
    bm[:CC, 0, :CC] = cmk_f; bm[CC:, 0, CC:] = cmk_b
    bm[:CC, 1, :CC] = (s_ > t_); bm[CC:, 1, CC:] = (s_ < t_)
    bm[:CC, 2, :CC] = (s_ <= t_); bm[CC:, 2, CC:] = (s_ >= t_)
    bm[:CC, 3, :CC] = -cmk_f; bm[CC:, 3, CC:] = -cmk_b
    c["bmats"] = bm
    mk = np.zeros((2 * CC, 8, 2 * CC), np.float32)
    mk[:CC, :, :CC] = (s_ <= t_)[:, None, :]
    mk[CC:, :, CC:] = (s_ >= t_)[:, None, :]
    c["bmask"] = mk
    rm = np.zeros((2 * CC, 2), np.float32)
    rm[:CC, 0] = 1.0
    rm[CC:, 1] = 1.0
    c["brm"] = rm
    return c


def _fm(v, k):
    return np.ascontiguousarray(np.swapaxes(v.reshape(v.shape[:-1] + (k, 128)), -1, -2))


def make_in_map(inp, b, S):
    f = lambda a: np.ascontiguousarray(np.asarray(a, dtype=np.float32))
    m = {}
    m["xin"] = np.concatenate([f(inp["ctx"])[b], f(inp["x"])[b, :S]], axis=0)
    m["cvec"] = np.ascontiguousarray(np.stack([_fm(f(inp["c"])[b], 8), _fm(f(inp["c_ctx"]), 8)], axis=-1))
    m["w_mod"] = f(inp["w_mod"])
    m["b_modT"] = _fm(f(inp["b_mod"]), 48)
    m["b_mod"] = f(inp["b_mod"])
    m["g1T"] = _fm(f(inp["g_norm1"]), 8)
    m["g2T"] = _fm(f(inp["g_norm2"]), 8)
    m["w_in"] = f(inp["w_in"])
    m["a_sink"] = f(inp["a_sink"])
    m["lb_logits"] = f(inp["b_lb_logits"])
    m["b_onorm"] = f(inp["b_onorm"])
    m["gqT"] = _fm(f(inp["c_qnorm"]), 2)
    m["gkvT"] = _fm(f(inp["c_kvnorm"]), 1)
    m["w_uq"] = f(inp["w_uq"])
    wk = f(inp["w_ukv"]).reshape(2, 128, 8, 128)
    m["w_ukv"] = np.ascontiguousarray(np.concatenate([wk[..., :64].reshape(2, 128, 512), wk[..., 64:].reshape(2, 128, 512)], axis=-1))
    m["w_br"] = f(inp["w_br"])
    m["w_out"] = f(inp["w_out"])
    m["w_r"] = np.ascontiguousarray(np.concatenate([f(inp["w_rg"]), f(inp["w_re"])], axis=-1))
    m["w1"] = f(inp["w1"]); m["w3"] = f(inp["w3"]); m["w2"] = f(inp["w2"])
    m["g_final"] = f(inp["g_final"])
    return m


_CACHE = {}


def kernel(**inputs):
    x = np.asarray(inputs["x"])
    B, S, _ = x.shape
    if S not in _CACHE:
        _CACHE[S] = (build_program(S)[0], _consts(S))
    nc, consts = _CACHE[S]
    in_maps = []
    for b in range(B):
        m = make_in_map(inputs, b, S)
        m.update(consts)
        in_maps.append(m)
    res = run_bass_kernel_spmd(nc, in_maps, core_ids=list(range(B)))
    return np.stack([np.asarray(r["out"], dtype=np.float32) for r in res.results], axis=0)
```

```python
from concourse.bass_utils import run_bass_kernel_spmd
import ml_dtypes

import numpy as np
import concourse.bass as bass
import concourse.mybir as mybir

F32 = mybir.dt.float32
BF16 = mybir.dt.bfloat16
AF = mybir.ActivationFunctionType
ALU = mybir.AluOpType
AX = mybir.AxisListType

COMPUTE = ("pe", "act", "dve", "pool")
NDMA_SEMS = 24
SEM_EPOCH = 30000


class Slot:
    __slots__ = ("name", "writers", "readers", "excl")

    def __init__(self, name):
        self.name = name
        self.excl = False
        self.writers = {}
        self.readers = {}


class Op:
    __slots__ = ("id", "eng", "fn", "deps", "is_dma", "idx", "signal", "queue", "dma_no")

    def __init__(self, id, eng, fn, is_dma, queue):
        self.id = id
        self.eng = eng
        self.fn = fn
        self.deps = []
        self.is_dma = is_dma
        self.queue = queue
        self.signal = False
        self.idx = -1
        self.dma_no = -1


class Prog:
    def __init__(self, nc):
        self.nc = nc
        self.ops = []
        self.queues = {k: [] for k in ("pe", "act", "dve", "pool", "sync")}
        self.nslot = 0
        self.cur_barrier = None
        self.last_barrier_pos = 0

    def barrier(self):
        op = Op(len(self.ops), "sync", lambda e: e.nop(), False, "sync")
        self.ops.append(op)
        q = self.queues["sync"]
        op.idx = len(q)
        q.append(op)
        deps = set()
        if self.cur_barrier is not None:
            deps.add(self.cur_barrier)
        for qn, qq in self.queues.items():
            nd = 0
            got_c = False
            for o in reversed(qq[:-1] if qn == "sync" else qq):
                if o.is_dma:
                    if nd < NDMA_SEMS:
                        deps.add(o.id)
                        nd += 1
                elif not got_c:
                    deps.add(o.id)
                    got_c = True
                if nd >= NDMA_SEMS and got_c:
                    break
        op.deps = sorted(deps)
        self.cur_barrier = op.id
        return op

    def slot(self, name=None):
        self.nslot += 1
        return Slot(name or f"s{self.nslot}")

    def slots(self, n, name=None):
        return [self.slot(f"{name}{i}") for i in range(n)]

    def _add(self, eng, fn, reads, writes, is_dma=False):
        op = Op(len(self.ops), eng, fn, is_dma, eng)
        self.ops.append(op)
        q = self.queues[eng]
        op.idx = len(q)
        q.append(op)
        key = ("dma", op.id) if is_dma else eng
        deps = set()
        xs_ = [s for s in reads if s.excl]
        if xs_:
            writes = list(writes) + xs_
        for s in reads:
            for k, oid in s.writers.items():
                deps.add(oid)
        for s in writes:
            for k, oid in s.writers.items():
                deps.add(oid)
            for k, oid in s.readers.items():
                deps.add(oid)
        deps.discard(op.id)
        if self.cur_barrier is not None:
            deps.add(self.cur_barrier)
        for s in reads:
            s.readers[key] = op.id
        for s in writes:
            s.writers = {key: op.id}
            s.readers = {}
        op.deps = sorted(deps)
        return op

    def pe(self, fn, reads=(), writes=()):
        return self._add("pe", fn, reads, writes)

    def act(self, fn, reads=(), writes=()):
        return self._add("act", fn, reads, writes)

    def dve(self, fn, reads=(), writes=()):
        return self._add("dve", fn, reads, writes)

    def pool(self, fn, reads=(), writes=()):
        return self._add("pool", fn, reads, writes)

    def dma(self, out, in_, reads=(), writes=(), q="sync", **kw):
        return self._add(q, lambda e: e.dma_start(out=out, in_=in_, **kw), reads, writes, is_dma=True)

    def emit(self, final_wait_ops=()):
        nc = self.nc
        ops = self.ops
        for op in ops:
            for d in op.deps:
                dop = ops[d]
                if dop.is_dma:
                    continue
                if dop.eng == "pe" and op.eng == "pe" and not op.is_dma:
                    continue
                dop.signal = True
        for o in final_wait_ops:
            if not o.is_dma:
                o.signal = True
        sems = {}
        sigval = {}
        for qn, q in self.queues.items():
            cnt = 0
            ep = 0
            for op in q:
                if op.is_dma or not op.signal:
                    continue
                if cnt >= SEM_EPOCH:
                    cnt = 0
                    ep += 1
                cnt += 1
                sigval[op.id] = (qn, ep, cnt)
                if (qn, ep) not in sems:
                    sems[(qn, ep)] = nc.alloc_semaphore(f"s_{qn}_{ep}")
        dma_sems = {}
        dma_cnt = {}
        for qn, q in self.queues.items():
            n = 0
            for op in q:
                if op.is_dma:
                    op.dma_no = n
                    n += 1
            dma_cnt[qn] = n
            if n:
                dma_sems[qn] = [nc.alloc_semaphore(f"d_{qn}_{i}") for i in range(min(n, NDMA_SEMS))]
        dma_ops = {qn: [op for op in q if op.is_dma] for qn, q in self.queues.items()}

        def dma_sem_val(op):
            return dma_sems[op.queue][op.dma_no % NDMA_SEMS], 16 * (op.dma_no // NDMA_SEMS + 1)

        snaps = [None] * len(ops)
        self.nwaits = 0

        def run_queue(qn, eng):
            q = self.queues[qn]
            clock = {}
            known_dma = set()

            def need(d):
                dop = ops[d]
                if dop.is_dma:
                    if d in known_dma:
                        return
                    s, v = dma_sem_val(dop)
                    eng.wait_ge(s, v)
                    self.nwaits += 1
                    known_dma.add(d)
                    return
                if clock.get(dop.eng, -1) >= dop.idx:
                    return
                _, ep, cnt = sigval[d]
                eng.wait_ge(sems[(dop.eng, ep)], cnt)
                self.nwaits += 1
                clock[dop.eng] = dop.idx
                sn = snaps[d]
                if sn:
                    for k, v in sn.items():
                        if clock.get(k, -1) < v:
                            clock[k] = v

            for op in q:
                for d in op.deps:
                    dop = ops[d]
                    if (not dop.is_dma) and dop.eng == "pe" and qn == "pe" and not op.is_dma:
                        continue
                    need(d)
                if op.is_dma and op.dma_no >= NDMA_SEMS:
                    need(dma_ops[qn][op.dma_no - NDMA_SEMS].id)
                snaps[op.id] = dict(clock)
                ins = op.fn(eng)
                if op.is_dma:
                    s, v = dma_sem_val(op)
                    ins.then_inc(s, 16)
                elif op.signal:
                    _, ep, cnt = sigval[op.id]
                    ins.then_inc(sems[(qn, ep)], 1)
            if qn == "sync":
                for o in final_wait_ops:
                    need(o.id)

        self._dry_snapshots(ops, sigval, snaps)

        with nc.Block() as block:
            @block.tensor
            def _(e):
                run_queue("pe", e)

            @block.scalar
            def _(e):
                run_queue("act", e)

            @block.vector
            def _(e):
                run_queue("dve", e)

            @block.gpsimd
            def _(e):
                run_queue("pool", e)

            @block.sync
            def _(e):
                run_queue("sync", e)

    def _dry_snapshots(self, ops, sigval, snaps):
        clocks = {qn: {} for qn in self.queues}
        for op in ops:
            clock = clocks[op.queue]
            for d in op.deps:
                dop = ops[d]
                if dop.is_dma:
                    continue
                if dop.eng == "pe" and op.queue == "pe" and not op.is_dma:
                    continue
                if clock.get(dop.eng, -1) >= dop.idx:
                    continue
                clock[dop.eng] = dop.idx
                sn = snaps[d]
                if sn:
                    for k, v in sn.items():
                        if clock.get(k, -1) < v:
                            clock[k] = v
            snaps[op.id] = dict(clock)


class Arena:
    def __init__(self, nc, nbytes, name="arena"):
        self.n = nbytes // 4
        self.t = nc.alloc_sbuf_tensor(name, [128, self.n], F32)
        self.off = 0
        self.peak = 0

    def reset(self, to=0):
        self.off = to

    def mark(self):
        return self.off

    def alloc(self, free_shape, dtype, parts=128):
        esz = 2 if dtype == BF16 else 4
        nel = int(np.prod(free_shape))
        nw = (nel * esz + 3) // 4
        nw = (nw + 7) // 8 * 8
        assert self.off + nw <= self.n, f"arena overflow {self.off}+{nw}>{self.n}"
        ap = self.t[0:parts, self.off:self.off + nw]
        self.off += nw
        self.peak = max(self.peak, self.off)
        if dtype != F32:
            ap = ap.bitcast(dtype)
        ap = ap[:, 0:nel]
        if len(free_shape) >= 2:
            names = [f"a{i}" for i in range(len(free_shape))]
            kw = {nm: int(v) for nm, v in zip(names[1:], free_shape[1:])}
            ap = ap.rearrange("p (" + " ".join(names) + ") -> p " + " ".join(names), **kw)
        return ap

U32 = mybir.dt.uint32
D = 1024
L = 256
EPS = 1e-6
O_AQ, O_AK, O_AV, O_BQ, O_BI, O_BZF, O_BZB, O_BG, O_CQ, O_CKV, O_CKR, O_GL = (
    0, 512, 640, 768, 1280, 1792, 2304, 2816, 3328, 3584, 3712, 3744)
IN_W = 6816
NEXP = 32


class H:
    def __init__(self, P):
        self.P = P

    def mm(self, out, lhsT, rhs, start, stop, reads, writes, tp=None):
        if tp is None:
            self.P.pe(lambda e: e.matmul(out, lhsT=lhsT, rhs=rhs, start=start, stop=stop), reads, writes)
        else:
            self.P.pe(lambda e: e.matmul(out, lhsT=lhsT, rhs=rhs, start=start, stop=stop, tile_position=tp), reads, writes)

    def tr(self, out, in_, ident, reads, writes):
        self.P.pe(lambda e: e.transpose(out=out, in_=in_, identity=ident), reads, writes)

    def act(self, out, in_, func, reads, writes, **kw):
        self.P.act(lambda e: e.activation(out=out, in_=in_, func=func, **kw), reads, writes)

    def tt(self, eng, out, in0, in1, op, reads, writes):
        self.P._add(eng, lambda e: e.tensor_tensor(out=out, in0=in0, in1=in1, op=op), reads, writes)

    def ts(self, eng, out, in0, s1, s2, op0, op1, reads, writes, **kw):
        if s2 is None:
            self.P._add(eng, lambda e: e.tensor_scalar(out=out, in0=in0, scalar1=s1, scalar2=None, op0=op0, **kw), reads, writes)
        else:
            self.P._add(eng, lambda e: e.tensor_scalar(out=out, in0=in0, scalar1=s1, scalar2=s2, op0=op0, op1=op1, **kw), reads, writes)

    def stt(self, eng, out, in0, scalar, in1, op0, op1, reads, writes):
        self.P._add(eng, lambda e: e.scalar_tensor_tensor(out=out, in0=in0, scalar=scalar, in1=in1, op0=op0, op1=op1), reads, writes)

    def cp(self, eng, out, in_, reads, writes):
        if eng == "act":
            self.P.act(lambda e: e.activation(out=out, in_=in_, func=AF.Identity), reads, writes)
        else:
            self.P._add(eng, lambda e: e.tensor_copy(out=out, in_=in_), reads, writes)

    def memset(self, eng, ap, val, writes):
        self.P._add(eng, lambda e: e.memset(ap, val), (), writes)

    def reduce(self, out, in_, op, reads, writes, axis=AX.X):
        self.P.dve(lambda e: e.tensor_reduce(out=out, in_=in_, axis=axis, op=op), reads, writes)


def build_program(S, dbg=False, layers=2, phases=None):
    T = L + S
    NT = T // 128
    nc = bass.Bass("TRN2", target_bir_lowering=False)
    P = Prog(nc)
    h = H(P)
    MM, TR, ACT, TT, TS, CP = h.mm, h.tr, h.act, h.tt, h.ts, h.cp
    LD = "sync"
    ST = "sync"

    def din(name, shape, dt=F32):
        return nc.dram_tensor(name, list(shape), dt, kind="ExternalInput").ap()

    dbg_names = []

    def dscr(name, shape, dt=BF16):
        if dbg:
            dbg_names.append(name)
            return nc.dram_tensor(name, list(shape), dt, kind="ExternalOutput").ap()
        return nc.dram_tensor(name, list(shape), dt).ap()

    xin = din("xin", [T, D])
    cvec = din("cvec", [128, 8, 2])
    w_mod = din("w_mod", [2, D, 6 * D])
    b_modT = din("b_modT", [2, 128, 48])
    b_mod = din("b_mod", [2, 6 * D])
    g1T = din("g1T", [2, 128, 8])
    g2T = din("g2T", [2, 128, 8])
    w_in = din("w_in", [2, D, IN_W])
    a_sink = din("a_sink", [2, 8])
    lb_logits = din("lb_logits", [2, 2, 512])
    b_onorm = din("b_onorm", [2, 512])
    gqT = din("gqT", [2, 128, 2])
    gkvT = din("gkvT", [2, 128, 1])
    w_uq = din("w_uq", [2, 256, 768])
    w_ukv = din("w_ukv", [2, 128, 1024])
    w_br = din("w_br", [2, 3, 512, D])
    w_out = din("w_out", [2, D, D])
    w_r = din("w_r", [2, D, 36])
    w1 = din("w1", [2, NEXP, D, 256])
    w3 = din("w3", [2, NEXP, D, 256])
    w2 = din("w2", [2, NEXP, 256, D])
    g_final = din("g_final", [D])
    ident_d = din("ident", [128, 128])
    ropeA = din("ropeA", [T, 4, 64])
    ropeC = din("ropeC", [T, 4, 32])
    amask = din("amask", [128, 2, 128])
    bmats = din("bmats", [64, 4, 64])
    bmask = din("bmask", [64, 8, 64], F32)
    brm = din("brm", [64, 2], F32)
    out = nc.dram_tensor("out", [S, D], F32, kind="ExternalOutput").ap()

    xs = dscr("xs", [T, D], F32)
    xs2 = dscr("xs2", [T, D], F32)
    wb_in = dscr("wb_in", [2, D, IN_W])
    wb_uq = dscr("wb_uq", [2, 256, 768])
    wb_ukv = dscr("wb_ukv", [2, 128, 1024])
    wb_br = dscr("wb_br", [2, 3, 512, D])
    wb_out = dscr("wb_out", [2, D, D])
    wb1 = dscr("wb1", [2, NEXP, D, 256])
    wb3 = dscr("wb3", [2, NEXP, D, 256])
    wb2 = dscr("wb2", [2, NEXP, 256, D])
    qaT = dscr("qaT", [512, T])
    kaT = dscr("kaT", [128, T])
    va = dscr("va", [T, 128])
    qbT = dscr("qbT", [512, T])
    ib = dscr("ib", [T, 512])
    zf = dscr("zf", [T, 512])
    zb = dscr("zb", [T, 512])
    sgb = dscr("sgb", [T, 512])
    qcT = dscr("qcT", [8, 96, T])
    kcT = dscr("kcT", [512, T])
    krT = dscr("krT", [32, T])
    vc = dscr("vc", [T, 512])
    gT = dscr("gT", [3072, T])
    oT = dscr("oT", [3, 512, T])
    ofb = dscr("ofb", [2, T, 512])
    h2T = dscr("h2T", [D, T])
    cmbd = dscr("cmbd", [T, 32], F32)
    gtd = dscr("gtd", [2, 2, 2, 1024], F32)

    AR = Arena(nc, 196 * 1024)
    ident = AR.alloc([128], F32)
    identb = AR.alloc([128], BF16)
    onesf = AR.alloc([128], F32)
    modA = AR.alloc([2, 2, 8, 2], F32)
    modB = AR.alloc([2, 2, 8, 2], F32)
    sexp = AR.alloc([2, 8], F32)
    s_const = P.slot("const")
    s_mod = P.slot("mod")
    persist_mark = AR.mark()

    PSB = [nc.alloc_psum_tensor(f"psb{i}", [128, 512], F32)[:, :] for i in range(8)]
    PSS = P.slots(8, "ps")
    for s__ in PSS:
        s__.excl = True
    ps_rr = [0]

    def ps():
        i = 2 + ps_rr[0] % 6
        ps_rr[0] += 1
        return PSB[i], PSS[i]

    acc_rr = [0]

    def psacc():
        i = acc_rr[0] % 2
        acc_rr[0] += 1
        return PSB[i], PSS[i]


    P.dma(ident, ident_d, writes=[s_const])
    CP("dve", identb, ident, [s_const], [s_const])
    h.memset("pool", onesf, 1.0, [s_const])
    P.dma(sexp, a_sink.rearrange("l h -> (l h)").unsqueeze(0).broadcast_to([128, 16]).rearrange("p (l h) -> p l h", h=8), writes=[s_const])
    ACT(sexp, sexp, AF.Exp, [s_const], [s_const])

    s_w = P.slot("wcast")
    if phases is None or "W" in phases:
        for l in range(layers):
            for r in range(8):
                P.dma(wb_in[l, r * 128:(r + 1) * 128, :], w_in[l, r * 128:(r + 1) * 128, :], q="pool")
            P.dma(wb_uq[l], w_uq[l], q="pool")
            P.dma(wb_ukv[l], w_ukv[l], q="pool")
            for n in range(3):
                for r in range(4):
                    P.dma(wb_br[l, n, r * 128:(r + 1) * 128, :], w_br[l, n, r * 128:(r + 1) * 128, :], q="pool")
            for r in range(8):
                P.dma(wb_out[l, r * 128:(r + 1) * 128, :], w_out[l, r * 128:(r + 1) * 128, :], q="pool")
            for e in range(NEXP):
                for r in range(0, 8, 4):
                    P.dma(wb1[l, e, r * 128:(r + 4) * 128, :], w1[l, e, r * 128:(r + 4) * 128, :], q="pool")
                    P.dma(wb3[l, e, r * 128:(r + 4) * 128, :], w3[l, e, r * 128:(r + 4) * 128, :], q="pool")
                P.dma(wb2[l, e], w2[l, e], q="pool")

    def phase_mod(l):
        AR.reset(persist_mark)
        cv = AR.alloc([8, 2], F32)
        scv = AR.alloc([8, 2], F32)
        screp = AR.alloc([8, 2, 128], F32)
        bm = AR.alloc([48], F32)
        g1 = AR.alloc([8], F32)
        g2 = AR.alloc([8], F32)
        modv = AR.alloc([48, 2], F32)
        brow = AR.alloc([2, 1024], F32)
        gst = AR.alloc([2, 1024], F32)
        s_gst = P.slots(2, "gst")
        wm = [AR.alloc([8, 1024], F32) for _ in range(2)]
        s_l = P.slot()
        s_wm = P.slots(2, "wm")
        P.dma(cv, cvec, writes=[s_l])
        P.dma(bm, b_modT[l], writes=[s_l])
        P.dma(g1, g1T[l], writes=[s_l])
        P.dma(g2, g2T[l], writes=[s_l])
        for w_i, c0 in enumerate((2048, 5120)):
            P.dma(brow[:, w_i, :], b_mod[l:l + 1, c0:c0 + 1024].broadcast_to([128, 1024]), writes=[s_l])
        ACT(scv, cv, AF.Silu, [s_l], [s_l])
        for k in range(8):
            for j in range(2):
                TS("dve", screp[:, k, j, :], onesf, scv[:, k, j:j + 1], None, ALU.mult, None, [s_l, s_const], [s_l])
        pmod, s_pmod = psacc()
        for g in range(6):
            b = g % 2
            P.dma(wm[b], w_mod[l, :, g * 1024:(g + 1) * 1024].rearrange("(k p) n -> p k n", p=128), writes=[s_wm[b]])
            for m in range(8):
                for k in range(8):
                    MM(pmod[:, (g * 8 + m) * 2:(g * 8 + m) * 2 + 2], wm[b][:, k, m * 128:(m + 1) * 128], scv[:, k, :], k == 0, k == 7,
                       [s_wm[b], s_l], [s_pmod])
            if g in (2, 5):
                w_i = 0 if g == 2 else 1
                for j in range(2):
                    for hf in range(2):
                        pg, s_pg = ps()
                        for k in range(8):
                            MM(pg, screp[:, k, j, :], wm[b][:, k, hf * 512:(hf + 1) * 512], k == 0, k == 7, [s_wm[b], s_l], [s_pg])
                        TT("dve", gst[:, j, hf * 512:(hf + 1) * 512], pg, brow[:, w_i, hf * 512:(hf + 1) * 512], ALU.add,
                           [s_pg, s_l], [s_gst[j]])
                    P.dma(gtd[l, w_i, j:j + 1, :], gst[0:1, j, :], reads=[s_gst[j]])
        TT("dve", modv, pmod[:, 0:96].rearrange("p (m j) -> p m j", j=2), bm.unsqueeze(2).broadcast_to([128, 48, 2]), ALU.add,
           [s_pmod, s_l], [s_l])
        for w_i, (sh0, sc0, g) in enumerate(((0, 8, g1), (24, 32, g2))):
            TS("dve", modA[:, l, w_i, :, :], modv[:, sc0:sc0 + 8, :], 1.0, None, ALU.add, None, [s_l], [s_mod])
            TT("dve", modA[:, l, w_i, :, :], modA[:, l, w_i, :, :], g.unsqueeze(2).broadcast_to([128, 8, 2]), ALU.mult, [s_l, s_mod], [s_mod])
            CP("dve", modB[:, l, w_i, :, :], modv[:, sh0:sh0 + 8, :], [s_l], [s_mod])

    def norm_mod_T(xt, s_x, ntile, l, which, jfun, hTb, s_hT, tmp, s_tmp, hTf=None, tok0=0):
        ms = tmp["ms"]
        for j in range(ntile):
            ACT(tmp["junk"], xt[:, j, :], AF.Square, [s_x], [s_tmp], scale=1.0 / 32.0, accum_out=ms[:, j:j + 1])
        TS("dve", ms[:, 0:ntile], ms[:, 0:ntile], EPS, None, ALU.add, None, [s_tmp], [s_tmp])
        ACT(ms[:, 0:ntile], ms[:, 0:ntile], AF.Ln, [s_tmp], [s_tmp])
        ACT(ms[:, 0:ntile], ms[:, 0:ntile], AF.Exp, [s_tmp], [s_tmp], scale=-0.5)
        for j in range(ntile):
            jj = jfun(j)
            if hTf is None:
                xn = tmp["xnb"]
                TS("dve", xn, xt[:, j, :], ms[:, j:j + 1], None, ALU.mult, None, [s_x, s_tmp], [s_tmp])
                pt, s_pt = ps()
                ptb = pt.bitcast(BF16)
                for k in range(8):
                    TR(ptb[:, k * 128:(k + 1) * 128], xn[:, k * 128:(k + 1) * 128], identb, [s_tmp, s_const], [s_pt])
                t2 = tmp["t2"]
                TT("dve", t2, ptb.rearrange("p (k t) -> p k t", t=128),
                   modA[:, l, which, :, jj:jj + 1].broadcast_to([128, 8, 128]), ALU.mult, [s_pt, s_mod], [s_tmp])
                TT("pool", hTb[:, :, tok0 + j * 128:tok0 + (j + 1) * 128], t2,
                   modB[:, l, which, :, jj:jj + 1].broadcast_to([128, 8, 128]), ALU.add, [s_tmp, s_mod], [s_hT])
            else:
                xn = tmp["xnf"]
                TS("dve", xn, xt[:, j, :], ms[:, j:j + 1], None, ALU.mult, None, [s_x, s_tmp], [s_tmp])
                for hf in range(2):
                    pt, s_pt = ps()
                    for k in range(4):
                        kk = hf * 4 + k
                        TR(pt[:, k * 128:(k + 1) * 128], xn[:, kk * 128:(kk + 1) * 128], ident, [s_tmp, s_const], [s_pt])
                    t2 = tmp["t2f"]
                    TT("dve", t2, pt.rearrange("p (k t) -> p k t", t=128),
                       modA[:, l, which, hf * 4:hf * 4 + 4, jj:jj + 1].broadcast_to([128, 4, 128]), ALU.mult, [s_pt, s_mod], [s_tmp])
                    TT("pool", hTf[:, hf * 4:hf * 4 + 4, j * 128:(j + 1) * 128], t2,
                       modB[:, l, which, hf * 4:hf * 4 + 4, jj:jj + 1].broadcast_to([128, 4, 128]), ALU.add, [s_tmp, s_mod], [s_hT])
                CP("act", hTb[:, :, tok0 + j * 128:tok0 + (j + 1) * 128], hTf[:, :, j * 128:(j + 1) * 128], [s_hT], [s_hT])

    def blocks(include_ctx=True, bs=512):
        bl = []
        if include_ctx:
            bl.append((0, L, True))
        t = L
        while t < T:
            n = min(bs, T - t)
            bl.append((t, n, False))
            t += n
        return bl


    NTM = O_GL

    def phase_A(l, xsrc):
        AR.reset(persist_mark)
        ST = "pool"
        wsb = AR.alloc([8, NTM], BF16)
        wuq = AR.alloc([2, 768], BF16)
        wukv = AR.alloc([1024], BF16)
        gq = AR.alloc([2], F32)
        gkv = AR.alloc([1], F32)
        s_wsb = P.slot("wsb")
        for k in range(8):
            P.dma(wsb[:, k, :], wb_in[l, k * 128:(k + 1) * 128, 0:NTM], writes=[s_wsb])
        P.dma(wuq, wb_uq[l].rearrange("(k p) n -> p k n", p=128), writes=[s_wsb])
        P.dma(wukv, wb_ukv[l], writes=[s_wsb])
        P.dma(gq, gqT[l], writes=[s_wsb])
        P.dma(gkv, gkvT[l], writes=[s_wsb])

        def dbl(shape, dt):
            return [AR.alloc(shape, dt) for _ in range(2)]
        wg = dbl([8, 512], BF16)
        s_wg = P.slots(2, "wg")
        xt = dbl([1, 1024], F32)
        s_x = P.slots(2, "xt")
        hT = dbl([8, 512], BF16)
        s_hT = P.slots(2, "hT")
        rA = dbl([4, 64], F32)
        rC = dbl([4, 32], F32)
        s_r = P.slots(2, "rope")
        tmp = dict(ms=AR.alloc([8], F32), junk=AR.alloc([1024], F32), xnb=AR.alloc([1024], BF16), t2=AR.alloc([8, 128], BF16))
        tmp_q = AR.alloc([768], F32)
        s_tq = P.slot("tq")
        s_tmp = P.slot("tmpA")
        qa_tm = dbl([512], BF16); ka_tm = dbl([128], BF16)
        ropet = dbl([512], F32); ropeu = dbl([512], F32)
        va_s = dbl([128], BF16)
        ib_s = dbl([512], BF16); zf_s = dbl([512], BF16); zb_s = dbl([512], BF16); sg_s = dbl([512], BF16)
        cst = dbl([8], F32)
        cqn = dbl([256], BF16); ckvn = dbl([128], BF16); kr_tm = dbl([32], BF16)
        qc_tm = dbl([768], BF16)
        vc_s = dbl([512], BF16)
        qaT_s = AR.alloc([4, 512], BF16); kaT_s = AR.alloc([512], BF16); qbT_s = AR.alloc([4, 512], BF16)
        cqnT = AR.alloc([2, 512], BF16); ckvnT = AR.alloc([512], BF16); krT_s = AR.alloc([512], BF16)
        qcT_s = AR.alloc([8, 512], BF16); kcT_s = AR.alloc([4, 512], BF16)
        gT_s = dbl([4, 512], BF16)
        s_ev = {}

        def sl(name, par=0):
            key = (name, par)
            if key not in s_ev:
                s_ev[key] = P.slot(name + str(par))
            return s_ev[key]

        tix = 0
        for bi_, (t0, n, isctx) in enumerate(blocks()):
            bp = bi_ % 2
            nt = n // 128
            jj = 1 if isctx else 0
            rd = [s_hT[bp], s_wsb]
            for j in range(nt):
                par = tix % 2
                tix += 1
                tt0 = t0 + j * 128
                P.dma(xt[par][:, 0, :], xsrc[tt0:tt0 + 128, :], writes=[s_x[par]], q=LD)
                P.dma(rA[par], ropeA[tt0:tt0 + 128], writes=[s_r[par]], q=LD)
                P.dma(rC[par], ropeC[tt0:tt0 + 128], writes=[s_r[par]], q=LD)
                norm_mod_T(xt[par], s_x[par], 1, l, 0, lambda j_: jj, hT[bp], s_hT[bp], tmp, s_tmp, tok0=j * 128)

                def tm_group(c0, ncols):
                    pg, s_pg = ps()
                    for k in range(8):
                        MM(pg[:, 0:ncols], hT[bp][:, k, j * 128:(j + 1) * 128], wsb[:, k, c0:c0 + ncols], k == 0, k == 7, rd, [s_pg])
                    return pg, s_pg

                def rope(dst, src, s_src, nh, hd, tab, a0, dsl, eng2="pool"):
                    half = hd // 2
                    s3 = src.rearrange("p (h d) -> p h d", d=hd)
                    t3 = ropet[par][:, 0:nh * hd].rearrange("p (h d) -> p h d", d=hd)
                    u3 = ropeu[par][:, 0:nh * hd].rearrange("p (h d) -> p h d", d=hd)
                    s_t = sl("ropet", par)
                    TT("dve", t3, s3, tab[:, a0, :].unsqueeze(1).broadcast_to([128, nh, hd]), ALU.mult, [s_src, s_r[par]], [s_t])
                    TT("dve", u3[:, :, 0:half], s3[:, :, half:hd], tab[:, a0 + 1, 0:half].unsqueeze(1).broadcast_to([128, nh, half]),
                       ALU.mult, [s_src, s_r[par]], [s_t])
                    TT("dve", u3[:, :, half:hd], s3[:, :, 0:half], tab[:, a0 + 1, half:hd].unsqueeze(1).broadcast_to([128, nh, half]),
                       ALU.mult, [s_src, s_r[par]], [s_t])
                    TT(eng2, dst, ropet[par][:, 0:nh * hd], ropeu[par][:, 0:nh * hd], ALU.add, [s_t], [dsl])

                tsl = slice(tt0, tt0 + 128)
                import os as _os
                KA = int(_os.environ.get("KA", "9"))
                if KA < 2:
                    continue
                KB = int(_os.environ.get("KB", "15"))
                if KB & 1:
                    pg, s_pg = tm_group(O_AQ, 512)
                    rope(qa_tm[par], pg, s_pg, 8, 64, rA[par], 0, sl("qa_tm", par))
                if KB & 2:
                    pt, s_pt = ps()
                    ptb = pt.bitcast(BF16)
                    for c in range(4):
                        TR(ptb[:, c * 128:(c + 1) * 128], qa_tm[par][:, c * 128:(c + 1) * 128], identb, [sl("qa_tm", par), s_const], [s_pt])
                    CP("act", qaT_s[:, :, j * 128:(j + 1) * 128], ptb[:, 0:512].rearrange("p (c t) -> p c t", t=128), [s_pt], [sl("qaT_s")])
                if KB & 4:
                    pg, s_pg = tm_group(O_AK, 256)
                    rope(ka_tm[par], pg[:, 0:128], s_pg, 2, 64, rA[par], 2, sl("ka_tm", par))
                    KC = int(_os.environ.get("KC", "3"))
                    if KC >= 2:
                        CP("act", va_s[par], pg[:, 128:256], [s_pg], [sl("va_s", par)])
                    if KC >= 3:
                        P.dma(va[tsl, :], va_s[par], reads=[sl("va_s", par)], q=ST)
                if KB & 8:
                    pt, s_pt = ps()
                    ptb = pt.bitcast(BF16)
                    TR(ptb[:, 0:128], ka_tm[par], identb, [sl("ka_tm", par), s_const], [s_pt])
                    CP("act", kaT_s[:, j * 128:(j + 1) * 128], ptb[:, 0:128], [s_pt], [sl("kaT_s")])
                if KA < 3:
                    continue
                for c0, dst, nm, dd in ((O_BI, ib_s, "ib_s", ib), (O_BZF, zf_s, "zf_s", zf), (O_BZB, zb_s, "zb_s", zb)):
                    pg, s_pg = tm_group(c0, 512)
                    CP("dve" if nm != "zb_s" else "act", dst[par], pg, [s_pg], [sl(nm, par)])
                    P.dma(dd[tsl, :], dst[par], reads=[sl(nm, par)], q=ST)
                pg, s_pg = tm_group(O_BG, 512)
                ACT(sg_s[par], pg, AF.Silu, [s_pg], [sl("sg_s", par)])
                P.dma(sgb[tsl, :], sg_s[par], reads=[sl("sg_s", par)], q=ST)
                if KA < 4:
                    continue
                pg, s_pg = tm_group(O_CQ, 416)
                cs = cst[par]
                s_cs = sl("cst", par)
                ACT(tmp["junk"][:, 0:256], pg[:, 0:256], AF.Square, [s_pg], [s_tmp, s_cs], scale=1.0 / 16.0, accum_out=cs[:, 0:1])
                ACT(tmp["junk"][:, 0:128], pg[:, 256:384], AF.Square, [s_pg], [s_tmp, s_cs], scale=float(128 ** -0.5), accum_out=cs[:, 1:2])
                TS("dve", cs[:, 0:2], cs[:, 0:2], EPS, None, ALU.add, None, [s_cs], [s_cs])
                ACT(cs[:, 0:2], cs[:, 0:2], AF.Ln, [s_cs], [s_cs])
                ACT(cs[:, 0:2], cs[:, 0:2], AF.Exp, [s_cs], [s_cs], scale=-0.5)
                TS("dve", cqn[par], pg[:, 0:256], cs[:, 0:1], None, ALU.mult, None, [s_pg, s_cs], [sl("cqn", par)])
                TS("dve", ckvn[par], pg[:, 256:384], cs[:, 1:2], None, ALU.mult, None, [s_pg, s_cs], [sl("ckvn", par)])
                rope(kr_tm[par], pg[:, 384:416], s_pg, 1, 32, rC[par], 2, sl("kr_tm", par))
                pt, s_pt = ps()
                ptb = pt.bitcast(BF16)
                TR(ptb[:, 0:128], cqn[par][:, 0:128], identb, [sl("cqn", par), s_const], [s_pt])
                TR(ptb[:, 128:256], cqn[par][:, 128:256], identb, [sl("cqn", par), s_const], [s_pt])
                TR(ptb[:, 256:384], ckvn[par], identb, [sl("ckvn", par), s_const], [s_pt])
                TR(ptb[0:32, 384:512], kr_tm[par], identb, [sl("kr_tm", par), s_const], [s_pt])
                for c in range(2):
                    ACT(cqnT[:, c, j * 128:(j + 1) * 128], ptb[:, c * 128:(c + 1) * 128], AF.Identity, [s_pt, s_wsb], [sl("cqnT")],
                        scale=gq[:, c:c + 1])
                ACT(ckvnT[:, j * 128:(j + 1) * 128], ptb[:, 256:384], AF.Identity, [s_pt, s_wsb], [sl("ckvnT")], scale=gkv[:, 0:1])
                CP("dve", krT_s[0:32, j * 128:(j + 1) * 128], ptb[0:32, 384:512], [s_pt], [sl("krT_s")])
                if KA < 5:
                    continue
                pq0, s_pq0 = ps()
                pq1, s_pq1 = ps()
                for c in range(2):
                    MM(pq0, cqnT[:, c, j * 128:(j + 1) * 128], wuq[:, c, 0:512], c == 0, c == 1, [sl("cqnT"), s_wsb], [s_pq0])
                for c in range(2):
                    MM(pq1[:, 0:256], cqnT[:, c, j * 128:(j + 1) * 128], wuq[:, c, 512:768], c == 0, c == 1, [sl("cqnT"), s_wsb], [s_pq1])
                qscale = float(96 ** -0.5)
                q3 = qc_tm[par].rearrange("p (h d) -> p h d", d=96)
                s_qc = sl("qc_tm", par)
                CP("act", tmp_q[:, 0:512], pq0, [s_pq0], [s_tq])
                CP("act", tmp_q[:, 512:768], pq1[:, 0:256], [s_pq1], [s_tq])
                tq3 = tmp_q.rearrange("p (h d) -> p h d", d=96)
                TS("dve", q3[:, :, 0:64], tq3[:, :, 0:64], qscale, None, ALU.mult, None, [s_tq], [s_qc])
                t3 = ropet[par][:, 0:256].rearrange("p (h d) -> p h d", d=32)
                u3 = ropeu[par][:, 0:256].rearrange("p (h d) -> p h d", d=32)
                s_t = sl("ropet", par)
                TT("dve", t3, tq3[:, :, 64:96], rC[par][:, 0, :].unsqueeze(1).broadcast_to([128, 8, 32]), ALU.mult, [s_tq, s_r[par]], [s_t])
                TT("dve", u3[:, :, 0:16], tq3[:, :, 80:96], rC[par][:, 1, 0:16].unsqueeze(1).broadcast_to([128, 8, 16]), ALU.mult,
                   [s_tq, s_r[par]], [s_t])
                TT("dve", u3[:, :, 16:32], tq3[:, :, 64:80], rC[par][:, 1, 16:32].unsqueeze(1).broadcast_to([128, 8, 16]), ALU.mult,
                   [s_tq, s_r[par]], [s_t])
                TT("pool", q3[:, :, 64:96], t3, u3, ALU.add, [s_t], [s_qc])
                pt, s_pt = ps()
                ptb = pt.bitcast(BF16)
                for hh in range(8):
                    TR(ptb[0:96, hh * 128:(hh + 1) * 128], qc_tm[par][:, hh * 96:(hh + 1) * 96], identb, [s_qc, s_const], [s_pt])
                CP("act", qcT_s[0:96, :, j * 128:(j + 1) * 128], ptb[0:96, :].rearrange("p (h t) -> p h t", t=128), [s_pt], [sl("qcT_s")])
                pv, s_pv = ps()
                MM(pv, ckvnT[:, j * 128:(j + 1) * 128], wukv[:, 512:1024], True, True, [sl("ckvnT"), s_wsb], [s_pv])
                CP("dve", vc_s[par], pv, [s_pv], [sl("vc_s", par)])
                P.dma(vc[tsl, :], vc_s[par], reads=[sl("vc_s", par)], q=ST)
            bsl = slice(t0, t0 + n)
            if KA < 6:
                continue
            for c in range(4):
                pg, s_pg = ps()
                for k in range(8):
                    MM(pg[:, 0:n], wsb[:, k, O_BQ + c * 128:O_BQ + (c + 1) * 128], hT[bp][:, k, 0:n], k == 0, k == 7, rd, [s_pg])
                CP("act" if c % 2 else "dve", qbT_s[:, c, 0:n], pg[:, 0:n], [s_pg], [sl("qbT_s")])
            for c in range(4):
                pg, s_pg = ps()
                MM(pg[:, 0:n], wukv[:, c * 128:(c + 1) * 128], ckvnT[:, 0:n], True, True, [sl("ckvnT"), s_wsb], [s_pg])
                CP("dve", kcT_s[:, c, 0:n], pg[:, 0:n], [s_pg], [sl("kcT_s")])
            P.dma(qaT[:, bsl].rearrange("(c p) t -> p c t", p=128), qaT_s[:, :, 0:n], reads=[sl("qaT_s")], q=ST)
            P.dma(kaT[:, bsl], kaT_s[:, 0:n], reads=[sl("kaT_s")], q=ST)
            P.dma(qbT[:, bsl].rearrange("(c p) t -> p c t", p=128), qbT_s[:, :, 0:n], reads=[sl("qbT_s")], q=ST)
            P.dma(qcT[:, :, bsl].rearrange("h d t -> d h t"), qcT_s[0:96, :, 0:n], reads=[sl("qcT_s")], q=ST)
            P.dma(kcT[:, bsl].rearrange("(c p) t -> p c t", p=128), kcT_s[:, :, 0:n], reads=[sl("kcT_s")], q=ST)
            P.dma(krT[:, bsl], krT_s[0:32, 0:n], reads=[sl("krT_s")], q=ST)
            for g in range(6):
                gp = g % 2
                P.dma(wg[gp], wb_in[l, :, O_GL + g * 512:O_GL + (g + 1) * 512].rearrange("(k p) n -> p k n", p=128), writes=[s_wg[gp]], q=LD)
                for c in range(4):
                    pg, s_pg = ps()
                    for k in range(8):
                        MM(pg[:, 0:n], wg[gp][:, k, c * 128:(c + 1) * 128], hT[bp][:, k, 0:n], k == 0, k == 7, [s_hT[bp], s_wg[gp]], [s_pg])
                    ACT(gT_s[gp][:, c, 0:n], pg[:, 0:n], AF.Sigmoid, [s_pg], [sl("gT_s", gp)])
                P.dma(gT[g * 512:(g + 1) * 512, bsl].rearrange("(c p) t -> p c t", p=128), gT_s[gp][:, :, 0:n], reads=[sl("gT_s", gp)], q=ST)

    def att_finalize(po, s_po, n, sink_ap, dst_dram, tmpo, rdt, s_fin, eng_dma=ST, nh=1):
        if sink_ap is not None:
            TT("dve", rdt[64:65, 0:n].rearrange("p (g t) -> p g t", g=nh), po[64:65, 0:n].rearrange("p (g t) -> p g t", g=nh),
               sink_ap, ALU.add, [s_po, s_const], [s_fin])
            P.dve(lambda e: e.reciprocal(out=rdt[64:65, 0:n], in_=rdt[64:65, 0:n]), [s_fin], [s_fin])
        else:
            P.dve(lambda e: e.reciprocal(out=rdt[64:65, 0:n], in_=po[64:65, 0:n]), [s_po], [s_fin])
        pb, s_pb = ps()
        MM(pb[0:64, 0:n], onesf[64:65, 0:64], rdt[64:65, 0:n], True, True, [s_fin, s_const], [s_pb])
        CP("act", tmpo[0:64, 0:n], po[0:64, 0:n], [s_po], [s_fin])
        o16 = tmpo[0:64, 512:1024].bitcast(BF16)[:, 0:n]
        TT("dve", o16, tmpo[0:64, 0:n], pb[0:64, 0:n], ALU.mult, [s_fin, s_pb], [s_fin])
        P.dma(dst_dram, o16 if nh == 1 else o16.rearrange("p (g t) -> p g t", g=nh), reads=[s_fin], q=eng_dma)

    def phase_attA(l, with_ctx):
        AR.reset(persist_mark)
        kT = AR.alloc([2, T], BF16)
        vt = AR.alloc([NT, 2, 65], BF16)
        msk = AR.alloc([2, 128], BF16)
        mskf = AR.alloc([2, 128], F32)
        s_kv = P.slot("attA_kv")
        for kvh in range(2):
            P.dma(kT[0:64, kvh, :], kaT[kvh * 64:(kvh + 1) * 64, :], writes=[s_kv])
        h.memset("pool", vt, 1.0, [s_kv])
        for kvh in range(2):
            P.dma(vt[:, :, kvh, 0:64], va[:, kvh * 64:(kvh + 1) * 64].rearrange("(j p) d -> p j d", p=128), writes=[s_kv])
        P.dma(mskf, amask, writes=[s_kv])
        CP("dve", msk, mskf, [s_kv], [s_kv])
        qt = [AR.alloc([8, 128], BF16) for _ in range(2)]
        s_q = P.slots(2, "attA_q")
        pT = [AR.alloc([512], BF16) for _ in range(4)]
        s_pT = P.slots(4, "attA_pT")
        pcnt_ = [0]
        tmpo = [AR.alloc([1024], F32) for _ in range(2)]
        rdt = [AR.alloc([512], F32) for _ in range(2)]
        s_fin = P.slots(2, "attA_fin")
        nlat = S // 128
        qtiles = ([0, 1] if with_ctx else []) + list(range(2, NT))
        cnt = 0
        pcnt = 0
        for qi_, gi in enumerate(qtiles):
            qp = qi_ % 2
            P.dma(qt[qp][0:64], qaT[:, gi * 128:(gi + 1) * 128].rearrange("(h d) t -> d h t", d=64), writes=[s_q[qp]], q=LD)
            if gi < 2:
                keys = [(0, None), (1, None)]
            else:
                nq = gi - 2
                keys = [(0, None), (1, None)]
                if nq >= 1:
                    keys.append((gi - 1, 0))
                keys.append((gi, None))
                if nq + 1 < nlat:
                    keys.append((gi + 1, 1))
            for kvh in range(2):
                po, s_po = psacc()
                LA = 2
                ppl = {}

                def qk(ki, kvh=kvh, qp=qp, keys=keys, ppl=ppl):
                    kt, mk = keys[ki]
                    pss, s_pss = ps()
                    MM(pss, kT[0:64, kvh, kt * 128:(kt + 1) * 128], qt[qp][0:64, kvh * 4:(kvh + 1) * 4, :], True, True,
                       [s_kv, s_q[qp]], [s_pss])
                    pp = pcnt_[0] % 4
                    pcnt_[0] += 1
                    ACT(pT[pp], pss, AF.Exp, [s_pss], [s_pT[pp]])
                    if mk is not None:
                        p3 = pT[pp].rearrange("p (g t) -> p g t", g=4)
                        TT("pool", p3, p3, msk[:, mk, :].unsqueeze(1).broadcast_to([128, 4, 128]), ALU.mult, [s_pT[pp], s_kv], [s_pT[pp]])
                    ppl[ki] = pp
                for ki in range(min(LA, len(keys))):
                    qk(ki)
                for ki, (kt, mk) in enumerate(keys):
                    if ki + LA < len(keys):
                        qk(ki + LA)
                    pp = ppl.pop(ki)
                    MM(po[0:65, :], vt[:, kt, kvh, :], pT[pp], ki == 0, ki == len(keys) - 1, [s_kv, s_pT[pp]], [s_po])
                fp = cnt % 2
                cnt += 1
                att_finalize(po, s_po, 512, sexp[64:65, l, kvh * 4:(kvh + 1) * 4].unsqueeze(2).broadcast_to([1, 4, 128]),
                             oT[0, kvh * 256:(kvh + 1) * 256, gi * 128:(gi + 1) * 128].rearrange("(g d) t -> d g t", d=64),
                             tmpo[fp], rdt[fp], s_fin[fp], nh=4)

    def phase_attC(l, with_ctx):
        AR.reset(persist_mark)
        kT = [AR.alloc([T], BF16) for _ in range(2)]
        vt = [AR.alloc([NT, 65], BF16) for _ in range(2)]
        s_kv = P.slots(2, "attC_kv")
        qt = [AR.alloc([512], BF16) for _ in range(2)]
        s_q = P.slots(2, "attC_q")
        pT = [AR.alloc([512], BF16) for _ in range(5)]
        s_pT = P.slots(5, "attC_pT")
        pcnt_ = [0]
        tmpo = [AR.alloc([1024], F32) for _ in range(2)]
        rdt = [AR.alloc([512], F32) for _ in range(2)]
        s_fin = P.slots(2, "attC_fin")
        qblocks = blocks(include_ctx=with_ctx)
        cnt = 0
        pcnt = 0
        for hh in range(8):
            hp = hh % 2
            P.dma(kT[hp][0:64, :], kcT[hh * 64:(hh + 1) * 64, :], writes=[s_kv[hp]], q=LD)
            P.dma(kT[hp][64:96, :], krT, writes=[s_kv[hp]], q=LD)
            h.memset("pool", vt[hp], 1.0, [s_kv[hp]])
            P.dma(vt[hp][:, :, 0:64], vc[:, hh * 64:(hh + 1) * 64].rearrange("(j p) d -> p j d", p=128), writes=[s_kv[hp]], q=LD)
            for (t0, n, isctx) in qblocks:
                qp = cnt % 2
                cnt += 1
                P.dma(qt[qp][0:96, 0:n], qcT[hh, :, t0:t0 + n], writes=[s_q[qp]], q=LD)
                keys = [0, 1] if isctx else list(range(NT))
                po, s_po = psacc()
                LA = 3
                ppl = {}

                def qk(ki, n=n, hp=hp, qp=qp, keys=keys, ppl=ppl):
                    kt = keys[ki]
                    pss, s_pss = ps()
                    MM(pss[:, 0:n], kT[hp][0:96, kt * 128:(kt + 1) * 128], qt[qp][0:96, 0:n], True, True, [s_kv[hp], s_q[qp]], [s_pss])
                    pp = pcnt_[0] % 5
                    pcnt_[0] += 1
                    ACT(pT[pp][:, 0:n], pss[:, 0:n], AF.Exp, [s_pss], [s_pT[pp]])
                    ppl[ki] = pp
                for ki in range(min(LA, len(keys))):
                    qk(ki)
                for ki, kt in enumerate(keys):
                    if ki + LA < len(keys):
                        qk(ki + LA)
                    pp = ppl.pop(ki)
                    MM(po[0:65, 0:n], vt[hp][:, kt, :], pT[pp][:, 0:n], ki == 0, ki == len(keys) - 1, [s_kv[hp], s_pT[pp]], [s_po])
                att_finalize(po, s_po, n, None, oT[2, hh * 64:(hh + 1) * 64, t0:t0 + n], tmpo[qp], rdt[qp], s_fin[qp])

    def phase_B(l):
        AR.reset(persist_mark)
        ST = "pool"
        C = 32
        R2 = 2 * C
        NCH = T // C
        GS = 4
        bm = AR.alloc([4, R2], F32)
        bmk = AR.alloc([8, R2], F32)
        rmk = AR.alloc([2], F32)
        lbt = AR.alloc([512], F32)
        c1t = AR.alloc([512], F32)
        l0 = AR.alloc([512], F32)
        s_c = P.slot("B_const")
        P.dma(bm[0:R2], bmats, writes=[s_c])
        P.dma(bmk[0:R2], bmask, writes=[s_c])
        P.dma(rmk[0:R2], brm, writes=[s_c])
        if l == 0:
            h.memset("pool", lbt, 0.0, [s_c])
            h.memset("pool", c1t, 1.0, [s_c])
        else:
            for d_ in range(2):
                P.dma(l0[d_ * C:(d_ + 1) * C, :], lb_logits[0, d_:d_ + 1, :].broadcast_to([C, 512]), writes=[s_c])
                P.dma(lbt[d_ * C:(d_ + 1) * C, :], lb_logits[1, d_:d_ + 1, :].broadcast_to([C, 512]), writes=[s_c])
            TT("dve", lbt[0:R2], lbt[0:R2], l0[0:R2], ALU.subtract, [s_c], [s_c])
            ACT(lbt[0:R2], lbt[0:R2], AF.Sigmoid, [s_c], [s_c])
            TS("dve", c1t[0:R2], lbt[0:R2], -1.0, 1.0, ALU.mult, ALU.add, [s_c], [s_c])
        Sst = AR.alloc([2, 8, 64], F32)
        Sb = AR.alloc([2, 8, 64], BF16)
        s_S = P.slot("B_S")
        s_Sb = P.slot("B_Sb")
        h.memset("pool", Sst, 0.0, [s_S])
        h.memset("pool", Sb, 0.0, [s_Sb])

        def dbl(shape, dt, n=2):
            return [AR.alloc(shape, dt) for _ in range(n)]
        z2 = dbl([GS, 512], BF16); v2 = dbl([GS, 512], BF16); q2 = dbl([8, GS, R2], BF16)
        s_z = P.slots(2, "B_z"); s_v = P.slots(2, "B_v"); s_q2 = P.slots(2, "B_q")
        sig = AR.alloc([GS, 512], F32); logf = dbl([GS, 512], F32); kk = dbl([GS, 512], F32)
        s_sig = P.slot("B_sig"); s_logf = P.slots(2, "B_logf"); s_kk = P.slots(2, "B_kk")
        ek = dbl([512], F32); e2 = dbl([512], F32); ktl = dbl([512], BF16); kh = dbl([2, 512], BF16)
        s_ek = P.slots(2, "B_ek"); s_e2 = P.slots(2, "B_e2"); s_kt = P.slots(2, "B_kt"); s_kh = P.slots(2, "B_kh")
        ktT = dbl([8, R2], BF16); s_ktT = P.slots(2, "B_ktT")
        eq = dbl([8, R2], F32); eqm = dbl([8, R2], F32); s_eq = P.slots(2, "B_eq"); s_eqm = P.slots(2, "B_eqm")
        qebf = dbl([8, R2], BF16); qebb = dbl([8, R2], BF16); qtl = dbl([8, R2], BF16)
        s_qe = P.slots(2, "B_qe"); s_qt = P.slots(2, "B_qt")
        attT = dbl([8, R2], BF16); s_att = P.slots(2, "B_att")
        o_s = dbl([GS, 512], BF16); s_os = P.slots(2, "B_os")
        for b_ in range(2):
            h.memset("pool", qebf[b_], 0.0, [s_qe[b_]])
            h.memset("pool", qebb[b_], 0.0, [s_qe[b_]])
        step = 0
        nctx = L // C
        for g in range(NCH // GS):
            gp = g % 2
            cf0 = g * GS
            gctx = nctx // GS
            cb0 = (nctx - GS * (g + 1)) if g < gctx else NCH - GS * (g - gctx + 1)
            fsl = slice(cf0 * C, (cf0 + GS) * C)
            P.dma(z2[gp][0:C], zf[fsl, :].rearrange("(s p) f -> p s f", p=C), writes=[s_z[gp]], q=LD)
            P.dma(v2[gp][0:C], ib[fsl, :].rearrange("(s p) f -> p s f", p=C), writes=[s_v[gp]], q=LD)
            for s_ in range(GS):
                cb = cb0 + GS - 1 - s_
                cf = cf0 + s_
                P.dma(z2[gp][C:R2, s_, :], zb[cb * C:(cb + 1) * C, :], writes=[s_z[gp]], q=LD)
                P.dma(v2[gp][C:R2, s_, :], ib[cb * C:(cb + 1) * C, :], writes=[s_v[gp]], q=LD)
                P.dma(q2[gp][0:64, :, s_, 0:C], qbT[:, cf * C:(cf + 1) * C].rearrange("(h d) t -> d h t", d=64), writes=[s_q2[gp]], q=LD)
                P.dma(q2[gp][0:64, :, s_, C:R2], qbT[:, cb * C:(cb + 1) * C].rearrange("(h d) t -> d h t", d=64), writes=[s_q2[gp]], q=LD)
            z2f = z2[gp][0:R2].rearrange("p s f -> p (s f)")
            sigf = sig[0:R2].rearrange("p s f -> p (s f)")
            ACT(sigf, z2f, AF.Sigmoid, [s_z[gp]], [s_sig])
            TT("dve", sig[0:R2], sig[0:R2], c1t[0:R2].unsqueeze(1).broadcast_to([R2, GS, 512]), ALU.mult, [s_sig, s_c], [s_sig])
            TT("dve", sig[0:R2], sig[0:R2], lbt[0:R2].unsqueeze(1).broadcast_to([R2, GS, 512]), ALU.add, [s_sig, s_c], [s_sig])
            ACT(logf[gp][0:R2].rearrange("p s f -> p (s f)"), sigf, AF.Ln, [s_sig], [s_logf[gp]])
            TS("pool", kk[gp][0:R2].rearrange("p s f -> p (s f)"), sigf, -1.0, 1.0, ALU.mult, ALU.add, [s_sig], [s_kk[gp]])
            for s_ in range(GS):
                sp = step % 2
                step += 1
                lf = logf[gp][0:R2, s_, :]
                pe1, s_pe1 = ps()
                MM(pe1[0:R2], bm[0:R2, 0, :], lf, True, True, [s_c, s_logf[gp]], [s_pe1])
                ACT(ek[sp][0:R2], pe1[0:R2], AF.Exp, [s_pe1], [s_ek[sp]])
                TT("dve", ktl[sp][0:R2], kk[gp][0:R2, s_, :], ek[sp][0:R2], ALU.mult, [s_kk[gp], s_ek[sp]], [s_kt[sp]])
                pe2, s_pe2 = ps()
                MM(pe2[0:R2], bm[0:R2, 1, :], lf, True, True, [s_c, s_logf[gp]], [s_pe2])
                ACT(e2[sp][0:R2], pe2[0:R2], AF.Exp, [s_pe2], [s_e2[sp]])
                TT("dve", e2[sp][0:R2], kk[gp][0:R2, s_, :], e2[sp][0:R2], ALU.mult, [s_kk[gp], s_e2[sp]], [s_e2[sp]])
                for d_ in range(2):
                    ACT(kh[sp][0:R2, d_, :], e2[sp][0:R2], AF.Identity, [s_e2[sp], s_c], [s_kh[sp]], scale=rmk[0:R2, d_:d_ + 1])
                pt, s_pt = ps()
                ptb = pt.bitcast(BF16)
                for hh in range(8):
                    TR(ptb[0:64, hh * R2:(hh + 1) * R2], ktl[sp][0:R2, hh * 64:(hh + 1) * 64], identb[0:R2, 0:R2], [s_kt[sp], s_const], [s_pt])
                CP("act", ktT[sp][0:64], ptb[0:64, 0:8 * R2].rearrange("p (c t) -> p c t", t=R2), [s_pt], [s_ktT[sp]])
                pbT, s_pbT = ps()
                for hh in range(8):
                    MM(pbT[0:64, hh * R2:(hh + 1) * R2], lf[:, hh * 64:(hh + 1) * 64], bm[0:R2, 2, :], True, True, [s_c, s_logf[gp]], [s_pbT])
                ACT(eq[sp][0:64], pbT[0:64, 0:8 * R2].rearrange("p (c t) -> p c t", t=R2), AF.Exp, [s_pbT], [s_eq[sp]])
                pbm, s_pbm = ps()
                for hh in range(8):
                    MM(pbm[0:64, hh * R2:(hh + 1) * R2], lf[:, hh * 64:(hh + 1) * 64], bm[0:R2, 3, :], True, True, [s_c, s_logf[gp]], [s_pbm])
                ACT(eqm[sp][0:64], pbm[0:64, 0:8 * R2].rearrange("p (c t) -> p c t", t=R2), AF.Exp, [s_pbm], [s_eqm[sp]])
                qs = q2[gp][0:64, :, s_, :]
                TT("dve", qebf[sp][0:64, :, 0:C], qs[:, :, 0:C], eq[sp][0:64, :, 0:C], ALU.mult, [s_q2[gp], s_eq[sp]], [s_qe[sp]])
                TT("dve", qebb[sp][0:64, :, C:R2], qs[:, :, C:R2], eq[sp][0:64, :, C:R2], ALU.mult, [s_q2[gp], s_eq[sp]], [s_qe[sp]])
                TT("pool", qtl[sp][0:64], qs, eqm[sp][0:64], ALU.mult, [s_q2[gp], s_eqm[sp]], [s_qt[sp]])
                pa, s_pa = ps()
                for hh in range(8):
                    MM(pa[0:R2, hh * R2:(hh + 1) * R2], ktT[sp][0:64, hh, :], qtl[sp][0:64, hh, :], True, True, [s_ktT[sp], s_qt[sp]], [s_pa])
                TT("dve", attT[sp][0:R2], pa[0:R2, 0:8 * R2].rearrange("p (h t) -> p h t", t=R2), bmk[0:R2], ALU.mult,
                   [s_pa, s_c], [s_att[sp]])
                po, s_po = psacc()
                for hh in range(8):
                    osl = po[0:R2, hh * 64:(hh + 1) * 64]
                    MM(osl, attT[sp][0:R2, hh, :], v2[gp][0:R2, s_, hh * 64:(hh + 1) * 64], True, False, [s_att[sp], s_v[gp]], [s_po])
                    MM(osl, qebf[sp][0:64, hh, :], Sb[0:64, 0, hh, :], False, False, [s_qe[sp], s_Sb], [s_po])
                    MM(osl, qebb[sp][0:64, hh, :], Sb[0:64, 1, hh, :], False, True, [s_qe[sp], s_Sb], [s_po])
                CP("act", o_s[gp][0:R2, s_, :], po[0:R2], [s_po], [s_os[gp]])
                for d_ in range(2):
                    pS, s_pS = ps()
                    for hh in range(8):
                        MM(pS[0:64, hh * 64:(hh + 1) * 64], kh[sp][0:R2, d_, hh * 64:(hh + 1) * 64], v2[gp][0:R2, s_, hh * 64:(hh + 1) * 64], True, True,
                           [s_kh[sp], s_v[gp]], [s_pS])
                    Sv = Sst[0:64, d_]
                    TT("dve", Sv, Sv, eq[sp][0:64, :, C - 1 + d_:C + d_].broadcast_to([64, 8, 64]), ALU.mult, [s_S, s_eq[sp]], [s_S])
                    TT("dve", Sv, Sv, pS[0:64].rearrange("p (h x) -> p h x", x=64), ALU.add, [s_S, s_pS], [s_S])
                CP("act", Sb[0:64], Sst[0:64], [s_S], [s_Sb])
            P.dma(ofb[0, fsl, :].rearrange("(s p) f -> p s f", p=C), o_s[gp][0:C], reads=[s_os[gp]], q=ST)
            for s_ in range(GS):
                cb = cb0 + GS - 1 - s_
                P.dma(ofb[1, cb * C:(cb + 1) * C, :], o_s[gp][C:R2, s_, :], reads=[s_os[gp]], q=ST)
        P.barrier()
        gon = AR.alloc([512], F32)
        s_g = P.slot("B_gon")
        P.dma(gon, b_onorm[l:l + 1, :].broadcast_to([128, 512]), writes=[s_g])
        of_ = dbl([512], BF16); ob_ = dbl([512], BF16); sg_ = dbl([512], BF16)
        s_in = P.slots(2, "Bf_in")
        osum = dbl([512], F32); osq = dbl([512], F32); st = dbl([8], F32); on_ = dbl([512], BF16); oTs = dbl([4, 128], BF16)
        s_w2 = P.slots(2, "Bf_w"); s_oTs = P.slots(2, "Bf_oT")
        for j in range(NT):
            p_ = j % 2
            tsl = slice(j * 128, (j + 1) * 128)
            P.dma(of_[p_], ofb[0, tsl, :], writes=[s_in[p_]], q=LD)
            P.dma(ob_[p_], ofb[1, tsl, :], writes=[s_in[p_]], q=LD)
            P.dma(sg_[p_], sgb[tsl, :], writes=[s_in[p_]], q=LD)
            TT("dve", osum[p_], of_[p_], ob_[p_], ALU.add, [s_in[p_]], [s_w2[p_]])
            ACT(osq[p_], osum[p_], AF.Square, [s_w2[p_]], [s_w2[p_]], scale=0.125)
            h.reduce(st[p_], osq[p_].rearrange("p (h d) -> p h d", d=64), ALU.add, [s_w2[p_]], [s_w2[p_]])
            TS("dve", st[p_], st[p_], EPS, None, ALU.add, None, [s_w2[p_]], [s_w2[p_]])
            ACT(st[p_], st[p_], AF.Ln, [s_w2[p_]], [s_w2[p_]])
            ACT(st[p_], st[p_], AF.Exp, [s_w2[p_]], [s_w2[p_]], scale=-0.5)
            o3 = osum[p_].rearrange("p (h d) -> p h d", d=64)
            TT("dve", o3, o3, st[p_].unsqueeze(2).broadcast_to([128, 8, 64]), ALU.mult, [s_w2[p_]], [s_w2[p_]])
            TT("pool", osum[p_], osum[p_], gon, ALU.mult, [s_w2[p_], s_g], [s_w2[p_]])
            TT("pool", on_[p_], osum[p_], sg_[p_], ALU.mult, [s_w2[p_], s_in[p_]], [s_w2[p_]])
            pt, s_pt = ps()
            ptb = pt.bitcast(BF16)
            for c in range(4):
                TR(ptb[:, c * 128:(c + 1) * 128], on_[p_][:, c * 128:(c + 1) * 128], identb, [s_w2[p_], s_const], [s_pt])
            CP("act", oTs[p_], ptb[:, 0:512].rearrange("p (c t) -> p c t", t=128), [s_pt], [s_oTs[p_]])
            P.dma(oT[1, :, tsl].rearrange("(c p) t -> p c t", p=128), oTs[p_], reads=[s_oTs[p_]], q=ST)

    def phase_merge(l, xsrc, with_ctx):
        AR.reset(persist_mark)
        ST = "pool"
        wbr = AR.alloc([3, 4, 1024], BF16)
        wout = AR.alloc([8, 1024], BF16)
        wr = AR.alloc([8, 36], F32)
        gt1 = AR.alloc([2, 1024], F32)
        s_wm = P.slot("M_w")
        for n_ in range(3):
            P.dma(wbr[:, n_], wb_br[l, n_].rearrange("(k p) n -> p k n", p=128), writes=[s_wm])
        P.dma(wout, wb_out[l].rearrange("(k p) n -> p k n", p=128), writes=[s_wm])
        P.dma(wr, w_r[l].rearrange("(k p) n -> p k n", p=128), writes=[s_wm])
        for j_ in range(2):
            P.dma(gt1[:, j_, :], gtd[l, 0, j_:j_ + 1, :].broadcast_to([128, 1024]), writes=[s_wm])
        oT3 = [AR.alloc([3, 4, 512], BF16) for _ in range(2)]
        gT3 = [AR.alloc([24, 512], BF16) for _ in range(2)]
        s_in = P.slots(2, "M_in")
        yT = AR.alloc([8, 512], BF16)
        s_yT = P.slot("M_yT")
        acc = [AR.alloc([512], F32) for _ in range(2)]
        tm1 = [AR.alloc([512], F32) for _ in range(2)]
        s_acc = P.slots(2, "M_acc")
        xt = [AR.alloc([1, 1024], F32) for _ in range(2)]
        xnew = [AR.alloc([1, 1024], F32) for _ in range(2)]
        s_x = P.slots(2, "M_x")
        s_xn = P.slots(2, "M_xn")
        h2f = AR.alloc([8, 128], F32)
        h2b = [AR.alloc([8, 512], BF16) for _ in range(2)]
        _sh2 = P.slot("M_h2")
        s_h2 = [_sh2, _sh2]
        s_h2f = P.slot("M_h2f")
        tmp = dict(ms=AR.alloc([8], F32), junk=AR.alloc([1024], F32), xnf=AR.alloc([1024], F32), t2f=AR.alloc([4, 128], F32))
        s_tmp = P.slot("M_tmp")
        R = {k: AR.alloc([n_], F32) for k, n_ in dict(lg=36, oh=4, ge=4, esel=8, es2=8, eq1=8, eq2=8, sc=8, csel=8).items()}
        cmb = [AR.alloc([32], F32) for _ in range(2)]
        s_R = P.slot("M_R")
        s_cmb = P.slots(2, "M_cmb")
        tix = 0
        for bi_, (t0, n, isctx) in enumerate(blocks(include_ctx=with_ctx)):
            bp = bi_ % 2
            nt = n // 128
            jj = 1 if isctx else 0
            bsl = slice(t0, t0 + n)
            for br in range(3):
                P.dma(oT3[bp][:, br, :, 0:n], oT[br, :, bsl].rearrange("(k p) t -> p k t", p=128), writes=[s_in[bp]], q=LD)
            P.dma(gT3[bp][:, :, 0:n], gT[:, bsl].rearrange("(c p) t -> p c t", p=128), writes=[s_in[bp]], q=LD)
            for m in range(8):
                pbs = []
                for br in range(3):
                    pb, s_pb = ps()
                    for k in range(4):
                        MM(pb[:, 0:n], wbr[:, br, k, m * 128:(m + 1) * 128], oT3[bp][:, br, k, 0:n], k == 0, k == 3, [s_wm, s_in[bp]], [s_pb])
                    pbs.append((pb, s_pb))
                ap_ = m % 2
                TT("dve", acc[ap_][:, 0:n], pbs[0][0][:, 0:n], gT3[bp][:, m, 0:n], ALU.mult, [pbs[0][1], s_in[bp]], [s_acc[ap_]])
                TT("dve", tm1[ap_][:, 0:n], pbs[1][0][:, 0:n], gT3[bp][:, 8 + m, 0:n], ALU.mult, [pbs[1][1], s_in[bp]], [s_acc[ap_]])
                TT("pool", acc[ap_][:, 0:n], acc[ap_][:, 0:n], tm1[ap_][:, 0:n], ALU.add, [s_acc[ap_]], [s_acc[ap_]])
                TT("dve", tm1[ap_][:, 0:n], pbs[2][0][:, 0:n], gT3[bp][:, 16 + m, 0:n], ALU.mult, [pbs[2][1], s_in[bp], s_acc[ap_]], [s_acc[ap_]])
                TT("pool", yT[:, m, 0:n], acc[ap_][:, 0:n], tm1[ap_][:, 0:n], ALU.add, [s_acc[ap_]], [s_yT])
            for j in range(nt):
                par = tix % 2
                tix += 1
                tsl = slice(t0 + j * 128, t0 + (j + 1) * 128)
                P.dma(xt[par][:, 0, :], xsrc[tsl, :], writes=[s_x[par]], q=LD)
                for hf in range(2):
                    pz, s_pz = ps()
                    for k in range(8):
                        MM(pz, yT[:, k, j * 128:(j + 1) * 128], wout[:, k, hf * 512:(hf + 1) * 512], k == 0, k == 7, [s_yT, s_wm], [s_pz])
                    TT("dve", xnew[par][:, 0, hf * 512:(hf + 1) * 512], pz, gt1[:, jj, hf * 512:(hf + 1) * 512], ALU.mult, [s_pz, s_wm], [s_xn[par]])
                TT("pool", xnew[par][:, 0, :], xnew[par][:, 0, :], xt[par][:, 0, :], ALU.add, [s_xn[par], s_x[par]], [s_xn[par]])
                P.dma(xs2[tsl, :], xnew[par][:, 0, :], reads=[s_xn[par]], q=ST)
                norm_mod_T(xnew[par], s_xn[par], 1, l, 1, lambda j_: jj, h2b[bp], s_h2[bp], tmp, s_tmp, hTf=h2f, tok0=j * 128)
                pr_, s_pr = ps()
                for k in range(8):
                    MM(pr_[:, 0:36], h2f[:, k, :], wr[:, k, :], k == 0, k == 7, [s_h2[bp], s_wm], [s_pr])
                lg = R["lg"]
                CP("dve", lg, pr_[:, 0:36], [s_pr], [s_R])
                sc_ = R["sc"]
                rs = [s_R]
                h.reduce(sc_[:, 0:1], lg[:, 0:4], ALU.max, rs, rs)
                TS("dve", R["oh"], lg[:, 0:4], sc_[:, 0:1], None, ALU.is_equal, None, rs, rs)
                TS("dve", sc_[:, 1:2], sc_[:, 0:1], -1.0, None, ALU.mult, None, rs, rs)
                ACT(R["ge"], lg[:, 0:4], AF.Exp, rs, rs, bias=sc_[:, 1:2], accum_out=sc_[:, 2:3])
                P.dve(lambda e, o_=sc_[:, 2:3]: e.reciprocal(out=o_, in_=o_), rs, rs)
                el = lg[:, 4:36].rearrange("p (g e) -> p g e", e=8)
                TS("dve", R["esel"], el[:, 0, :], R["oh"][:, 0:1], None, ALU.mult, None, rs, rs)
                for g_ in range(1, 4):
                    h.stt("dve", R["esel"], el[:, g_, :], R["oh"][:, g_:g_ + 1], R["esel"], ALU.mult, ALU.add, rs, rs)
                h.reduce(sc_[:, 3:4], R["esel"], ALU.max, rs, rs)
                TS("dve", R["eq1"], R["esel"], sc_[:, 3:4], None, ALU.is_equal, None, rs, rs)
                h.stt("dve", R["es2"], R["eq1"], -1e30, R["esel"], ALU.mult, ALU.add, rs, rs)
                h.reduce(sc_[:, 4:5], R["es2"], ALU.max, rs, rs)
                TS("dve", R["eq2"], R["es2"], sc_[:, 4:5], None, ALU.is_equal, None, rs, rs)
                TS("dve", sc_[:, 5:6], sc_[:, 3:4], -1.0, None, ALU.mult, None, rs, rs)
                ACT(sc_[:, 6:7], sc_[:, 4:5], AF.Exp, rs, rs, bias=sc_[:, 5:6])
                TS("dve", sc_[:, 7:8], sc_[:, 6:7], 1.0, None, ALU.add, None, rs, rs)
                P.dve(lambda e, o_=sc_[:, 7:8]: e.reciprocal(out=o_, in_=o_), rs, rs)
                TT("dve", sc_[:, 7:8], sc_[:, 7:8], sc_[:, 2:3], ALU.mult, rs, rs)
                TT("dve", sc_[:, 6:7], sc_[:, 6:7], sc_[:, 7:8], ALU.mult, rs, rs)
                TS("dve", R["csel"], R["eq1"], sc_[:, 7:8], None, ALU.mult, None, rs, rs)
                h.stt("dve", R["csel"], R["eq2"], sc_[:, 6:7], R["csel"], ALU.mult, ALU.add, rs, rs)
                c3 = cmb[par].rearrange("p (g e) -> p g e", e=8)
                for g_ in range(4):
                    TS("dve", c3[:, g_, :], R["csel"], R["oh"][:, g_:g_ + 1], None, ALU.mult, None, rs, [s_cmb[par]])
                P.dma(cmbd[tsl, :], cmb[par], reads=[s_cmb[par]], q=ST)
            P.dma(h2T[:, bsl].rearrange("(k p) t -> p k t", p=128), h2b[bp][:, :, 0:n], reads=[s_h2[bp]], q=ST)

    def phase_moe(l, with_ctx, last):
        AR.reset(persist_mark)
        BS = 2048
        gt2 = AR.alloc([2, 1024], F32)
        gfin = AR.alloc([1024], F32)
        s_c = P.slot("E_c")
        for j_ in range(2):
            P.dma(gt2[:, j_, :], gtd[l, 1, j_:j_ + 1, :].broadcast_to([128, 1024]), writes=[s_c])
        P.dma(gfin, g_final.unsqueeze(0).broadcast_to([128, 1024]), writes=[s_c])
        hb = AR.alloc([8, BS], BF16)
        cm = AR.alloc([BS // 128, 32], F32)
        yacc = AR.alloc([BS // 128, 1024], F32)
        s_hb = P.slot("E_hb"); s_y = P.slots(BS // 128, "E_y")
        w1e = [AR.alloc([8, 256], BF16) for _ in range(2)]
        w3e = [AR.alloc([8, 256], BF16) for _ in range(2)]
        w2e = [AR.alloc([2, 1024], BF16) for _ in range(2)]
        s_we = P.slots(2, "E_w")
        su = [AR.alloc([512], F32) for _ in range(2)]
        aT = [AR.alloc([2, 512], BF16) for _ in range(2)]
        s_su = P.slots(2, "E_su"); s_aT = P.slots(2, "E_aT")
        xt = [AR.alloc([1024], F32) for _ in range(2)]
        xo = [AR.alloc([1024], F32) for _ in range(2)]
        st = [AR.alloc([2], F32) for _ in range(2)]
        junk = AR.alloc([1024], F32)
        s_x = P.slots(2, "E_x"); s_xo = P.slots(2, "E_xo")
        for (t0, n, isctx) in blocks(include_ctx=with_ctx, bs=BS):
            nt = n // 128
            jj = 1 if isctx else 0
            bsl = slice(t0, t0 + n)
            P.dma(hb[:, :, 0:n], h2T[:, bsl].rearrange("(k p) t -> p k t", p=128), writes=[s_hb], q=LD)
            P.dma(cm[:, 0:nt, :], cmbd[bsl, :].rearrange("(j p) e -> p j e", p=128), writes=[s_hb], q=LD)
            for j in range(nt):
                h.memset("pool", yacc[:, j, :], 0.0, [s_y[j]])
            items = [(e_, sb0, min(512, n - sb0)) for e_ in range(NEXP) for sb0 in range(0, n, 512)]
            loaded = set()

            def uv(ii):
                e_, sb0, nn = items[ii]
                ep = e_ % 2
                ap_ = ii % 2
                if e_ not in loaded:
                    loaded.add(e_)
                    P.dma(w1e[ep], wb1[l, e_].rearrange("(k p) n -> p k n", p=128), writes=[s_we[ep]], q=LD)
                    P.dma(w3e[ep], wb3[l, e_].rearrange("(k p) n -> p k n", p=128), writes=[s_we[ep]], q=LD)
                    P.dma(w2e[ep], wb2[l, e_].rearrange("(k p) n -> p k n", p=128), writes=[s_we[ep]], q=LD)
                for cc in range(2):
                    pu, s_pu = ps()
                    for k in range(8):
                        MM(pu[:, 0:nn], w1e[ep][:, k, cc * 128:(cc + 1) * 128], hb[:, k, sb0:sb0 + nn], k == 0, k == 7, [s_we[ep], s_hb], [s_pu])
                    pv, s_pv = ps()
                    for k in range(8):
                        MM(pv[:, 0:nn], w3e[ep][:, k, cc * 128:(cc + 1) * 128], hb[:, k, sb0:sb0 + nn], k == 0, k == 7, [s_we[ep], s_hb], [s_pv])
                    ACT(su[cc][:, 0:nn], pu[:, 0:nn], AF.Silu, [s_pu], [s_su[cc]])
                    TT("dve", aT[ap_][:, cc, 0:nn], su[cc][:, 0:nn], pv[:, 0:nn], ALU.mult, [s_su[cc], s_pv], [s_aT[ap_]])

            def yy(ii):
                e_, sb0, nn = items[ii]
                ep = e_ % 2
                ap_ = ii % 2
                for j in range(nn // 128):
                    tj = sb0 // 128 + j
                    for hf in range(2):
                        py, s_py = ps()
                        for cc in range(2):
                            MM(py, aT[ap_][:, cc, j * 128:(j + 1) * 128], w2e[ep][:, cc, hf * 512:(hf + 1) * 512], cc == 0, cc == 1,
                               [s_aT[ap_], s_we[ep]], [s_py])
                        ysl = yacc[:, tj, hf * 512:(hf + 1) * 512]
                        h.stt("dve", ysl, py, cm[:, tj, e_:e_ + 1], ysl, ALU.mult, ALU.add, [s_py, s_hb, s_y[tj]], [s_y[tj]])

            uv(0)
            for ii in range(len(items)):
                if ii + 1 < len(items):
                    uv(ii + 1)
                yy(ii)
            for j in range(nt):
                p_ = j % 2
                tsl = slice(t0 + j * 128, t0 + (j + 1) * 128)
                P.dma(xt[p_], xs2[tsl, :], writes=[s_x[p_]], q=LD)
                TT("dve", xo[p_], yacc[:, j, :], gt2[:, jj, :], ALU.mult, [s_y[j], s_c], [s_xo[p_]])
                TT("pool", xo[p_], xo[p_], xt[p_], ALU.add, [s_xo[p_], s_x[p_]], [s_xo[p_]])
                if not last:
                    P.dma(xs[tsl, :], xo[p_], reads=[s_xo[p_]], q=ST)
                elif not isctx:
                    ACT(junk, xo[p_], AF.Square, [s_xo[p_]], [s_xo[p_]], scale=1.0 / 32.0, accum_out=st[p_][:, 0:1])
                    TS("dve", st[p_][:, 0:1], st[p_][:, 0:1], EPS, None, ALU.add, None, [s_xo[p_]], [s_xo[p_]])
                    ACT(st[p_][:, 0:1], st[p_][:, 0:1], AF.Ln, [s_xo[p_]], [s_xo[p_]])
                    ACT(st[p_][:, 0:1], st[p_][:, 0:1], AF.Exp, [s_xo[p_]], [s_xo[p_]], scale=-0.5)
                    h.stt("dve", xo[p_], xo[p_], st[p_][:, 0:1], gfin, ALU.mult, ALU.mult, [s_xo[p_], s_c], [s_xo[p_]])
                    final_ops.append(P.dma(out[t0 - L + j * 128:t0 - L + (j + 1) * 128, :], xo[p_], reads=[s_xo[p_]], q=ST))

    final_ops = []
    P.barrier()

    def on(name):
        return phases is None or name in phases

    for l in range(layers):
        last = (l == layers - 1)
        if on("mod"):
            phase_mod(l)
            P.barrier()
        if on("A"):
            phase_A(l, xin if l == 0 else xs)
            P.barrier()
        if on("attA"):
            phase_attA(l, not last)
            P.barrier()
        if on("attC"):
            phase_attC(l, not last)
            P.barrier()
        if on("B"):
            phase_B(l)
            P.barrier()
        if on("merge"):
            phase_merge(l, xin if l == 0 else xs, not last)
            P.barrier()
        if on("moe"):
            phase_moe(l, not last, last)
            P.barrier()
    if not final_ops:
        final_ops = [P.barrier()]
    P.emit(final_wait_ops=final_ops)
    return nc, dbg_names


def _rope_tables(S, rot_dim, qscale):
    rows = S // 64
    row = np.repeat(np.arange(rows, dtype=np.float32), 64)
    col = np.tile(np.arange(64, dtype=np.float32), rows)
    n_freq = rot_dim // 4
    inv = (10000.0 ** (-np.arange(n_freq, dtype=np.float32) / n_freq)).astype(np.float32)
    ang = np.concatenate([row[:, None] * inv, col[:, None] * inv], axis=-1).astype(np.float32)
    c, s_ = np.cos(ang).astype(np.float32), np.sin(ang).astype(np.float32)
    T = L + S
    tab = np.zeros((T, 4, rot_dim), np.float32)
    c2 = np.concatenate([c, c], -1)
    s2 = np.concatenate([-s_, s_], -1)
    tab[L:, 0] = c2 * qscale
    tab[L:, 1] = s2 * qscale
    tab[L:, 2] = c2
    tab[L:, 3] = s2
    tab[:L, 0] = qscale
    tab[:L, 2] = 1.0
    return tab


def _consts(S):
    c = {}
    c["ident"] = np.eye(128, dtype=np.float32)
    c["ropeA"] = _rope_tables(S, 64, 64 ** -0.5)
    c["ropeC"] = _rope_tables(S, 32, 96 ** -0.5)
    j = np.arange(128)[:, None]
    i = np.arange(128)[None, :]
    am = np.zeros((128, 2, 128), np.float32)
    am[:, 0] = (j >= i)
    am[:, 1] = (j <= i)
    c["amask"] = am
    CC = 32
    mid = CC // 2 - 1
    s_ = np.arange(CC)[:, None]
    t_ = np.arange(CC)[None, :]
    bm = np.zeros((2 * CC, 4, 2 * CC), np.float32)
    cmk_f = (s_ <= mid).astype(np.float32) - (s_ <= t_)
    cmk_b = (s_ >= CC - 1 - mid).astype(np.float32) - (s_ >= t_)
    bm[:CC, 0, :CC] = cmk_f; bm[CC:, 0, CC:] = cmk_b
    bm[:CC, 1, :CC] = (s_ > t_); bm[CC:, 1, CC:] = (s_ < t_)
    bm[:CC, 2, :CC] = (s_ <= t_); bm[CC:, 2, CC:] = (s_ >= t_)
    bm[:CC, 3, :CC] = -cmk_f; bm[CC:, 3, CC:] = -cmk_b
    c["bmats"] = bm
    mk = np.zeros((2 * CC, 8, 2 * CC), np.float32)
    mk[:CC, :, :CC] = (s_ <= t_)[:, None, :]
    mk[CC:, :, CC:] = (s_ >= t_)[:, None, :]
    c["bmask"] = mk
    rm = np.zeros((2 * CC, 2), np.float32)
    rm[:CC, 0] = 1.0
    rm[CC:, 1] = 1.0
    c["brm"] = rm
    return c


def _fm(v, k):
    return np.ascontiguousarray(np.swapaxes(v.reshape(v.shape[:-1] + (k, 128)), -1, -2))


def make_in_map(inp, b, S):
    f = lambda a: np.ascontiguousarray(np.asarray(a, dtype=np.float32))
    m = {}
    m["xin"] = np.concatenate([f(inp["ctx"])[b], f(inp["x"])[b, :S]], axis=0)
    m["cvec"] = np.ascontiguousarray(np.stack([_fm(f(inp["c"])[b], 8), _fm(f(inp["c_ctx"]), 8)], axis=-1))
    m["w_mod"] = f(inp["w_mod"])
    m["b_modT"] = _fm(f(inp["b_mod"]), 48)
    m["b_mod"] = f(inp["b_mod"])
    m["g1T"] = _fm(f(inp["g_norm1"]), 8)
    m["g2T"] = _fm(f(inp["g_norm2"]), 8)
    m["w_in"] = f(inp["w_in"])
    m["a_sink"] = f(inp["a_sink"])
    m["lb_logits"] = f(inp["b_lb_logits"])
    m["b_onorm"] = f(inp["b_onorm"])
    m["gqT"] = _fm(f(inp["c_qnorm"]), 2)
    m["gkvT"] = _fm(f(inp["c_kvnorm"]), 1)
    m["w_uq"] = f(inp["w_uq"])
    wk = f(inp["w_ukv"]).reshape(2, 128, 8, 128)
    m["w_ukv"] = np.ascontiguousarray(np.concatenate([wk[..., :64].reshape(2, 128, 512), wk[..., 64:].reshape(2, 128, 512)], axis=-1))
    m["w_br"] = f(inp["w_br"])
    m["w_out"] = f(inp["w_out"])
    m["w_r"] = np.ascontiguousarray(np.concatenate([f(inp["w_rg"]), f(inp["w_re"])], axis=-1))
    m["w1"] = f(inp["w1"]); m["w3"] = f(inp["w3"]); m["w2"] = f(inp["w2"])
    m["g_final"] = f(inp["g_final"])
    return m


_CACHE = {}


def kernel(**inputs):
    x = np.asarray(inputs["x"])
    B, S, _ = x.shape
    if S not in _CACHE:
        _CACHE[S] = (build_program(S)[0], _consts(S))
    nc, consts = _CACHE[S]
    in_maps = []
    for b in range(B):
        m = make_in_map(inputs, b, S)
        m.update(consts)
        in_maps.append(m)
    res = run_bass_kernel_spmd(nc, in_maps, core_ids=list(range(B)))
    return np.stack([np.asarray(r["out"], dtype=np.float32) for r in res.results], axis=0)
```

```python
from concourse.bass_utils import run_bass_kernel_spmd
import ml_dtypes

import numpy as np
import concourse.bass as bass
import concourse.mybir as mybir

F32 = mybir.dt.float32
BF16 = mybir.dt.bfloat16
AF = mybir.ActivationFunctionType
ALU = mybir.AluOpType
AX = mybir.AxisListType

COMPUTE = ("pe", "act", "dve", "pool")
NDMA_SEMS = 24
SEM_EPOCH = 30000


class Slot:
    __slots__ = ("name", "writers", "readers", "excl")

    def __init__(self, name):
        self.name = name
        self.excl = False
        self.writers = {}
        self.readers = {}


class Op:
    __slots__ = ("id", "eng", "fn", "deps", "is_dma", "idx", "signal", "queue", "dma_no")

    def __init__(self, id, eng, fn, is_dma, queue):
        self.id = id
        self.eng = eng
        self.fn = fn
        self.deps = []
        self.is_dma = is_dma
        self.queue = queue
        self.signal = False
        self.idx = -1
        self.dma_no = -1


class Prog:
    def __init__(self, nc):
        self.nc = nc
        self.ops = []
        self.queues = {k: [] for k in ("pe", "act", "dve", "pool", "sync")}
        self.nslot = 0
        self.cur_barrier = None
        self.last_barrier_pos = 0

    def barrier(self):
        op = Op(len(self.ops), "sync", lambda e: e.nop(), False, "sync")
        self.ops.append(op)
        q = self.queues["sync"]
        op.idx = len(q)
        q.append(op)
        deps = set()
        if self.cur_barrier is not None:
            deps.add(self.cur_barrier)
        for qn, qq in self.queues.items():
            nd = 0
            got_c = False
            for o in reversed(qq[:-1] if qn == "sync" else qq):
                if o.is_dma:
                    if nd < NDMA_SEMS:
                        deps.add(o.id)
                        nd += 1
                elif not got_c:
                    deps.add(o.id)
                    got_c = True
                if nd >= NDMA_SEMS and got_c:
                    break
        op.deps = sorted(deps)
        self.cur_barrier = op.id
        return op

    def slot(self, name=None):
        self.nslot += 1
        return Slot(name or f"s{self.nslot}")

    def slots(self, n, name=None):
        return [self.slot(f"{name}{i}") for i in range(n)]

    def _add(self, eng, fn, reads, writes, is_dma=False):
        op = Op(len(self.ops), eng, fn, is_dma, eng)
        self.ops.append(op)
        q = self.queues[eng]
        op.idx = len(q)
        q.append(op)
        key = ("dma", op.id) if is_dma else eng
        deps = set()
        xs_ = [s for s in reads if s.excl]
        if xs_:
            writes = list(writes) + xs_
        for s in reads:
            for k, oid in s.writers.items():
                deps.add(oid)
        for s in writes:
            for k, oid in s.writers.items():
                deps.add(oid)
            for k, oid in s.readers.items():
                deps.add(oid)
        deps.discard(op.id)
        if self.cur_barrier is not None:
            deps.add(self.cur_barrier)
        for s in reads:
            s.readers[key] = op.id
        for s in writes:
            s.writers = {key: op.id}
            s.readers = {}
        op.deps = sorted(deps)
        return op

    def pe(self, fn, reads=(), writes=()):
        return self._add("pe", fn, reads, writes)

    def act(self, fn, reads=(), writes=()):
        return self._add("act", fn, reads, writes)

    def dve(self, fn, reads=(), writes=()):
        return self._add("dve", fn, reads, writes)

    def pool(self, fn, reads=(), writes=()):
        return self._add("pool", fn, reads, writes)

    def dma(self, out, in_, reads=(), writes=(), q="sync", **kw):
        return self._add(q, lambda e: e.dma_start(out=out, in_=in_, **kw), reads, writes, is_dma=True)

    def emit(self, final_wait_ops=()):
        nc = self.nc
        ops = self.ops
        for op in ops:
            for d in op.deps:
                dop = ops[d]
                if dop.is_dma:
                    continue
                if dop.eng == "pe" and op.eng == "pe" and not op.is_dma:
                    continue
                dop.signal = True
        for o in final_wait_ops:
            if not o.is_dma:
                o.signal = True
        sems = {}
        sigval = {}
        for qn, q in self.queues.items():
            cnt = 0
            ep = 0
            for op in q:
                if op.is_dma or not op.signal:
                    continue
                if cnt >= SEM_EPOCH:
                    cnt = 0
                    ep += 1
                cnt += 1
                sigval[op.id] = (qn, ep, cnt)
                if (qn, ep) not in sems:
                    sems[(qn, ep)] = nc.alloc_semaphore(f"s_{qn}_{ep}")
        dma_sems = {}
        dma_cnt = {}
        for qn, q in self.queues.items():
            n = 0
            for op in q:
                if op.is_dma:
                    op.dma_no = n
                    n += 1
            dma_cnt[qn] = n
            if n:
                dma_sems[qn] = [nc.alloc_semaphore(f"d_{qn}_{i}") for i in range(min(n, NDMA_SEMS))]
        dma_ops = {qn: [op for op in q if op.is_dma] for qn, q in self.queues.items()}

        def dma_sem_val(op):
            return dma_sems[op.queue][op.dma_no % NDMA_SEMS], 16 * (op.dma_no // NDMA_SEMS + 1)

        snaps = [None] * len(ops)
        self.nwaits = 0

        def run_queue(qn, eng):
            q = self.queues[qn]
            clock = {}
            known_dma = set()

            def need(d):
                dop = ops[d]
                if dop.is_dma:
                    if d in known_dma:
                        return
                    s, v = dma_sem_val(dop)
                    eng.wait_ge(s, v)
                    self.nwaits += 1
                    known_dma.add(d)
                    return
                if clock.get(dop.eng, -1) >= dop.idx:
                    return
                _, ep, cnt = sigval[d]
                eng.wait_ge(sems[(dop.eng, ep)], cnt)
                self.nwaits += 1
                clock[dop.eng] = dop.idx
                sn = snaps[d]
                if sn:
                    for k, v in sn.items():
                        if clock.get(k, -1) < v:
                            clock[k] = v

            for op in q:
                for d in op.deps:
                    dop = ops[d]
                    if (not dop.is_dma) and dop.eng == "pe" and qn == "pe" and not op.is_dma:
                        continue
                    need(d)
                if op.is_dma and op.dma_no >= NDMA_SEMS:
                    need(dma_ops[qn][op.dma_no - NDMA_SEMS].id)
                snaps[op.id] = dict(clock)
                ins = op.fn(eng)
                if op.is_dma:
                    s, v = dma_sem_val(op)
                    ins.then_inc(s, 16)
                elif op.signal:
                    _, ep, cnt = sigval[op.id]
                    ins.then_inc(sems[(qn, ep)], 1)
            if qn == "sync":
                for o in final_wait_ops:
                    need(o.id)

        self._dry_snapshots(ops, sigval, snaps)

        with nc.Block() as block:
            @block.tensor
            def _(e):
                run_queue("pe", e)

            @block.scalar
            def _(e):
                run_queue("act", e)

            @block.vector
            def _(e):
                run_queue("dve", e)

            @block.gpsimd
            def _(e):
                run_queue("pool", e)

            @block.sync
            def _(e):
                run_queue("sync", e)

    def _dry_snapshots(self, ops, sigval, snaps):
        clocks = {qn: {} for qn in self.queues}
        for op in ops:
            clock = clocks[op.queue]
            for d in op.deps:
                dop = ops[d]
                if dop.is_dma:
                    continue
                if dop.eng == "pe" and op.queue == "pe" and not op.is_dma:
                    continue
                if clock.get(dop.eng, -1) >= dop.idx:
                    continue
                clock[dop.eng] = dop.idx
                sn = snaps[d]
                if sn:
                    for k, v in sn.items():
                        if clock.get(k, -1) < v:
                            clock[k] = v
            snaps[op.id] = dict(clock)


class Arena:
    def __init__(self, nc, nbytes, name="arena"):
        self.n = nbytes // 4
        self.t = nc.alloc_sbuf_tensor(name, [128, self.n], F32)
        self.off = 0
        self.peak = 0

    def reset(self, to=0):
        self.off = to

    def mark(self):
        return self.off

    def alloc(self, free_shape, dtype, parts=128):
        esz = 2 if dtype == BF16 else 4
        nel = int(np.prod(free_shape))
        nw = (nel * esz + 3) // 4
        nw = (nw + 7) // 8 * 8
        assert self.off + nw <= self.n, f"arena overflow {self.off}+{nw}>{self.n}"
        ap = self.t[0:parts, self.off:self.off + nw]
        self.off += nw
        self.peak = max(self.peak, self.off)
        if dtype != F32:
            ap = ap.bitcast(dtype)
        ap = ap[:, 0:nel]
        if len(free_shape) >= 2:
            names = [f"a{i}" for i in range(len(free_shape))]
            kw = {nm: int(v) for nm, v in zip(names[1:], free_shape[1:])}
            ap = ap.rearrange("p (" + " ".join(names) + ") -> p " + " ".join(names), **kw)
        return ap

U32 = mybir.dt.uint32
D = 1024
L = 256
EPS = 1e-6
O_AQ, O_AK, O_AV, O_BQ, O_BI, O_BZF, O_BZB, O_BG, O_CQ, O_CKV, O_CKR, O_GL = (
    0, 512, 640, 768, 1280, 1792, 2304, 2816, 3328, 3584, 3712, 3744)
IN_W = 6816
NEXP = 32


class H:
    def __init__(self, P):
        self.P = P

    def mm(self, out, lhsT, rhs, start, stop, reads, writes, tp=None):
        if tp is None:
            self.P.pe(lambda e: e.matmul(out, lhsT=lhsT, rhs=rhs, start=start, stop=stop), reads, writes)
        else:
            self.P.pe(lambda e: e.matmul(out, lhsT=lhsT, rhs=rhs, start=start, stop=stop, tile_position=tp), reads, writes)

    def tr(self, out, in_, ident, reads, writes):
        self.P.pe(lambda e: e.transpose(out=out, in_=in_, identity=ident), reads, writes)

    def act(self, out, in_, func, reads, writes, **kw):
        self.P.act(lambda e: e.activation(out=out, in_=in_, func=func, **kw), reads, writes)

    def tt(self, eng, out, in0, in1, op, reads, writes):
        self.P._add(eng, lambda e: e.tensor_tensor(out=out, in0=in0, in1=in1, op=op), reads, writes)

    def ts(self, eng, out, in0, s1, s2, op0, op1, reads, writes, **kw):
        if s2 is None:
            self.P._add(eng, lambda e: e.tensor_scalar(out=out, in0=in0, scalar1=s1, scalar2=None, op0=op0, **kw), reads, writes)
        else:
            self.P._add(eng, lambda e: e.tensor_scalar(out=out, in0=in0, scalar1=s1, scalar2=s2, op0=op0, op1=op1, **kw), reads, writes)

    def stt(self, eng, out, in0, scalar, in1, op0, op1, reads, writes):
        self.P._add(eng, lambda e: e.scalar_tensor_tensor(out=out, in0=in0, scalar=scalar, in1=in1, op0=op0, op1=op1), reads, writes)

    def cp(self, eng, out, in_, reads, writes):
        if eng == "act":
            self.P.act(lambda e: e.activation(out=out, in_=in_, func=AF.Identity), reads, writes)
        else:
            self.P._add(eng, lambda e: e.tensor_copy(out=out, in_=in_), reads, writes)

    def memset(self, eng, ap, val, writes):
        self.P._add(eng, lambda e: e.memset(ap, val), (), writes)

    def reduce(self, out, in_, op, reads, writes, axis=AX.X):
        self.P.dve(lambda e: e.tensor_reduce(out=out, in_=in_, axis=axis, op=op), reads, writes)


def build_program(S, dbg=False, layers=2, phases=None):
    T = L + S
    NT = T // 128
    nc = bass.Bass("TRN2", target_bir_lowering=False)
    P = Prog(nc)
    h = H(P)
    MM, TR, ACT, TT, TS, CP = h.mm, h.tr, h.act, h.tt, h.ts, h.cp
    LD = "sync"
    ST = "sync"

    def din(name, shape, dt=F32):
        return nc.dram_tensor(name, list(shape), dt, kind="ExternalInput").ap()

    dbg_names = []

    def dscr(name, shape, dt=BF16):
        if dbg:
            dbg_names.append(name)
            return nc.dram_tensor(name, list(shape), dt, kind="ExternalOutput").ap()
        return nc.dram_tensor(name, list(shape), dt).ap()

    xin = din("xin", [T, D])
    cvec = din("cvec", [128, 8, 2])
    w_mod = din("w_mod", [2, D, 6 * D])
    b_modT = din("b_modT", [2, 128, 48])
    b_mod = din("b_mod", [2, 6 * D])
    g1T = din("g1T", [2, 128, 8])
    g2T = din("g2T", [2, 128, 8])
    w_in = din("w_in", [2, D, IN_W])
    a_sink = din("a_sink", [2, 8])
    lb_logits = din("lb_logits", [2, 2, 512])
    b_onorm = din("b_onorm", [2, 512])
    gqT = din("gqT", [2, 128, 2])
    gkvT = din("gkvT", [2, 128, 1])
    w_uq = din("w_uq", [2, 256, 768])
    w_ukv = din("w_ukv", [2, 128, 1024])
    w_br = din("w_br", [2, 3, 512, D])
    w_out = din("w_out", [2, D, D])
    w_r = din("w_r", [2, D, 36])
    w1 = din("w1", [2, NEXP, D, 256])
    w3 = din("w3", [2, NEXP, D, 256])
    w2 = din("w2", [2, NEXP, 256, D])
    g_final = din("g_final", [D])
    ident_d = din("ident", [128, 128])
    ropeA = din("ropeA", [T, 4, 64])
    ropeC = din("ropeC", [T, 4, 32])
    amask = din("amask", [128, 2, 128])
    bmats = din("bmats", [64, 4, 64])
    bmask = din("bmask", [64, 8, 64], F32)
    brm = din("brm", [64, 2], F32)
    out = nc.dram_tensor("out", [S, D], F32, kind="ExternalOutput").ap()

    xs = dscr("xs", [T, D], F32)
    xs2 = dscr("xs2", [T, D], F32)
    wb_in = dscr("wb_in", [2, D, IN_W])
    wb_uq = dscr("wb_uq", [2, 256, 768])
    wb_ukv = dscr("wb_ukv", [2, 128, 1024])
    wb_br = dscr("wb_br", [2, 3, 512, D])
    wb_out = dscr("wb_out", [2, D, D])
    wb1 = dscr("wb1", [2, NEXP, D, 256])
    wb3 = dscr("wb3", [2, NEXP, D, 256])
    wb2 = dscr("wb2", [2, NEXP, 256, D])
    qaT = dscr("qaT", [512, T])
    kaT = dscr("kaT", [128, T])
    va = dscr("va", [T, 128])
    qbT = dscr("qbT", [512, T])
    ib = dscr("ib", [T, 512])
    zf = dscr("zf", [T, 512])
    zb = dscr("zb", [T, 512])
    sgb = dscr("sgb", [T, 512])
    qcT = dscr("qcT", [8, 96, T])
    kcT = dscr("kcT", [512, T])
    krT = dscr("krT", [32, T])
    vc = dscr("vc", [T, 512])
    gT = dscr("gT", [3072, T])
    oT = dscr("oT", [3, 512, T])
    ofb = dscr("ofb", [2, T, 512])
    h2T = dscr("h2T", [D, T])
    cmbd = dscr("cmbd", [T, 32], F32)
    gtd = dscr("gtd", [2, 2, 2, 1024], F32)

    AR = Arena(nc, 196 * 1024)
    ident = AR.alloc([128], F32)
    identb = AR.alloc([128], BF16)
    onesf = AR.alloc([128], F32)
    modA = AR.alloc([2, 2, 8, 2], F32)
    modB = AR.alloc([2, 2, 8, 2], F32)
    sexp = AR.alloc([2, 8], F32)
    s_const = P.slot("const")
    s_mod = P.slot("mod")
    persist_mark = AR.mark()

    PSB = [nc.alloc_psum_tensor(f"psb{i}", [128, 512], F32)[:, :] for i in range(8)]
    PSS = P.slots(8, "ps")
    for s__ in PSS:
        s__.excl = True
    ps_rr = [0]

    def ps():
        i = 2 + ps_rr[0] % 6
        ps_rr[0] += 1
        return PSB[i], PSS[i]

    acc_rr = [0]

    def psacc(fixed=None):
        if fixed is not None:
            return PSB[fixed], PSS[fixed]
        i = acc_rr[0] % 2
        acc_rr[0] += 1
        return PSB[i], PSS[i]


    P.dma(ident, ident_d, writes=[s_const])
    CP("dve", identb, ident, [s_const], [s_const])
    h.memset("pool", onesf, 1.0, [s_const])
    P.dma(sexp, a_sink.rearrange("l h -> (l h)").unsqueeze(0).broadcast_to([128, 16]).rearrange("p (l h) -> p l h", h=8), writes=[s_const])
    ACT(sexp, sexp, AF.Exp, [s_const], [s_const])

    s_w = P.slot("wcast")
    if phases is None or "W" in phases:
        for l in range(layers):
            for r in range(8):
                P.dma(wb_in[l, r * 128:(r + 1) * 128, :], w_in[l, r * 128:(r + 1) * 128, :], q="pool")
            P.dma(wb_uq[l], w_uq[l], q="pool")
            P.dma(wb_ukv[l], w_ukv[l], q="pool")
            for n in range(3):
                for r in range(4):
                    P.dma(wb_br[l, n, r * 128:(r + 1) * 128, :], w_br[l, n, r * 128:(r + 1) * 128, :], q="pool")
            for r in range(8):
                P.dma(wb_out[l, r * 128:(r + 1) * 128, :], w_out[l, r * 128:(r + 1) * 128, :], q="pool")
            for e in range(NEXP):
                for r in range(0, 8, 4):
                    P.dma(wb1[l, e, r * 128:(r + 4) * 128, :], w1[l, e, r * 128:(r + 4) * 128, :], q="pool")
                    P.dma(wb3[l, e, r * 128:(r + 4) * 128, :], w3[l, e, r * 128:(r + 4) * 128, :], q="pool")
                P.dma(wb2[l, e], w2[l, e], q="pool")

    def phase_mod(l):
        AR.reset(persist_mark)
        cv = AR.alloc([8, 2], F32)
        scv = AR.alloc([8, 2], F32)
        screp = AR.alloc([8, 2, 128], F32)
        bm = AR.alloc([48], F32)
        g1 = AR.alloc([8], F32)
        g2 = AR.alloc([8], F32)
        modv = AR.alloc([48, 2], F32)
        brow = AR.alloc([2, 1024], F32)
        gst = AR.alloc([2, 1024], F32)
        s_gst = P.slots(2, "gst")
        wm = [AR.alloc([8, 1024], F32) for _ in range(2)]
        s_l = P.slot()
        s_wm = P.slots(2, "wm")
        P.dma(cv, cvec, writes=[s_l])
        P.dma(bm, b_modT[l], writes=[s_l])
        P.dma(g1, g1T[l], writes=[s_l])
        P.dma(g2, g2T[l], writes=[s_l])
        for w_i, c0 in enumerate((2048, 5120)):
            P.dma(brow[:, w_i, :], b_mod[l:l + 1, c0:c0 + 1024].broadcast_to([128, 1024]), writes=[s_l])
        ACT(scv, cv, AF.Silu, [s_l], [s_l])
        for k in range(8):
            for j in range(2):
                TS("dve", screp[:, k, j, :], onesf, scv[:, k, j:j + 1], None, ALU.mult, None, [s_l, s_const], [s_l])
        pmod, s_pmod = psacc()
        for g in range(6):
            b = g % 2
            P.dma(wm[b], w_mod[l, :, g * 1024:(g + 1) * 1024].rearrange("(k p) n -> p k n", p=128), writes=[s_wm[b]])
            for m in range(8):
                for k in range(8):
                    MM(pmod[:, (g * 8 + m) * 2:(g * 8 + m) * 2 + 2], wm[b][:, k, m * 128:(m + 1) * 128], scv[:, k, :], k == 0, k == 7,
                       [s_wm[b], s_l], [s_pmod])
            if g in (2, 5):
                w_i = 0 if g == 2 else 1
                for j in range(2):
                    for hf in range(2):
                        pg, s_pg = ps()
                        for k in range(8):
                            MM(pg, screp[:, k, j, :], wm[b][:, k, hf * 512:(hf + 1) * 512], k == 0, k == 7, [s_wm[b], s_l], [s_pg])
                        TT("dve", gst[:, j, hf * 512:(hf + 1) * 512], pg, brow[:, w_i, hf * 512:(hf + 1) * 512], ALU.add,
                           [s_pg, s_l], [s_gst[j]])
                    P.dma(gtd[l, w_i, j:j + 1, :], gst[0:1, j, :], reads=[s_gst[j]])
        TT("dve", modv, pmod[:, 0:96].rearrange("p (m j) -> p m j", j=2), bm.unsqueeze(2).broadcast_to([128, 48, 2]), ALU.add,
           [s_pmod, s_l], [s_l])
        for w_i, (sh0, sc0, g) in enumerate(((0, 8, g1), (24, 32, g2))):
            TS("dve", modA[:, l, w_i, :, :], modv[:, sc0:sc0 + 8, :], 1.0, None, ALU.add, None, [s_l], [s_mod])
            TT("dve", modA[:, l, w_i, :, :], modA[:, l, w_i, :, :], g.unsqueeze(2).broadcast_to([128, 8, 2]), ALU.mult, [s_l, s_mod], [s_mod])
            CP("dve", modB[:, l, w_i, :, :], modv[:, sh0:sh0 + 8, :], [s_l], [s_mod])

    def norm_mod_T(xt, s_x, ntile, l, which, jfun, hTb, s_hT, tmp, s_tmp, hTf=None, tok0=0):
        ms = tmp["ms"]
        for j in range(ntile):
            ACT(tmp["junk"], xt[:, j, :], AF.Square, [s_x], [s_tmp], scale=1.0 / 32.0, accum_out=ms[:, j:j + 1])
        TS("dve", ms[:, 0:ntile], ms[:, 0:ntile], EPS, None, ALU.add, None, [s_tmp], [s_tmp])
        ACT(ms[:, 0:ntile], ms[:, 0:ntile], AF.Ln, [s_tmp], [s_tmp])
        ACT(ms[:, 0:ntile], ms[:, 0:ntile], AF.Exp, [s_tmp], [s_tmp], scale=-0.5)
        for j in range(ntile):
            jj = jfun(j)
            if hTf is None:
                xn = tmp["xnb"]
                TS("dve", xn, xt[:, j, :], ms[:, j:j + 1], None, ALU.mult, None, [s_x, s_tmp], [s_tmp])
                pt, s_pt = ps()
                ptb = pt.bitcast(BF16)
                for k in range(8):
                    TR(ptb[:, k * 128:(k + 1) * 128], xn[:, k * 128:(k + 1) * 128], identb, [s_tmp, s_const], [s_pt])
                t2 = tmp["t2"]
                TT("dve", t2, ptb.rearrange("p (k t) -> p k t", t=128),
                   modA[:, l, which, :, jj:jj + 1].broadcast_to([128, 8, 128]), ALU.mult, [s_pt, s_mod], [s_tmp])
                TT("pool", hTb[:, :, tok0 + j * 128:tok0 + (j + 1) * 128], t2,
                   modB[:, l, which, :, jj:jj + 1].broadcast_to([128, 8, 128]), ALU.add, [s_tmp, s_mod], [s_hT])
            else:
                xn = tmp["xnf"]
                TS("dve", xn, xt[:, j, :], ms[:, j:j + 1], None, ALU.mult, None, [s_x, s_tmp], [s_tmp])
                for hf in range(2):
                    pt, s_pt = ps()
                    for k in range(4):
                        kk = hf * 4 + k
                        TR(pt[:, k * 128:(k + 1) * 128], xn[:, kk * 128:(kk + 1) * 128], ident, [s_tmp, s_const], [s_pt])
                    t2 = tmp["t2f"]
                    TT("dve", t2, pt.rearrange("p (k t) -> p k t", t=128),
                       modA[:, l, which, hf * 4:hf * 4 + 4, jj:jj + 1].broadcast_to([128, 4, 128]), ALU.mult, [s_pt, s_mod], [s_tmp])
                    TT("pool", hTf[:, hf * 4:hf * 4 + 4, j * 128:(j + 1) * 128], t2,
                       modB[:, l, which, hf * 4:hf * 4 + 4, jj:jj + 1].broadcast_to([128, 4, 128]), ALU.add, [s_tmp, s_mod], [s_hT])
                CP("act", hTb[:, :, tok0 + j * 128:tok0 + (j + 1) * 128], hTf[:, :, j * 128:(j + 1) * 128], [s_hT], [s_hT])

    def blocks(include_ctx=True, bs=512):
        bl = []
        if include_ctx:
            bl.append((0, L, True))
        t = L
        while t < T:
            n = min(bs, T - t)
            bl.append((t, n, False))
            t += n
        return bl


    NTM = O_GL

    def phase_A(l, xsrc):
        AR.reset(persist_mark)
        ST = "pool"
        wsb = AR.alloc([8, NTM], BF16)
        wuq = AR.alloc([2, 768], BF16)
        wukv = AR.alloc([1024], BF16)
        gq = AR.alloc([2], F32)
        gkv = AR.alloc([1], F32)
        s_wsb = P.slot("wsb")
        for k in range(8):
            P.dma(wsb[:, k, :], wb_in[l, k * 128:(k + 1) * 128, 0:NTM], writes=[s_wsb])
        P.dma(wuq, wb_uq[l].rearrange("(k p) n -> p k n", p=128), writes=[s_wsb])
        P.dma(wukv, wb_ukv[l], writes=[s_wsb])
        P.dma(gq, gqT[l], writes=[s_wsb])
        P.dma(gkv, gkvT[l], writes=[s_wsb])

        def dbl(shape, dt):
            return [AR.alloc(shape, dt) for _ in range(2)]
        wg = dbl([8, 512], BF16)
        s_wg = P.slots(2, "wg")
        xt = dbl([1, 1024], F32)
        s_x = P.slots(2, "xt")
        hT = dbl([8, 512], BF16)
        s_hT = P.slots(2, "hT")
        rA = dbl([4, 64], F32)
        rC = dbl([4, 32], F32)
        s_r = P.slots(2, "rope")
        tmp = dict(ms=AR.alloc([8], F32), junk=AR.alloc([1024], F32), xnb=AR.alloc([1024], BF16), t2=AR.alloc([8, 128], BF16))
        tmp_q = AR.alloc([768], F32)
        s_tq = P.slot("tq")
        s_tmp = P.slot("tmpA")
        qa_tm = dbl([512], BF16); ka_tm = dbl([128], BF16)
        ropet = dbl([512], F32); ropeu = dbl([512], F32)
        va_s = dbl([128], BF16)
        ib_s = dbl([512], BF16); zf_s = dbl([512], BF16); zb_s = dbl([512], BF16); sg_s = dbl([512], BF16)
        cst = dbl([8], F32)
        cqn = dbl([256], BF16); ckvn = dbl([128], BF16); kr_tm = dbl([32], BF16)
        qc_tm = dbl([768], BF16)
        vc_s = dbl([512], BF16)
        qaT_s = AR.alloc([4, 512], BF16); kaT_s = AR.alloc([512], BF16); qbT_s = AR.alloc([4, 512], BF16)
        cqnT = AR.alloc([2, 512], BF16); ckvnT = AR.alloc([512], BF16); krT_s = AR.alloc([512], BF16)
        qcT_s = AR.alloc([8, 512], BF16); kcT_s = AR.alloc([4, 512], BF16)
        gT_s = dbl([4, 512], BF16)
        s_ev = {}

        def sl(name, par=0):
            key = (name, par)
            if key not in s_ev:
                s_ev[key] = P.slot(name + str(par))
            return s_ev[key]

        tix = 0
        for bi_, (t0, n, isctx) in enumerate(blocks()):
            bp = bi_ % 2
            nt = n // 128
            jj = 1 if isctx else 0
            rd = [s_hT[bp], s_wsb]
            for j in range(nt):
                par = tix % 2
                tix += 1
                tt0 = t0 + j * 128
                P.dma(xt[par][:, 0, :], xsrc[tt0:tt0 + 128, :], writes=[s_x[par]], q=LD)
                P.dma(rA[par], ropeA[tt0:tt0 + 128], writes=[s_r[par]], q=LD)
                P.dma(rC[par], ropeC[tt0:tt0 + 128], writes=[s_r[par]], q=LD)
                norm_mod_T(xt[par], s_x[par], 1, l, 0, lambda j_: jj, hT[bp], s_hT[bp], tmp, s_tmp, tok0=j * 128)

                def tm_group(c0, ncols):
                    pg, s_pg = ps()
                    for k in range(8):
                        MM(pg[:, 0:ncols], hT[bp][:, k, j * 128:(j + 1) * 128], wsb[:, k, c0:c0 + ncols], k == 0, k == 7, rd, [s_pg])
                    return pg, s_pg

                def rope(dst, src, s_src, nh, hd, tab, a0, dsl, eng2="pool"):
                    half = hd // 2
                    s3 = src.rearrange("p (h d) -> p h d", d=hd)
                    t3 = ropet[par][:, 0:nh * hd].rearrange("p (h d) -> p h d", d=hd)
                    u3 = ropeu[par][:, 0:nh * hd].rearrange("p (h d) -> p h d", d=hd)
                    s_t = sl("ropet", par)
                    TT("dve", t3, s3, tab[:, a0, :].unsqueeze(1).broadcast_to([128, nh, hd]), ALU.mult, [s_src, s_r[par]], [s_t])
                    TT("dve", u3[:, :, 0:half], s3[:, :, half:hd], tab[:, a0 + 1, 0:half].unsqueeze(1).broadcast_to([128, nh, half]),
                       ALU.mult, [s_src, s_r[par]], [s_t])
                    TT("dve", u3[:, :, half:hd], s3[:, :, 0:half], tab[:, a0 + 1, half:hd].unsqueeze(1).broadcast_to([128, nh, half]),
                       ALU.mult, [s_src, s_r[par]], [s_t])
                    TT(eng2, dst, ropet[par][:, 0:nh * hd], ropeu[par][:, 0:nh * hd], ALU.add, [s_t], [dsl])

                tsl = slice(tt0, tt0 + 128)
                import os as _os
                KA = int(_os.environ.get("KA", "9"))
                if KA < 2:
                    continue
                KB = int(_os.environ.get("KB", "15"))
                if KB & 1:
                    pg, s_pg = tm_group(O_AQ, 512)
                    rope(qa_tm[par], pg, s_pg, 8, 64, rA[par], 0, sl("qa_tm", par))
                if KB & 2:
                    pt, s_pt = ps()
                    ptb = pt.bitcast(BF16)
                    for c in range(4):
                        TR(ptb[:, c * 128:(c + 1) * 128], qa_tm[par][:, c * 128:(c + 1) * 128], identb, [sl("qa_tm", par), s_const], [s_pt])
                    CP("act", qaT_s[:, :, j * 128:(j + 1) * 128], ptb[:, 0:512].rearrange("p (c t) -> p c t", t=128), [s_pt], [sl("qaT_s")])
                if KB & 4:
                    pg, s_pg = tm_group(O_AK, 256)
                    rope(ka_tm[par], pg[:, 0:128], s_pg, 2, 64, rA[par], 2, sl("ka_tm", par))
                    KC = int(_os.environ.get("KC", "3"))
                    if KC >= 2:
                        CP("act", va_s[par], pg[:, 128:256], [s_pg], [sl("va_s", par)])
                    if KC >= 3:
                        P.dma(va[tsl, :], va_s[par], reads=[sl("va_s", par)], q=ST)
                if KB & 8:
                    pt, s_pt = ps()
                    ptb = pt.bitcast(BF16)
                    TR(ptb[:, 0:128], ka_tm[par], identb, [sl("ka_tm", par), s_const], [s_pt])
                    CP("act", kaT_s[:, j * 128:(j + 1) * 128], ptb[:, 0:128], [s_pt], [sl("kaT_s")])
                if KA < 3:
                    continue
                for c0, dst, nm, dd in ((O_BI, ib_s, "ib_s", ib), (O_BZF, zf_s, "zf_s", zf), (O_BZB, zb_s, "zb_s", zb)):
                    pg, s_pg = tm_group(c0, 512)
                    CP("dve" if nm != "zb_s" else "act", dst[par], pg, [s_pg], [sl(nm, par)])
                    P.dma(dd[tsl, :], dst[par], reads=[sl(nm, par)], q=ST)
                pg, s_pg = tm_group(O_BG, 512)
                ACT(sg_s[par], pg, AF.Silu, [s_pg], [sl("sg_s", par)])
                P.dma(sgb[tsl, :], sg_s[par], reads=[sl("sg_s", par)], q=ST)
                if KA < 4:
                    continue
                pg, s_pg = tm_group(O_CQ, 416)
                cs = cst[par]
                s_cs = sl("cst", par)
                ACT(tmp["junk"][:, 0:256], pg[:, 0:256], AF.Square, [s_pg], [s_tmp, s_cs], scale=1.0 / 16.0, accum_out=cs[:, 0:1])
                ACT(tmp["junk"][:, 0:128], pg[:, 256:384], AF.Square, [s_pg], [s_tmp, s_cs], scale=float(128 ** -0.5), accum_out=cs[:, 1:2])
                TS("dve", cs[:, 0:2], cs[:, 0:2], EPS, None, ALU.add, None, [s_cs], [s_cs])
                ACT(cs[:, 0:2], cs[:, 0:2], AF.Ln, [s_cs], [s_cs])
                ACT(cs[:, 0:2], cs[:, 0:2], AF.Exp, [s_cs], [s_cs], scale=-0.5)
                TS("dve", cqn[par], pg[:, 0:256], cs[:, 0:1], None, ALU.mult, None, [s_pg, s_cs], [sl("cqn", par)])
                TS("dve", ckvn[par], pg[:, 256:384], cs[:, 1:2], None, ALU.mult, None, [s_pg, s_cs], [sl("ckvn", par)])
                rope(kr_tm[par], pg[:, 384:416], s_pg, 1, 32, rC[par], 2, sl("kr_tm", par))
                pt, s_pt = ps()
                ptb = pt.bitcast(BF16)
                TR(ptb[:, 0:128], cqn[par][:, 0:128], identb, [sl("cqn", par), s_const], [s_pt])
                TR(ptb[:, 128:256], cqn[par][:, 128:256], identb, [sl("cqn", par), s_const], [s_pt])
                TR(ptb[:, 256:384], ckvn[par], identb, [sl("ckvn", par), s_const], [s_pt])
                TR(ptb[0:32, 384:512], kr_tm[par], identb, [sl("kr_tm", par), s_const], [s_pt])
                for c in range(2):
                    ACT(cqnT[:, c, j * 128:(j + 1) * 128], ptb[:, c * 128:(c + 1) * 128], AF.Identity, [s_pt, s_wsb], [sl("cqnT")],
                        scale=gq[:, c:c + 1])
                ACT(ckvnT[:, j * 128:(j + 1) * 128], ptb[:, 256:384], AF.Identity, [s_pt, s_wsb], [sl("ckvnT")], scale=gkv[:, 0:1])
                CP("dve", krT_s[0:32, j * 128:(j + 1) * 128], ptb[0:32, 384:512], [s_pt], [sl("krT_s")])
                if KA < 5:
                    continue
                pq0, s_pq0 = ps()
                pq1, s_pq1 = ps()
                for c in range(2):
                    MM(pq0, cqnT[:, c, j * 128:(j + 1) * 128], wuq[:, c, 0:512], c == 0, c == 1, [sl("cqnT"), s_wsb], [s_pq0])
                for c in range(2):
                    MM(pq1[:, 0:256], cqnT[:, c, j * 128:(j + 1) * 128], wuq[:, c, 512:768], c == 0, c == 1, [sl("cqnT"), s_wsb], [s_pq1])
                qscale = float(96 ** -0.5)
                q3 = qc_tm[par].rearrange("p (h d) -> p h d", d=96)
                s_qc = sl("qc_tm", par)
                CP("act", tmp_q[:, 0:512], pq0, [s_pq0], [s_tq])
                CP("act", tmp_q[:, 512:768], pq1[:, 0:256], [s_pq1], [s_tq])
                tq3 = tmp_q.rearrange("p (h d) -> p h d", d=96)
                TS("dve", q3[:, :, 0:64], tq3[:, :, 0:64], qscale, None, ALU.mult, None, [s_tq], [s_qc])
                t3 = ropet[par][:, 0:256].rearrange("p (h d) -> p h d", d=32)
                u3 = ropeu[par][:, 0:256].rearrange("p (h d) -> p h d", d=32)
                s_t = sl("ropet", par)
                TT("dve", t3, tq3[:, :, 64:96], rC[par][:, 0, :].unsqueeze(1).broadcast_to([128, 8, 32]), ALU.mult, [s_tq, s_r[par]], [s_t])
                TT("dve", u3[:, :, 0:16], tq3[:, :, 80:96], rC[par][:, 1, 0:16].unsqueeze(1).broadcast_to([128, 8, 16]), ALU.mult,
                   [s_tq, s_r[par]], [s_t])
                TT("dve", u3[:, :, 16:32], tq3[:, :, 64:80], rC[par][:, 1, 16:32].unsqueeze(1).broadcast_to([128, 8, 16]), ALU.mult,
                   [s_tq, s_r[par]], [s_t])
                TT("pool", q3[:, :, 64:96], t3, u3, ALU.add, [s_t], [s_qc])
                pt, s_pt = ps()
                ptb = pt.bitcast(BF16)
                for hh in range(8):
                    TR(ptb[0:96, hh * 128:(hh + 1) * 128], qc_tm[par][:, hh * 96:(hh + 1) * 96], identb, [s_qc, s_const], [s_pt])
                CP("act", qcT_s[0:96, :, j * 128:(j + 1) * 128], ptb[0:96, :].rearrange("p (h t) -> p h t", t=128), [s_pt], [sl("qcT_s")])
                pv, s_pv = ps()
                MM(pv, ckvnT[:, j * 128:(j + 1) * 128], wukv[:, 512:1024], True, True, [sl("ckvnT"), s_wsb], [s_pv])
                CP("dve", vc_s[par], pv, [s_pv], [sl("vc_s", par)])
                P.dma(vc[tsl, :], vc_s[par], reads=[sl("vc_s", par)], q=ST)
            bsl = slice(t0, t0 + n)
            if KA < 6:
                continue
            for c in range(4):
                pg, s_pg = ps()
                for k in range(8):
                    MM(pg[:, 0:n], wsb[:, k, O_BQ + c * 128:O_BQ + (c + 1) * 128], hT[bp][:, k, 0:n], k == 0, k == 7, rd, [s_pg])
                CP("act" if c % 2 else "dve", qbT_s[:, c, 0:n], pg[:, 0:n], [s_pg], [sl("qbT_s")])
            for c in range(4):
                pg, s_pg = ps()
                MM(pg[:, 0:n], wukv[:, c * 128:(c + 1) * 128], ckvnT[:, 0:n], True, True, [sl("ckvnT"), s_wsb], [s_pg])
                CP("dve", kcT_s[:, c, 0:n], pg[:, 0:n], [s_pg], [sl("kcT_s")])
            P.dma(qaT[:, bsl].rearrange("(c p) t -> p c t", p=128), qaT_s[:, :, 0:n], reads=[sl("qaT_s")], q=ST)
            P.dma(kaT[:, bsl], kaT_s[:, 0:n], reads=[sl("kaT_s")], q=ST)
            P.dma(qbT[:, bsl].rearrange("(c p) t -> p c t", p=128), qbT_s[:, :, 0:n], reads=[sl("qbT_s")], q=ST)
            P.dma(qcT[:, :, bsl].rearrange("h d t -> d h t"), qcT_s[0:96, :, 0:n], reads=[sl("qcT_s")], q=ST)
            P.dma(kcT[:, bsl].rearrange("(c p) t -> p c t", p=128), kcT_s[:, :, 0:n], reads=[sl("kcT_s")], q=ST)
            P.dma(krT[:, bsl], krT_s[0:32, 0:n], reads=[sl("krT_s")], q=ST)
            for g in range(6):
                gp = g % 2
                P.dma(wg[gp], wb_in[l, :, O_GL + g * 512:O_GL + (g + 1) * 512].rearrange("(k p) n -> p k n", p=128), writes=[s_wg[gp]], q=LD)
                for c in range(4):
                    pg, s_pg = ps()
                    for k in range(8):
                        MM(pg[:, 0:n], wg[gp][:, k, c * 128:(c + 1) * 128], hT[bp][:, k, 0:n], k == 0, k == 7, [s_hT[bp], s_wg[gp]], [s_pg])
                    ACT(gT_s[gp][:, c, 0:n], pg[:, 0:n], AF.Sigmoid, [s_pg], [sl("gT_s", gp)])
                P.dma(gT[g * 512:(g + 1) * 512, bsl].rearrange("(c p) t -> p c t", p=128), gT_s[gp][:, :, 0:n], reads=[sl("gT_s", gp)], q=ST)

    def att_finalize(po, s_po, n, sink_ap, dst_dram, tmpo, rdt, s_fin, eng_dma=ST, nh=1):
        if sink_ap is not None:
            TT("dve", rdt[64:65, 0:n].rearrange("p (g t) -> p g t", g=nh), po[64:65, 0:n].rearrange("p (g t) -> p g t", g=nh),
               sink_ap, ALU.add, [s_po, s_const], [s_fin])
            P.dve(lambda e: e.reciprocal(out=rdt[64:65, 0:n], in_=rdt[64:65, 0:n]), [s_fin], [s_fin])
        else:
            P.dve(lambda e: e.reciprocal(out=rdt[64:65, 0:n], in_=po[64:65, 0:n]), [s_po], [s_fin])
        pb, s_pb = ps()
        MM(pb[0:64, 0:n], onesf[64:65, 0:64], rdt[64:65, 0:n], True, True, [s_fin, s_const], [s_pb])
        CP("act", tmpo[0:64, 0:n], po[0:64, 0:n], [s_po], [s_fin])
        o16 = tmpo[0:64, 512:1024].bitcast(BF16)[:, 0:n]
        TT("dve", o16, tmpo[0:64, 0:n], pb[0:64, 0:n], ALU.mult, [s_fin, s_pb], [s_fin])
        P.dma(dst_dram, o16 if nh == 1 else o16.rearrange("p (g t) -> p g t", g=nh), reads=[s_fin], q=eng_dma)

    def phase_attA(l, with_ctx):
        AR.reset(persist_mark)
        kT = AR.alloc([2, T], BF16)
        vt = AR.alloc([NT, 2, 65], BF16)
        msk = AR.alloc([2, 128], BF16)
        mskf = AR.alloc([2, 128], F32)
        s_kv = P.slot("attA_kv")
        for kvh in range(2):
            P.dma(kT[0:64, kvh, :], kaT[kvh * 64:(kvh + 1) * 64, :], writes=[s_kv])
        h.memset("pool", vt, 1.0, [s_kv])
        for kvh in range(2):
            P.dma(vt[:, :, kvh, 0:64], va[:, kvh * 64:(kvh + 1) * 64].rearrange("(j p) d -> p j d", p=128), writes=[s_kv])
        P.dma(mskf, amask, writes=[s_kv])
        CP("dve", msk, mskf, [s_kv], [s_kv])
        qt = [AR.alloc([8, 128], BF16) for _ in range(2)]
        s_q = P.slots(2, "attA_q")
        pT = [AR.alloc([512], BF16) for _ in range(4)]
        s_pT = P.slots(4, "attA_pT")
        pcnt_ = [0]
        tmpo = [AR.alloc([1024], F32) for _ in range(2)]
        rdt = [AR.alloc([512], F32) for _ in range(2)]
        s_fin = P.slots(2, "attA_fin")
        nlat = S // 128
        qtiles = ([0, 1] if with_ctx else []) + list(range(2, NT))
        cnt = 0
        pcnt = 0
        for qi_, gi in enumerate(qtiles):
            qp = qi_ % 2
            P.dma(qt[qp][0:64], qaT[:, gi * 128:(gi + 1) * 128].rearrange("(h d) t -> d h t", d=64), writes=[s_q[qp]], q=LD)
            if gi < 2:
                keys = [(0, None), (1, None)]
            else:
                nq = gi - 2
                keys = [(0, None), (1, None)]
                if nq >= 1:
                    keys.append((gi - 1, 0))
                keys.append((gi, None))
                if nq + 1 < nlat:
                    keys.append((gi + 1, 1))
            for kvh in range(2):
                po, s_po = psacc()
                LA = 2
                ppl = {}

                def qk(ki, kvh=kvh, qp=qp, keys=keys, ppl=ppl):
                    kt, mk = keys[ki]
                    pss, s_pss = ps()
                    MM(pss, kT[0:64, kvh, kt * 128:(kt + 1) * 128], qt[qp][0:64, kvh * 4:(kvh + 1) * 4, :], True, True,
                       [s_kv, s_q[qp]], [s_pss])
                    pp = pcnt_[0] % 4
                    pcnt_[0] += 1
                    ACT(pT[pp], pss, AF.Exp, [s_pss], [s_pT[pp]])
                    if mk is not None:
                        p3 = pT[pp].rearrange("p (g t) -> p g t", g=4)
                        TT("pool", p3, p3, msk[:, mk, :].unsqueeze(1).broadcast_to([128, 4, 128]), ALU.mult, [s_pT[pp], s_kv], [s_pT[pp]])
                    ppl[ki] = pp
                for ki in range(min(LA, len(keys))):
                    qk(ki)
                for ki, (kt, mk) in enumerate(keys):
                    if ki + LA < len(keys):
                        qk(ki + LA)
                    pp = ppl.pop(ki)
                    MM(po[0:65, :], vt[:, kt, kvh, :], pT[pp], ki == 0, ki == len(keys) - 1, [s_kv, s_pT[pp]], [s_po])
                fp = cnt % 2
                cnt += 1
                att_finalize(po, s_po, 512, sexp[64:65, l, kvh * 4:(kvh + 1) * 4].unsqueeze(2).broadcast_to([1, 4, 128]),
                             oT[0, kvh * 256:(kvh + 1) * 256, gi * 128:(gi + 1) * 128].rearrange("(g d) t -> d g t", d=64),
                             tmpo[fp], rdt[fp], s_fin[fp], nh=4)

    def phase_attC(l, with_ctx, reset=True, accb=None):
        if reset:
            AR.reset(persist_mark)
        kT = [AR.alloc([T], BF16) for _ in range(2)]
        vt = [AR.alloc([NT, 65], BF16) for _ in range(2)]
        s_kv = P.slots(2, "attC_kv")
        qt = [AR.alloc([512], BF16) for _ in range(2)]
        s_q = P.slots(2, "attC_q")
        pT = [AR.alloc([512], BF16) for _ in range(5)]
        s_pT = P.slots(5, "attC_pT")
        pcnt_ = [0]
        tmpo = [AR.alloc([1024], F32) for _ in range(2)]
        rdt = [AR.alloc([512], F32) for _ in range(2)]
        s_fin = P.slots(2, "attC_fin")
        qblocks = blocks(include_ctx=with_ctx)
        cnt = 0
        pcnt = 0
        for hh in range(8):
            hp = hh % 2
            P.dma(kT[hp][0:64, :], kcT[hh * 64:(hh + 1) * 64, :], writes=[s_kv[hp]], q=LD)
            P.dma(kT[hp][64:96, :], krT, writes=[s_kv[hp]], q=LD)
            h.memset("pool", vt[hp], 1.0, [s_kv[hp]])
            P.dma(vt[hp][:, :, 0:64], vc[:, hh * 64:(hh + 1) * 64].rearrange("(j p) d -> p j d", p=128), writes=[s_kv[hp]], q=LD)
            for (t0, n, isctx) in qblocks:
                qp = cnt % 2
                cnt += 1
                P.dma(qt[qp][0:96, 0:n], qcT[hh, :, t0:t0 + n], writes=[s_q[qp]], q=LD)
                keys = [0, 1] if isctx else list(range(NT))
                po, s_po = psacc(accb)
                LA = 3
                ppl = {}

                def qk(ki, n=n, hp=hp, qp=qp, keys=keys, ppl=ppl):
                    kt = keys[ki]
                    pss, s_pss = ps()
                    MM(pss[:, 0:n], kT[hp][0:96, kt * 128:(kt + 1) * 128], qt[qp][0:96, 0:n], True, True, [s_kv[hp], s_q[qp]], [s_pss])
                    pp = pcnt_[0] % 5
                    pcnt_[0] += 1
                    ACT(pT[pp][:, 0:n], pss[:, 0:n], AF.Exp, [s_pss], [s_pT[pp]])
                    ppl[ki] = pp
                for ki in range(min(LA, len(keys))):
                    qk(ki)
                for ki, kt in enumerate(keys):
                    if ki + LA < len(keys):
                        qk(ki + LA)
                    pp = ppl.pop(ki)
                    MM(po[0:65, 0:n], vt[hp][:, kt, :], pT[pp][:, 0:n], ki == 0, ki == len(keys) - 1, [s_kv[hp], s_pT[pp]], [s_po])
                    yield 1
                att_finalize(po, s_po, n, None, oT[2, hh * 64:(hh + 1) * 64, t0:t0 + n], tmpo[qp], rdt[qp], s_fin[qp])

    def phase_B(l, accb=None):
        AR.reset(persist_mark)
        ST = "pool"
        C = 32
        R2 = 2 * C
        NCH = T // C
        GS = 4
        bm = AR.alloc([4, R2], F32)
        bmk = AR.alloc([8, R2], F32)
        rmk = AR.alloc([2], F32)
        lbt = AR.alloc([512], F32)
        c1t = AR.alloc([512], F32)
        l0 = AR.alloc([512], F32)
        s_c = P.slot("B_const")
        P.dma(bm[0:R2], bmats, writes=[s_c])
        P.dma(bmk[0:R2], bmask, writes=[s_c])
        P.dma(rmk[0:R2], brm, writes=[s_c])
        if l == 0:
            h.memset("pool", lbt, 0.0, [s_c])
            h.memset("pool", c1t, 1.0, [s_c])
        else:
            for d_ in range(2):
                P.dma(l0[d_ * C:(d_ + 1) * C, :], lb_logits[0, d_:d_ + 1, :].broadcast_to([C, 512]), writes=[s_c])
                P.dma(lbt[d_ * C:(d_ + 1) * C, :], lb_logits[1, d_:d_ + 1, :].broadcast_to([C, 512]), writes=[s_c])
            TT("dve", lbt[0:R2], lbt[0:R2], l0[0:R2], ALU.subtract, [s_c], [s_c])
            ACT(lbt[0:R2], lbt[0:R2], AF.Sigmoid, [s_c], [s_c])
            TS("dve", c1t[0:R2], lbt[0:R2], -1.0, 1.0, ALU.mult, ALU.add, [s_c], [s_c])
        Sst = AR.alloc([2, 8, 64], F32)
        Sb = AR.alloc([2, 8, 64], BF16)
        s_S = P.slot("B_S")
        s_Sb = P.slot("B_Sb")
        h.memset("pool", Sst, 0.0, [s_S])
        h.memset("pool", Sb, 0.0, [s_Sb])

        def dbl(shape, dt, n=2):
            return [AR.alloc(shape, dt) for _ in range(n)]
        z2 = dbl([GS, 512], BF16); v2 = dbl([GS, 512], BF16); q2 = dbl([8, GS, R2], BF16)
        s_z = P.slots(2, "B_z"); s_v = P.slots(2, "B_v"); s_q2 = P.slots(2, "B_q")
        sig = AR.alloc([GS, 512], F32); logf = dbl([GS, 512], F32); kk = dbl([GS, 512], F32)
        s_sig = P.slot("B_sig"); s_logf = P.slots(2, "B_logf"); s_kk = P.slots(2, "B_kk")
        ek = dbl([512], F32); e2 = dbl([512], F32); ktl = dbl([512], BF16); kh = dbl([2, 512], BF16)
        s_ek = P.slots(2, "B_ek"); s_e2 = P.slots(2, "B_e2"); s_kt = P.slots(2, "B_kt"); s_kh = P.slots(2, "B_kh")
        ktT = dbl([8, R2], BF16); s_ktT = P.slots(2, "B_ktT")
        eq = dbl([8, R2], F32); eqm = dbl([8, R2], F32); s_eq = P.slots(2, "B_eq"); s_eqm = P.slots(2, "B_eqm")
        qebf = dbl([8, R2], BF16); qebb = dbl([8, R2], BF16); qtl = dbl([8, R2], BF16)
        s_qe = P.slots(2, "B_qe"); s_qt = P.slots(2, "B_qt")
        attT = dbl([8, R2], BF16); s_att = P.slots(2, "B_att")
        o_s = dbl([GS, 512], BF16); s_os = P.slots(2, "B_os")
        for b_ in range(2):
            h.memset("pool", qebf[b_], 0.0, [s_qe[b_]])
            h.memset("pool", qebb[b_], 0.0, [s_qe[b_]])
        step = 0
        nctx = L // C
        for g in range(NCH // GS):
            gp = g % 2
            cf0 = g * GS
            gctx = nctx // GS
            cb0 = (nctx - GS * (g + 1)) if g < gctx else NCH - GS * (g - gctx + 1)
            fsl = slice(cf0 * C, (cf0 + GS) * C)
            P.dma(z2[gp][0:C], zf[fsl, :].rearrange("(s p) f -> p s f", p=C), writes=[s_z[gp]], q=LD)
            P.dma(v2[gp][0:C], ib[fsl, :].rearrange("(s p) f -> p s f", p=C), writes=[s_v[gp]], q=LD)
            for s_ in range(GS):
                cb = cb0 + GS - 1 - s_
                cf = cf0 + s_
                P.dma(z2[gp][C:R2, s_, :], zb[cb * C:(cb + 1) * C, :], writes=[s_z[gp]], q=LD)
                P.dma(v2[gp][C:R2, s_, :], ib[cb * C:(cb + 1) * C, :], writes=[s_v[gp]], q=LD)
                P.dma(q2[gp][0:64, :, s_, 0:C], qbT[:, cf * C:(cf + 1) * C].rearrange("(h d) t -> d h t", d=64), writes=[s_q2[gp]], q=LD)
                P.dma(q2[gp][0:64, :, s_, C:R2], qbT[:, cb * C:(cb + 1) * C].rearrange("(h d) t -> d h t", d=64), writes=[s_q2[gp]], q=LD)
            z2f = z2[gp][0:R2].rearrange("p s f -> p (s f)")
            sigf = sig[0:R2].rearrange("p s f -> p (s f)")
            ACT(sigf, z2f, AF.Sigmoid, [s_z[gp]], [s_sig])
            TT("dve", sig[0:R2], sig[0:R2], c1t[0:R2].unsqueeze(1).broadcast_to([R2, GS, 512]), ALU.mult, [s_sig, s_c], [s_sig])
            TT("dve", sig[0:R2], sig[0:R2], lbt[0:R2].unsqueeze(1).broadcast_to([R2, GS, 512]), ALU.add, [s_sig, s_c], [s_sig])
            ACT(logf[gp][0:R2].rearrange("p s f -> p (s f)"), sigf, AF.Ln, [s_sig], [s_logf[gp]])
            TS("pool", kk[gp][0:R2].rearrange("p s f -> p (s f)"), sigf, -1.0, 1.0, ALU.mult, ALU.add, [s_sig], [s_kk[gp]])
            for s_ in range(GS):
                sp = step % 2
                step += 1
                lf = logf[gp][0:R2, s_, :]
                pe1, s_pe1 = ps()
                MM(pe1[0:R2], bm[0:R2, 0, :], lf, True, True, [s_c, s_logf[gp]], [s_pe1])
                ACT(ek[sp][0:R2], pe1[0:R2], AF.Exp, [s_pe1], [s_ek[sp]])
                TT("dve", ktl[sp][0:R2], kk[gp][0:R2, s_, :], ek[sp][0:R2], ALU.mult, [s_kk[gp], s_ek[sp]], [s_kt[sp]])
                pe2, s_pe2 = ps()
                MM(pe2[0:R2], bm[0:R2, 1, :], lf, True, True, [s_c, s_logf[gp]], [s_pe2])
                ACT(e2[sp][0:R2], pe2[0:R2], AF.Exp, [s_pe2], [s_e2[sp]])
                TT("dve", e2[sp][0:R2], kk[gp][0:R2, s_, :], e2[sp][0:R2], ALU.mult, [s_kk[gp], s_e2[sp]], [s_e2[sp]])
                for d_ in range(2):
                    ACT(kh[sp][0:R2, d_, :], e2[sp][0:R2], AF.Identity, [s_e2[sp], s_c], [s_kh[sp]], scale=rmk[0:R2, d_:d_ + 1])
                pt, s_pt = ps()
                ptb = pt.bitcast(BF16)
                for hh in range(8):
                    TR(ptb[0:64, hh * R2:(hh + 1) * R2], ktl[sp][0:R2, hh * 64:(hh + 1) * 64], identb[0:R2, 0:R2], [s_kt[sp], s_const], [s_pt])
                CP("act", ktT[sp][0:64], ptb[0:64, 0:8 * R2].rearrange("p (c t) -> p c t", t=R2), [s_pt], [s_ktT[sp]])
                pbT, s_pbT = ps()
                for hh in range(8):
                    MM(pbT[0:64, hh * R2:(hh + 1) * R2], lf[:, hh * 64:(hh + 1) * 64], bm[0:R2, 2, :], True, True, [s_c, s_logf[gp]], [s_pbT])
                ACT(eq[sp][0:64], pbT[0:64, 0:8 * R2].rearrange("p (c t) -> p c t", t=R2), AF.Exp, [s_pbT], [s_eq[sp]])
                pbm, s_pbm = ps()
                for hh in range(8):
                    MM(pbm[0:64, hh * R2:(hh + 1) * R2], lf[:, hh * 64:(hh + 1) * 64], bm[0:R2, 3, :], True, True, [s_c, s_logf[gp]], [s_pbm])
                ACT(eqm[sp][0:64], pbm[0:64, 0:8 * R2].rearrange("p (c t) -> p c t", t=R2), AF.Exp, [s_pbm], [s_eqm[sp]])
                qs = q2[gp][0:64, :, s_, :]
                TT("dve", qebf[sp][0:64, :, 0:C], qs[:, :, 0:C], eq[sp][0:64, :, 0:C], ALU.mult, [s_q2[gp], s_eq[sp]], [s_qe[sp]])
                TT("dve", qebb[sp][0:64, :, C:R2], qs[:, :, C:R2], eq[sp][0:64, :, C:R2], ALU.mult, [s_q2[gp], s_eq[sp]], [s_qe[sp]])
                TT("pool", qtl[sp][0:64], qs, eqm[sp][0:64], ALU.mult, [s_q2[gp], s_eqm[sp]], [s_qt[sp]])
                pa, s_pa = ps()
                for hh in range(8):
                    MM(pa[0:R2, hh * R2:(hh + 1) * R2], ktT[sp][0:64, hh, :], qtl[sp][0:64, hh, :], True, True, [s_ktT[sp], s_qt[sp]], [s_pa])
                TT("dve", attT[sp][0:R2], pa[0:R2, 0:8 * R2].rearrange("p (h t) -> p h t", t=R2), bmk[0:R2], ALU.mult,
                   [s_pa, s_c], [s_att[sp]])
                po, s_po = psacc(accb)
                for hh in range(8):
                    osl = po[0:R2, hh * 64:(hh + 1) * 64]
                    MM(osl, attT[sp][0:R2, hh, :], v2[gp][0:R2, s_, hh * 64:(hh + 1) * 64], True, False, [s_att[sp], s_v[gp]], [s_po])
                    MM(osl, qebf[sp][0:64, hh, :], Sb[0:64, 0, hh, :], False, False, [s_qe[sp], s_Sb], [s_po])
                    MM(osl, qebb[sp][0:64, hh, :], Sb[0:64, 1, hh, :], False, True, [s_qe[sp], s_Sb], [s_po])
                CP("act", o_s[gp][0:R2, s_, :], po[0:R2], [s_po], [s_os[gp]])
                for d_ in range(2):
                    pS, s_pS = ps()
                    for hh in range(8):
                        MM(pS[0:64, hh * 64:(hh + 1) * 64], kh[sp][0:R2, d_, hh * 64:(hh + 1) * 64], v2[gp][0:R2, s_, hh * 64:(hh + 1) * 64], True, True,
                           [s_kh[sp], s_v[gp]], [s_pS])
                    Sv = Sst[0:64, d_]
                    TT("dve", Sv, Sv, eq[sp][0:64, :, C - 1 + d_:C + d_].broadcast_to([64, 8, 64]), ALU.mult, [s_S, s_eq[sp]], [s_S])
                    TT("dve", Sv, Sv, pS[0:64].rearrange("p (h x) -> p h x", x=64), ALU.add, [s_S, s_pS], [s_S])
                CP("act", Sb[0:64], Sst[0:64], [s_S], [s_Sb])
                yield 1
            P.dma(ofb[0, fsl, :].rearrange("(s p) f -> p s f", p=C), o_s[gp][0:C], reads=[s_os[gp]], q=ST)
            for s_ in range(GS):
                cb = cb0 + GS - 1 - s_
                P.dma(ofb[1, cb * C:(cb + 1) * C, :], o_s[gp][C:R2, s_, :], reads=[s_os[gp]], q=ST)

    def phase_Bfin(l):
        AR.reset(persist_mark)
        ST = "pool"

        def dbl(shape, dt, n=2):
            return [AR.alloc(shape, dt) for _ in range(n)]
        gon = AR.alloc([512], F32)
        s_g = P.slot("B_gon")
        P.dma(gon, b_onorm[l:l + 1, :].broadcast_to([128, 512]), writes=[s_g])
        of_ = dbl([512], BF16); ob_ = dbl([512], BF16); sg_ = dbl([512], BF16)
        s_in = P.slots(2, "Bf_in")
        osum = dbl([512], F32); osq = dbl([512], F32); st = dbl([8], F32); on_ = dbl([512], BF16); oTs = dbl([4, 128], BF16)
        s_w2 = P.slots(2, "Bf_w"); s_oTs = P.slots(2, "Bf_oT")
        for j in range(NT):
            p_ = j % 2
            tsl = slice(j * 128, (j + 1) * 128)
            P.dma(of_[p_], ofb[0, tsl, :], writes=[s_in[p_]], q=LD)
            P.dma(ob_[p_], ofb[1, tsl, :], writes=[s_in[p_]], q=LD)
            P.dma(sg_[p_], sgb[tsl, :], writes=[s_in[p_]], q=LD)
            TT("dve", osum[p_], of_[p_], ob_[p_], ALU.add, [s_in[p_]], [s_w2[p_]])
            ACT(osq[p_], osum[p_], AF.Square, [s_w2[p_]], [s_w2[p_]], scale=0.125)
            h.reduce(st[p_], osq[p_].rearrange("p (h d) -> p h d", d=64), ALU.add, [s_w2[p_]], [s_w2[p_]])
            TS("dve", st[p_], st[p_], EPS, None, ALU.add, None, [s_w2[p_]], [s_w2[p_]])
            ACT(st[p_], st[p_], AF.Ln, [s_w2[p_]], [s_w2[p_]])
            ACT(st[p_], st[p_], AF.Exp, [s_w2[p_]], [s_w2[p_]], scale=-0.5)
            o3 = osum[p_].rearrange("p (h d) -> p h d", d=64)
            TT("dve", o3, o3, st[p_].unsqueeze(2).broadcast_to([128, 8, 64]), ALU.mult, [s_w2[p_]], [s_w2[p_]])
            TT("pool", osum[p_], osum[p_], gon, ALU.mult, [s_w2[p_], s_g], [s_w2[p_]])
            TT("pool", on_[p_], osum[p_], sg_[p_], ALU.mult, [s_w2[p_], s_in[p_]], [s_w2[p_]])
            pt, s_pt = ps()
            ptb = pt.bitcast(BF16)
            for c in range(4):
                TR(ptb[:, c * 128:(c + 1) * 128], on_[p_][:, c * 128:(c + 1) * 128], identb, [s_w2[p_], s_const], [s_pt])
            CP("act", oTs[p_], ptb[:, 0:512].rearrange("p (c t) -> p c t", t=128), [s_pt], [s_oTs[p_]])
            P.dma(oT[1, :, tsl].rearrange("(c p) t -> p c t", p=128), oTs[p_], reads=[s_oTs[p_]], q=ST)

    def phase_merge(l, xsrc, with_ctx):
        AR.reset(persist_mark)
        ST = "pool"
        wbr = AR.alloc([3, 4, 1024], BF16)
        wout = AR.alloc([8, 1024], BF16)
        wr = AR.alloc([8, 36], F32)
        gt1 = AR.alloc([2, 1024], F32)
        s_wm = P.slot("M_w")
        for n_ in range(3):
            P.dma(wbr[:, n_], wb_br[l, n_].rearrange("(k p) n -> p k n", p=128), writes=[s_wm])
        P.dma(wout, wb_out[l].rearrange("(k p) n -> p k n", p=128), writes=[s_wm])
        P.dma(wr, w_r[l].rearrange("(k p) n -> p k n", p=128), writes=[s_wm])
        for j_ in range(2):
            P.dma(gt1[:, j_, :], gtd[l, 0, j_:j_ + 1, :].broadcast_to([128, 1024]), writes=[s_wm])
        oT3 = [AR.alloc([3, 4, 512], BF16) for _ in range(2)]
        gT3 = [AR.alloc([24, 512], BF16) for _ in range(2)]
        s_in = P.slots(2, "M_in")
        yT = AR.alloc([8, 512], BF16)
        s_yT = P.slot("M_yT")
        acc = [AR.alloc([512], F32) for _ in range(2)]
        tm1 = [AR.alloc([512], F32) for _ in range(2)]
        s_acc = P.slots(2, "M_acc")
        xt = [AR.alloc([1, 1024], F32) for _ in range(2)]
        xnew = [AR.alloc([1, 1024], F32) for _ in range(2)]
        s_x = P.slots(2, "M_x")
        s_xn = P.slots(2, "M_xn")
        h2f = AR.alloc([8, 128], F32)
        h2b = [AR.alloc([8, 512], BF16) for _ in range(2)]
        _sh2 = P.slot("M_h2")
        s_h2 = [_sh2, _sh2]
        s_h2f = P.slot("M_h2f")
        tmp = dict(ms=AR.alloc([8], F32), junk=AR.alloc([1024], F32), xnf=AR.alloc([1024], F32), t2f=AR.alloc([4, 128], F32))
        s_tmp = P.slot("M_tmp")
        R = {k: AR.alloc([n_], F32) for k, n_ in dict(lg=36, oh=4, ge=4, esel=8, es2=8, eq1=8, eq2=8, sc=8, csel=8).items()}
        cmb = [AR.alloc([32], F32) for _ in range(2)]
        s_R = P.slot("M_R")
        s_cmb = P.slots(2, "M_cmb")
        tix = 0
        for bi_, (t0, n, isctx) in enumerate(blocks(include_ctx=with_ctx)):
            bp = bi_ % 2
            nt = n // 128
            jj = 1 if isctx else 0
            bsl = slice(t0, t0 + n)
            for br in range(3):
                P.dma(oT3[bp][:, br, :, 0:n], oT[br, :, bsl].rearrange("(k p) t -> p k t", p=128), writes=[s_in[bp]], q=LD)
            P.dma(gT3[bp][:, :, 0:n], gT[:, bsl].rearrange("(c p) t -> p c t", p=128), writes=[s_in[bp]], q=LD)
            for m in range(8):
                pbs = []
                for br in range(3):
                    pb, s_pb = ps()
                    for k in range(4):
                        MM(pb[:, 0:n], wbr[:, br, k, m * 128:(m + 1) * 128], oT3[bp][:, br, k, 0:n], k == 0, k == 3, [s_wm, s_in[bp]], [s_pb])
                    pbs.append((pb, s_pb))
                ap_ = m % 2
                TT("dve", acc[ap_][:, 0:n], pbs[0][0][:, 0:n], gT3[bp][:, m, 0:n], ALU.mult, [pbs[0][1], s_in[bp]], [s_acc[ap_]])
                TT("dve", tm1[ap_][:, 0:n], pbs[1][0][:, 0:n], gT3[bp][:, 8 + m, 0:n], ALU.mult, [pbs[1][1], s_in[bp]], [s_acc[ap_]])
                TT("pool", acc[ap_][:, 0:n], acc[ap_][:, 0:n], tm1[ap_][:, 0:n], ALU.add, [s_acc[ap_]], [s_acc[ap_]])
                TT("dve", tm1[ap_][:, 0:n], pbs[2][0][:, 0:n], gT3[bp][:, 16 + m, 0:n], ALU.mult, [pbs[2][1], s_in[bp], s_acc[ap_]], [s_acc[ap_]])
                TT("pool", yT[:, m, 0:n], acc[ap_][:, 0:n], tm1[ap_][:, 0:n], ALU.add, [s_acc[ap_]], [s_yT])
            for j in range(nt):
                par = tix % 2
                tix += 1
                tsl = slice(t0 + j * 128, t0 + (j + 1) * 128)
                P.dma(xt[par][:, 0, :], xsrc[tsl, :], writes=[s_x[par]], q=LD)
                for hf in range(2):
                    pz, s_pz = ps()
                    for k in range(8):
                        MM(pz, yT[:, k, j * 128:(j + 1) * 128], wout[:, k, hf * 512:(hf + 1) * 512], k == 0, k == 7, [s_yT, s_wm], [s_pz])
                    TT("dve", xnew[par][:, 0, hf * 512:(hf + 1) * 512], pz, gt1[:, jj, hf * 512:(hf + 1) * 512], ALU.mult, [s_pz, s_wm], [s_xn[par]])
                TT("pool", xnew[par][:, 0, :], xnew[par][:, 0, :], xt[par][:, 0, :], ALU.add, [s_xn[par], s_x[par]], [s_xn[par]])
                P.dma(xs2[tsl, :], xnew[par][:, 0, :], reads=[s_xn[par]], q=ST)
                norm_mod_T(xnew[par], s_xn[par], 1, l, 1, lambda j_: jj, h2b[bp], s_h2[bp], tmp, s_tmp, hTf=h2f, tok0=j * 128)
                pr_, s_pr = ps()
                for k in range(8):
                    MM(pr_[:, 0:36], h2f[:, k, :], wr[:, k, :], k == 0, k == 7, [s_h2[bp], s_wm], [s_pr])
                lg = R["lg"]
                CP("dve", lg, pr_[:, 0:36], [s_pr], [s_R])
                sc_ = R["sc"]
                rs = [s_R]
                h.reduce(sc_[:, 0:1], lg[:, 0:4], ALU.max, rs, rs)
                TS("dve", R["oh"], lg[:, 0:4], sc_[:, 0:1], None, ALU.is_equal, None, rs, rs)
                TS("dve", sc_[:, 1:2], sc_[:, 0:1], -1.0, None, ALU.mult, None, rs, rs)
                ACT(R["ge"], lg[:, 0:4], AF.Exp, rs, rs, bias=sc_[:, 1:2], accum_out=sc_[:, 2:3])
                P.dve(lambda e, o_=sc_[:, 2:3]: e.reciprocal(out=o_, in_=o_), rs, rs)
                el = lg[:, 4:36].rearrange("p (g e) -> p g e", e=8)
                TS("dve", R["esel"], el[:, 0, :], R["oh"][:, 0:1], None, ALU.mult, None, rs, rs)
                for g_ in range(1, 4):
                    h.stt("dve", R["esel"], el[:, g_, :], R["oh"][:, g_:g_ + 1], R["esel"], ALU.mult, ALU.add, rs, rs)
                h.reduce(sc_[:, 3:4], R["esel"], ALU.max, rs, rs)
                TS("dve", R["eq1"], R["esel"], sc_[:, 3:4], None, ALU.is_equal, None, rs, rs)
                h.stt("dve", R["es2"], R["eq1"], -1e30, R["esel"], ALU.mult, ALU.add, rs, rs)
                h.reduce(sc_[:, 4:5], R["es2"], ALU.max, rs, rs)
                TS("dve", R["eq2"], R["es2"], sc_[:, 4:5], None, ALU.is_equal, None, rs, rs)
                TS("dve", sc_[:, 5:6], sc_[:, 3:4], -1.0, None, ALU.mult, None, rs, rs)
                ACT(sc_[:, 6:7], sc_[:, 4:5], AF.Exp, rs, rs, bias=sc_[:, 5:6])
                TS("dve", sc_[:, 7:8], sc_[:, 6:7], 1.0, None, ALU.add, None, rs, rs)
                P.dve(lambda e, o_=sc_[:, 7:8]: e.reciprocal(out=o_, in_=o_), rs, rs)
                TT("dve", sc_[:, 7:8], sc_[:, 7:8], sc_[:, 2:3], ALU.mult, rs, rs)
                TT("dve", sc_[:, 6:7], sc_[:, 6:7], sc_[:, 7:8], ALU.mult, rs, rs)
                TS("dve", R["csel"], R["eq1"], sc_[:, 7:8], None, ALU.mult, None, rs, rs)
                h.stt("dve", R["csel"], R["eq2"], sc_[:, 6:7], R["csel"], ALU.mult, ALU.add, rs, rs)
                c3 = cmb[par].rearrange("p (g e) -> p g e", e=8)
                for g_ in range(4):
                    TS("dve", c3[:, g_, :], R["csel"], R["oh"][:, g_:g_ + 1], None, ALU.mult, None, rs, [s_cmb[par]])
                P.dma(cmbd[tsl, :], cmb[par], reads=[s_cmb[par]], q=ST)
            P.dma(h2T[:, bsl].rearrange("(k p) t -> p k t", p=128), h2b[bp][:, :, 0:n], reads=[s_h2[bp]], q=ST)

    def phase_moe(l, with_ctx, last):
        AR.reset(persist_mark)
        BS = 2048
        gt2 = AR.alloc([2, 1024], F32)
        gfin = AR.alloc([1024], F32)
        s_c = P.slot("E_c")
        for j_ in range(2):
            P.dma(gt2[:, j_, :], gtd[l, 1, j_:j_ + 1, :].broadcast_to([128, 1024]), writes=[s_c])
        P.dma(gfin, g_final.unsqueeze(0).broadcast_to([128, 1024]), writes=[s_c])
        hb = AR.alloc([8, BS], BF16)
        cm = AR.alloc([BS // 128, 32], F32)
        yacc = AR.alloc([BS // 128, 1024], F32)
        s_hb = P.slot("E_hb"); s_y = P.slots(BS // 128, "E_y")
        w1e = [AR.alloc([8, 256], BF16) for _ in range(2)]
        w3e = [AR.alloc([8, 256], BF16) for _ in range(2)]
        w2e = [AR.alloc([2, 1024], BF16) for _ in range(2)]
        s_we = P.slots(2, "E_w")
        su = [AR.alloc([512], F32) for _ in range(2)]
        aT = [AR.alloc([2, 512], BF16) for _ in range(2)]
        s_su = P.slots(2, "E_su"); s_aT = P.slots(2, "E_aT")
        xt = [AR.alloc([1024], F32) for _ in range(2)]
        xo = [AR.alloc([1024], F32) for _ in range(2)]
        st = [AR.alloc([2], F32) for _ in range(2)]
        junk = AR.alloc([1024], F32)
        s_x = P.slots(2, "E_x"); s_xo = P.slots(2, "E_xo")
        for (t0, n, isctx) in blocks(include_ctx=with_ctx, bs=BS):
            nt = n // 128
            jj = 1 if isctx else 0
            bsl = slice(t0, t0 + n)
            P.dma(hb[:, :, 0:n], h2T[:, bsl].rearrange("(k p) t -> p k t", p=128), writes=[s_hb], q=LD)
            P.dma(cm[:, 0:nt, :], cmbd[bsl, :].rearrange("(j p) e -> p j e", p=128), writes=[s_hb], q=LD)
            for j in range(nt):
                h.memset("pool", yacc[:, j, :], 0.0, [s_y[j]])
            items = [(e_, sb0, min(512, n - sb0)) for e_ in range(NEXP) for sb0 in range(0, n, 512)]
            loaded = set()

            def uv(ii):
                e_, sb0, nn = items[ii]
                ep = e_ % 2
                ap_ = ii % 2
                if e_ not in loaded:
                    loaded.add(e_)
                    P.dma(w1e[ep], wb1[l, e_].rearrange("(k p) n -> p k n", p=128), writes=[s_we[ep]], q=LD)
                    P.dma(w3e[ep], wb3[l, e_].rearrange("(k p) n -> p k n", p=128), writes=[s_we[ep]], q=LD)
                    P.dma(w2e[ep], wb2[l, e_].rearrange("(k p) n -> p k n", p=128), writes=[s_we[ep]], q=LD)
                for cc in range(2):
                    pu, s_pu = ps()
                    for k in range(8):
                        MM(pu[:, 0:nn], w1e[ep][:, k, cc * 128:(cc + 1) * 128], hb[:, k, sb0:sb0 + nn], k == 0, k == 7, [s_we[ep], s_hb], [s_pu])
                    pv, s_pv = ps()
                    for k in range(8):
                        MM(pv[:, 0:nn], w3e[ep][:, k, cc * 128:(cc + 1) * 128], hb[:, k, sb0:sb0 + nn], k == 0, k == 7, [s_we[ep], s_hb], [s_pv])
                    ACT(su[cc][:, 0:nn], pu[:, 0:nn], AF.Silu, [s_pu], [s_su[cc]])
                    TT("dve", aT[ap_][:, cc, 0:nn], su[cc][:, 0:nn], pv[:, 0:nn], ALU.mult, [s_su[cc], s_pv], [s_aT[ap_]])

            def yy(ii):
                e_, sb0, nn = items[ii]
                ep = e_ % 2
                ap_ = ii % 2
                for j in range(nn // 128):
                    tj = sb0 // 128 + j
                    for hf in range(2):
                        py, s_py = ps()
                        for cc in range(2):
                            MM(py, aT[ap_][:, cc, j * 128:(j + 1) * 128], w2e[ep][:, cc, hf * 512:(hf + 1) * 512], cc == 0, cc == 1,
                               [s_aT[ap_], s_we[ep]], [s_py])
                        ysl = yacc[:, tj, hf * 512:(hf + 1) * 512]
                        h.stt("dve", ysl, py, cm[:, tj, e_:e_ + 1], ysl, ALU.mult, ALU.add, [s_py, s_hb, s_y[tj]], [s_y[tj]])

            uv(0)
            for ii in range(len(items)):
                if ii + 1 < len(items):
                    uv(ii + 1)
                yy(ii)
            for j in range(nt):
                p_ = j % 2
                tsl = slice(t0 + j * 128, t0 + (j + 1) * 128)
                P.dma(xt[p_], xs2[tsl, :], writes=[s_x[p_]], q=LD)
                TT("dve", xo[p_], yacc[:, j, :], gt2[:, jj, :], ALU.mult, [s_y[j], s_c], [s_xo[p_]])
                TT("pool", xo[p_], xo[p_], xt[p_], ALU.add, [s_xo[p_], s_x[p_]], [s_xo[p_]])
                if not last:
                    P.dma(xs[tsl, :], xo[p_], reads=[s_xo[p_]], q=ST)
                elif not isctx:
                    ACT(junk, xo[p_], AF.Square, [s_xo[p_]], [s_xo[p_]], scale=1.0 / 32.0, accum_out=st[p_][:, 0:1])
                    TS("dve", st[p_][:, 0:1], st[p_][:, 0:1], EPS, None, ALU.add, None, [s_xo[p_]], [s_xo[p_]])
                    ACT(st[p_][:, 0:1], st[p_][:, 0:1], AF.Ln, [s_xo[p_]], [s_xo[p_]])
                    ACT(st[p_][:, 0:1], st[p_][:, 0:1], AF.Exp, [s_xo[p_]], [s_xo[p_]], scale=-0.5)
                    h.stt("dve", xo[p_], xo[p_], st[p_][:, 0:1], gfin, ALU.mult, ALU.mult, [s_xo[p_], s_c], [s_xo[p_]])
                    final_ops.append(P.dma(out[t0 - L + j * 128:t0 - L + (j + 1) * 128, :], xo[p_], reads=[s_xo[p_]], q=ST))

    final_ops = []
    P.barrier()

    def on(name):
        return phases is None or name in phases

    for l in range(layers):
        last = (l == layers - 1)
        if on("mod"):
            phase_mod(l)
            P.barrier()
        if on("A"):
            phase_A(l, xin if l == 0 else xs)
            P.barrier()
        if on("attA"):
            phase_attA(l, not last)
            P.barrier()
        if on("attC") and on("B"):
            gB = phase_B(l, accb=1)
            offB = AR.mark()
            nB = T // 32
            nC = 8 * ((NT if True else 0) * (S // 512) + (2 if not last else 0))
            per = max(1, nC // nB)
            gC = phase_attC(l, not last, reset=False, accb=0)
            doneB = doneC = False
            while not (doneB and doneC):
                if not doneB:
                    try:
                        next(gB)
                    except StopIteration:
                        doneB = True
                for _ in range(per):
                    if doneC:
                        break
                    try:
                        next(gC)
                    except StopIteration:
                        doneC = True
            P.barrier()
            phase_Bfin(l)
            P.barrier()
        if on("merge"):
            phase_merge(l, xin if l == 0 else xs, not last)
            P.barrier()
        if on("moe"):
            phase_moe(l, not last, last)
            P.barrier()
    if not final_ops:
        final_ops = [P.barrier()]
    P.emit(final_wait_ops=final_ops)
    return nc, dbg_names


def _rope_tables(S, rot_dim, qscale):
    rows = S // 64
    row = np.repeat(np.arange(rows, dtype=np.float32), 64)
    col = np.tile(np.arange(64, dtype=np.float32), rows)
    n_freq = rot_dim // 4
    inv = (10000.0 ** (-np.arange(n_freq, dtype=np.float32) / n_freq)).astype(np.float32)
    ang = np.concatenate([row[:, None] * inv, col[:, None] * inv], axis=-1).astype(np.float32)
    c, s_ = np.cos(ang).astype(np.float32), np.sin(ang).astype(np.float32)
    T = L + S
    tab = np.zeros((T, 4, rot_dim), np.float32)
    c2 = np.concatenate([c, c], -1)
    s2 = np.concatenate([-s_, s_], -1)
    tab[L:, 0] = c2 * qscale
    tab[L:, 1] = s2 * qscale
    tab[L:, 2] = c2
    tab[L:, 3] = s2
    tab[:L, 0] = qscale
    tab[:L, 2] = 1.0
    return tab


def _consts(S):
    c = {}
    c["ident"] = np.eye(128, dtype=np.float32)
    c["ropeA"] = _rope_tables(S, 64, 64 ** -0.5)
    c["ropeC"] = _rope_tables(S, 32, 96 ** -0.5)
    j = np.arange(128)[:, None]
    i = np.arange(128)[None, :]
    am = np.zeros((128, 2, 128), np.float32)
    am[:, 0] = (j >= i)
    am[:, 1] = (j <= i)
    c["amask"] = am
    CC = 32
    mid = CC // 2 - 1
    s_ = np.arange(CC)[:, None]
    t_ = np.arange(CC)[None, :]
    bm = np.zeros((2 * CC, 4, 2 * CC), np.float32)
    cmk_f = (s_ <= mid).astype(np.float32) - (s_ <= t_)
    cmk_b = (s_ >= CC - 1 - mid).astype(np.float32) - (s_ >= t_)
    bm[:CC, 0, :CC] = cmk_f; bm[CC:, 0, CC:] = cmk_b
    bm[:CC, 1, :CC] = (s_ > t_); bm[CC:, 1, CC:] = (s_ < t_)
    bm[:CC, 2, :CC] = (s_ <= t_); bm[CC:, 2, CC:] = (s_ >= t_)
    bm[:CC, 3, :CC] = -cmk_f; bm[CC:, 3, CC:] = -cmk_b
    c["bmats"] = bm
    mk = np.zeros((2 * CC, 8, 2 * CC), np.float32)
    mk[:CC, :, :CC] = (s_ <= t_)[:, None, :]
    mk[CC:, :, CC:] = (s_ >= t_)[:, None, :]
    c["bmask"] = mk
    rm = np.zeros((2 * CC, 2), np.float32)
    rm[:CC, 0] = 1.0
    rm[CC:, 1] = 1.0
    c["brm"] = rm
    return c


def _fm(v, k):
    return np.ascontiguousarray(np.swapaxes(v.reshape(v.shape[:-1] + (k, 128)), -1, -2))


def make_in_map(inp, b, S):
    f = lambda a: np.ascontiguousarray(np.asarray(a, dtype=np.float32))
    m = {}
    m["xin"] = np.concatenate([f(inp["ctx"])[b], f(inp["x"])[b, :S]], axis=0)
    m["cvec"] = np.ascontiguousarray(np.stack([_fm(f(inp["c"])[b], 8), _fm(f(inp["c_ctx"]), 8)], axis=-1))
    m["w_mod"] = f(inp["w_mod"])
    m["b_modT"] = _fm(f(inp["b_mod"]), 48)
    m["b_mod"] = f(inp["b_mod"])
    m["g1T"] = _fm(f(inp["g_norm1"]), 8)
    m["g2T"] = _fm(f(inp["g_norm2"]), 8)
    m["w_in"] = f(inp["w_in"])
    m["a_sink"] = f(inp["a_sink"])
    m["lb_logits"] = f(inp["b_lb_logits"])
    m["b_onorm"] = f(inp["b_onorm"])
    m["gqT"] = _fm(f(inp["c_qnorm"]), 2)
    m["gkvT"] = _fm(f(inp["c_kvnorm"]), 1)
    m["w_uq"] = f(inp["w_uq"])
    wk = f(inp["w_ukv"]).reshape(2, 128, 8, 128)
    m["w_ukv"] = np.ascontiguousarray(np.concatenate([wk[..., :64].reshape(2, 128, 512), wk[..., 64:].reshape(2, 128, 512)], axis=-1))
    m["w_br"] = f(inp["w_br"])
    m["w_out"] = f(inp["w_out"])
    m["w_r"] = np.ascontiguousarray(np.concatenate([f(inp["w_rg"]), f(inp["w_re"])], axis=-1))
    m["w1"] = f(inp["w1"]); m["w3"] = f(inp["w3"]); m["w2"] = f(inp["w2"])
    m["g_final"] = f(inp["g_final"])
    return m


_CACHE = {}


def kernel(**inputs):
    x = np.asarray(inputs["x"])
    B, S, _ = x.shape
    if S not in _CACHE:
        _CACHE[S] = (build_program(S)[0], _consts(S))
    nc, consts = _CACHE[S]
    in_maps = []
    for b in range(B):
        m = make_in_map(inputs, b, S)
        m.update(consts)
        in_maps.append(m)
    res = run_bass_kernel_spmd(nc, in_maps, core_ids=list(range(B)))
    return np.stack([np.asarray(r["out"], dtype=np.float32) for r in res.results], axis=0)
```

```python
from concourse.bass_utils import run_bass_kernel_spmd
import ml_dtypes

import numpy as np
import concourse.bass as bass
import concourse.mybir as mybir

F32 = mybir.dt.float32
BF16 = mybir.dt.bfloat16
AF = mybir.ActivationFunctionType
ALU = mybir.AluOpType
AX = mybir.AxisListType

COMPUTE = ("pe", "act", "dve", "pool")
NDMA_SEMS = 24
SEM_EPOCH = 30000


class Slot:
    __slots__ = ("name", "writers", "readers", "excl")

    def __init__(self, name):
        self.name = name
        self.excl = False
        self.writers = {}
        self.readers = {}


class Op:
    __slots__ = ("id", "eng", "fn", "deps", "is_dma", "idx", "signal", "queue", "dma_no")

    def __init__(self, id, eng, fn, is_dma, queue):
        self.id = id
        self.eng = eng
        self.fn = fn
        self.deps = []
        self.is_dma = is_dma
        self.queue = queue
        self.signal = False
        self.idx = -1
        self.dma_no = -1


class Prog:
    def __init__(self, nc):
        self.nc = nc
        self.ops = []
        self.queues = {k: [] for k in ("pe", "act", "dve", "pool", "sync")}
        self.nslot = 0
        self.cur_barrier = None
        self.last_barrier_pos = 0

    def barrier(self):
        op = Op(len(self.ops), "sync", lambda e: e.nop(), False, "sync")
        self.ops.append(op)
        q = self.queues["sync"]
        op.idx = len(q)
        q.append(op)
        deps = set()
        if self.cur_barrier is not None:
            deps.add(self.cur_barrier)
        for qn, qq in self.queues.items():
            nd = 0
            got_c = False
            for o in reversed(qq[:-1] if qn == "sync" else qq):
                if o.is_dma:
                    if nd < NDMA_SEMS:
                        deps.add(o.id)
                        nd += 1
                elif not got_c:
                    deps.add(o.id)
                    got_c = True
                if nd >= NDMA_SEMS and got_c:
                    break
        op.deps = sorted(deps)
        self.cur_barrier = op.id
        return op

    def slot(self, name=None):
        self.nslot += 1
        return Slot(name or f"s{self.nslot}")

    def slots(self, n, name=None):
        return [self.slot(f"{name}{i}") for i in range(n)]

    def _add(self, eng, fn, reads, writes, is_dma=False):
        op = Op(len(self.ops), eng, fn, is_dma, eng)
        self.ops.append(op)
        q = self.queues[eng]
        op.idx = len(q)
        q.append(op)
        key = ("dma", op.id) if is_dma else eng
        deps = set()
        xs_ = [s for s in reads if s.excl]
        if xs_:
            writes = list(writes) + xs_
        for s in reads:
            for k, oid in s.writers.items():
                deps.add(oid)
        for s in writes:
            for k, oid in s.writers.items():
                deps.add(oid)
            for k, oid in s.readers.items():
                deps.add(oid)
        deps.discard(op.id)
        if self.cur_barrier is not None:
            deps.add(self.cur_barrier)
        for s in reads:
            s.readers[key] = op.id
        for s in writes:
            s.writers = {key: op.id}
            s.readers = {}
        op.deps = sorted(deps)
        return op

    def pe(self, fn, reads=(), writes=()):
        return self._add("pe", fn, reads, writes)

    def act(self, fn, reads=(), writes=()):
        return self._add("act", fn, reads, writes)

    def dve(self, fn, reads=(), writes=()):
        return self._add("dve", fn, reads, writes)

    def pool(self, fn, reads=(), writes=()):
        return self._add("pool", fn, reads, writes)

    def dma(self, out, in_, reads=(), writes=(), q="sync", **kw):
        return self._add(q, lambda e: e.dma_start(out=out, in_=in_, **kw), reads, writes, is_dma=True)

    def emit(self, final_wait_ops=()):
        nc = self.nc
        ops = self.ops
        for op in ops:
            for d in op.deps:
                dop = ops[d]
                if dop.is_dma:
                    continue
                if dop.eng == "pe" and op.eng == "pe" and not op.is_dma:
                    continue
                dop.signal = True
        for o in final_wait_ops:
            if not o.is_dma:
                o.signal = True
        sems = {}
        sigval = {}
        for qn, q in self.queues.items():
            cnt = 0
            ep = 0
            for op in q:
                if op.is_dma or not op.signal:
                    continue
                if cnt >= SEM_EPOCH:
                    cnt = 0
                    ep += 1
                cnt += 1
                sigval[op.id] = (qn, ep, cnt)
                if (qn, ep) not in sems:
                    sems[(qn, ep)] = nc.alloc_semaphore(f"s_{qn}_{ep}")
        dma_sems = {}
        dma_cnt = {}
        for qn, q in self.queues.items():
            n = 0
            for op in q:
                if op.is_dma:
                    op.dma_no = n
                    n += 1
            dma_cnt[qn] = n
            if n:
                dma_sems[qn] = [nc.alloc_semaphore(f"d_{qn}_{i}") for i in range(min(n, NDMA_SEMS))]
        dma_ops = {qn: [op for op in q if op.is_dma] for qn, q in self.queues.items()}

        def dma_sem_val(op):
            return dma_sems[op.queue][op.dma_no % NDMA_SEMS], 16 * (op.dma_no // NDMA_SEMS + 1)

        snaps = [None] * len(ops)
        self.nwaits = 0

        def run_queue(qn, eng):
            q = self.queues[qn]
            clock = {}
            known_dma = set()

            def need(d):
                dop = ops[d]
                if dop.is_dma:
                    if d in known_dma:
                        return
                    s, v = dma_sem_val(dop)
                    eng.wait_ge(s, v)
                    self.nwaits += 1
                    known_dma.add(d)
                    return
                if clock.get(dop.eng, -1) >= dop.idx:
                    return
                _, ep, cnt = sigval[d]
                eng.wait_ge(sems[(dop.eng, ep)], cnt)
                self.nwaits += 1
                clock[dop.eng] = dop.idx
                sn = snaps[d]
                if sn:
                    for k, v in sn.items():
                        if clock.get(k, -1) < v:
                            clock[k] = v

            for op in q:
                for d in op.deps:
                    dop = ops[d]
                    if (not dop.is_dma) and dop.eng == "pe" and qn == "pe" and not op.is_dma:
                        continue
                    need(d)
                if op.is_dma and op.dma_no >= NDMA_SEMS:
                    need(dma_ops[qn][op.dma_no - NDMA_SEMS].id)
                snaps[op.id] = dict(clock)
                ins = op.fn(eng)
                if op.is_dma:
                    s, v = dma_sem_val(op)
                    ins.then_inc(s, 16)
                elif op.signal:
                    _, ep, cnt = sigval[op.id]
                    ins.then_inc(sems[(qn, ep)], 1)
            if qn == "sync":
                for o in final_wait_ops:
                    need(o.id)

        self._dry_snapshots(ops, sigval, snaps)

        with nc.Block() as block:
            @block.tensor
            def _(e):
                run_queue("pe", e)

            @block.scalar
            def _(e):
                run_queue("act", e)

            @block.vector
            def _(e):
                run_queue("dve", e)

            @block.gpsimd
            def _(e):
                run_queue("pool", e)

            @block.sync
            def _(e):
                run_queue("sync", e)

    def _dry_snapshots(self, ops, sigval, snaps):
        clocks = {qn: {} for qn in self.queues}
        for op in ops:
            clock = clocks[op.queue]
            for d in op.deps:
                dop = ops[d]
                if dop.is_dma:
                    continue
                if dop.eng == "pe" and op.queue == "pe" and not op.is_dma:
                    continue
                if clock.get(dop.eng, -1) >= dop.idx:
                    continue
                clock[dop.eng] = dop.idx
                sn = snaps[d]
                if sn:
                    for k, v in sn.items():
                        if clock.get(k, -1) < v:
                            clock[k] = v
            snaps[op.id] = dict(clock)


class Arena:
    def __init__(self, nc, nbytes, name="arena"):
        self.n = nbytes // 4
        self.t = nc.alloc_sbuf_tensor(name, [128, self.n], F32)
        self.off = 0
        self.peak = 0

    def reset(self, to=0):
        self.off = to

    def mark(self):
        return self.off

    def alloc(self, free_shape, dtype, parts=128):
        esz = 2 if dtype == BF16 else 4
        nel = int(np.prod(free_shape))
        nw = (nel * esz + 3) // 4
        nw = (nw + 7) // 8 * 8
        assert self.off + nw <= self.n, f"arena overflow {self.off}+{nw}>{self.n}"
        ap = self.t[0:parts, self.off:self.off + nw]
        self.off += nw
        self.peak = max(self.peak, self.off)
        if dtype != F32:
            ap = ap.bitcast(dtype)
        ap = ap[:, 0:nel]
        if len(free_shape) >= 2:
            names = [f"a{i}" for i in range(len(free_shape))]
            kw = {nm: int(v) for nm, v in zip(names[1:], free_shape[1:])}
            ap = ap.rearrange("p (" + " ".join(names) + ") -> p " + " ".join(names), **kw)
        return ap

U32 = mybir.dt.uint32
D = 1024
L = 256
EPS = 1e-6
O_AQ, O_AK, O_AV, O_BQ, O_BI, O_BZF, O_BZB, O_BG, O_CQ, O_CKV, O_CKR, O_GL = (
    0, 512, 640, 768, 1280, 1792, 2304, 2816, 3328, 3584, 3712, 3744)
IN_W = 6816
NEXP = 32


class H:
    def __init__(self, P):
        self.P = P

    def mm(self, out, lhsT, rhs, start, stop, reads, writes, tp=None):
        if tp is None:
            self.P.pe(lambda e: e.matmul(out, lhsT=lhsT, rhs=rhs, start=start, stop=stop), reads, writes)
        else:
            self.P.pe(lambda e: e.matmul(out, lhsT=lhsT, rhs=rhs, start=start, stop=stop, tile_position=tp), reads, writes)

    def tr(self, out, in_, ident, reads, writes):
        self.P.pe(lambda e: e.transpose(out=out, in_=in_, identity=ident), reads, writes)

    def act(self, out, in_, func, reads, writes, **kw):
        self.P.act(lambda e: e.activation(out=out, in_=in_, func=func, **kw), reads, writes)

    def tt(self, eng, out, in0, in1, op, reads, writes):
        self.P._add(eng, lambda e: e.tensor_tensor(out=out, in0=in0, in1=in1, op=op), reads, writes)

    def ts(self, eng, out, in0, s1, s2, op0, op1, reads, writes, **kw):
        if s2 is None:
            self.P._add(eng, lambda e: e.tensor_scalar(out=out, in0=in0, scalar1=s1, scalar2=None, op0=op0, **kw), reads, writes)
        else:
            self.P._add(eng, lambda e: e.tensor_scalar(out=out, in0=in0, scalar1=s1, scalar2=s2, op0=op0, op1=op1, **kw), reads, writes)

    def stt(self, eng, out, in0, scalar, in1, op0, op1, reads, writes):
        self.P._add(eng, lambda e: e.scalar_tensor_tensor(out=out, in0=in0, scalar=scalar, in1=in1, op0=op0, op1=op1), reads, writes)

    def cp(self, eng, out, in_, reads, writes):
        if eng == "act":
            self.P.act(lambda e: e.activation(out=out, in_=in_, func=AF.Identity), reads, writes)
        else:
            self.P._add(eng, lambda e: e.tensor_copy(out=out, in_=in_), reads, writes)

    def memset(self, eng, ap, val, writes):
        self.P._add(eng, lambda e: e.memset(ap, val), (), writes)

    def reduce(self, out, in_, op, reads, writes, axis=AX.X):
        self.P.dve(lambda e: e.tensor_reduce(out=out, in_=in_, axis=axis, op=op), reads, writes)


def build_program(S, dbg=False, layers=2, phases=None):
    T = L + S
    NT = T // 128
    nc = bass.Bass("TRN2", target_bir_lowering=False)
    P = Prog(nc)
    h = H(P)
    MM, TR, ACT, TT, TS, CP = h.mm, h.tr, h.act, h.tt, h.ts, h.cp
    LD = "sync"
    ST = "sync"

    def din(name, shape, dt=F32):
        return nc.dram_tensor(name, list(shape), dt, kind="ExternalInput").ap()

    dbg_names = []

    def dscr(name, shape, dt=BF16):
        if dbg:
            dbg_names.append(name)
            return nc.dram_tensor(name, list(shape), dt, kind="ExternalOutput").ap()
        return nc.dram_tensor(name, list(shape), dt).ap()

    xin = din("xin", [T, D])
    cvec = din("cvec", [128, 8, 2])
    w_mod = din("w_mod", [2, D, 6 * D])
    b_modT = din("b_modT", [2, 128, 48])
    b_mod = din("b_mod", [2, 6 * D])
    g1T = din("g1T", [2, 128, 8])
    g2T = din("g2T", [2, 128, 8])
    w_in = din("w_in", [2, D, IN_W])
    a_sink = din("a_sink", [2, 8])
    lb_logits = din("lb_logits", [2, 2, 512])
    b_onorm = din("b_onorm", [2, 512])
    gqT = din("gqT", [2, 128, 2])
    gkvT = din("gkvT", [2, 128, 1])
    w_uq = din("w_uq", [2, 256, 768])
    w_ukv = din("w_ukv", [2, 128, 1024])
    w_br = din("w_br", [2, 3, 512, D])
    w_out = din("w_out", [2, D, D])
    w_r = din("w_r", [2, D, 36])
    w1 = din("w1", [2, NEXP, D, 256])
    w3 = din("w3", [2, NEXP, D, 256])
    w2 = din("w2", [2, NEXP, 256, D])
    g_final = din("g_final", [D])
    ident_d = din("ident", [128, 128])
    ropeA = din("ropeA", [T, 4, 64])
    ropeC = din("ropeC", [T, 4, 32])
    amask = din("amask", [128, 2, 128])
    bmats = din("bmats", [64, 4, 64])
    bmask = din("bmask", [64, 8, 64], F32)
    brm = din("brm", [64, 2], F32)
    out = nc.dram_tensor("out", [S, D], F32, kind="ExternalOutput").ap()

    xs = dscr("xs", [T, D], F32)
    xs2 = dscr("xs2", [T, D], F32)
    wb_in = dscr("wb_in", [2, D, IN_W])
    wb_uq = dscr("wb_uq", [2, 256, 768])
    wb_ukv = dscr("wb_ukv", [2, 128, 1024])
    wb_br = dscr("wb_br", [2, 3, 512, D])
    wb_out = dscr("wb_out", [2, D, D])
    wb1 = dscr("wb1", [2, NEXP, D, 256])
    wb3 = dscr("wb3", [2, NEXP, D, 256])
    wb2 = dscr("wb2", [2, NEXP, 256, D])
    qaT = dscr("qaT", [512, T])
    kaT = dscr("kaT", [128, T])
    va = dscr("va", [T, 128])
    qbT = dscr("qbT", [512, T])
    ib = dscr("ib", [T, 512])
    zf = dscr("zf", [T, 512])
    zb = dscr("zb", [T, 512])
    sgb = dscr("sgb", [T, 512])
    qcT = dscr("qcT", [8, 96, T])
    kcT = dscr("kcT", [512, T])
    krT = dscr("krT", [32, T])
    vc = dscr("vc", [T, 512])
    gT = dscr("gT", [3072, T])
    oT = dscr("oT", [3, 512, T])
    ofb = dscr("ofb", [2, T, 512])
    h2T = dscr("h2T", [D, T])
    cmbd = dscr("cmbd", [T, 32], F32)
    gtd = dscr("gtd", [2, 2, 2, 1024], F32)

    AR = Arena(nc, 196 * 1024)
    ident = AR.alloc([128], F32)
    identb = AR.alloc([128], BF16)
    onesf = AR.alloc([128], F32)
    modA = AR.alloc([2, 2, 8, 2], F32)
    modB = AR.alloc([2, 2, 8, 2], F32)
    sexp = AR.alloc([2, 8], F32)
    s_const = P.slot("const")
    s_mod = P.slot("mod")
    persist_mark = AR.mark()

    PSB = [nc.alloc_psum_tensor(f"psb{i}", [128, 512], F32)[:, :] for i in range(8)]
    PSS = P.slots(8, "ps")
    for s__ in PSS:
        s__.excl = True
    ps_rr = [0]

    def ps():
        i = 2 + ps_rr[0] % 6
        ps_rr[0] += 1
        return PSB[i], PSS[i]

    acc_rr = [0]

    def psacc():
        i = acc_rr[0] % 2
        acc_rr[0] += 1
        return PSB[i], PSS[i]


    P.dma(ident, ident_d, writes=[s_const])
    CP("dve", identb, ident, [s_const], [s_const])
    h.memset("pool", onesf, 1.0, [s_const])
    P.dma(sexp, a_sink.rearrange("l h -> (l h)").unsqueeze(0).broadcast_to([128, 16]).rearrange("p (l h) -> p l h", h=8), writes=[s_const])
    ACT(sexp, sexp, AF.Exp, [s_const], [s_const])

    s_w = P.slot("wcast")
    if phases is None or "W" in phases:
        for l in range(layers):
            for r in range(8):
                P.dma(wb_in[l, r * 128:(r + 1) * 128, :], w_in[l, r * 128:(r + 1) * 128, :], q="pool")
            P.dma(wb_uq[l], w_uq[l], q="pool")
            P.dma(wb_ukv[l], w_ukv[l], q="pool")
            for n in range(3):
                for r in range(4):
                    P.dma(wb_br[l, n, r * 128:(r + 1) * 128, :], w_br[l, n, r * 128:(r + 1) * 128, :], q="pool")
            for r in range(8):
                P.dma(wb_out[l, r * 128:(r + 1) * 128, :], w_out[l, r * 128:(r + 1) * 128, :], q="pool")
            for e in range(NEXP):
                for r in range(0, 8, 4):
                    P.dma(wb1[l, e, r * 128:(r + 4) * 128, :], w1[l, e, r * 128:(r + 4) * 128, :], q="pool")
                    P.dma(wb3[l, e, r * 128:(r + 4) * 128, :], w3[l, e, r * 128:(r + 4) * 128, :], q="pool")
                P.dma(wb2[l, e], w2[l, e], q="pool")

    def phase_mod(l):
        AR.reset(persist_mark)
        cv = AR.alloc([8, 2], F32)
        scv = AR.alloc([8, 2], F32)
        screp = AR.alloc([8, 2, 128], F32)
        bm = AR.alloc([48], F32)
        g1 = AR.alloc([8], F32)
        g2 = AR.alloc([8], F32)
        modv = AR.alloc([48, 2], F32)
        brow = AR.alloc([2, 1024], F32)
        gst = AR.alloc([2, 1024], F32)
        s_gst = P.slots(2, "gst")
        wm = [AR.alloc([8, 1024], F32) for _ in range(2)]
        s_l = P.slot()
        s_wm = P.slots(2, "wm")
        P.dma(cv, cvec, writes=[s_l])
        P.dma(bm, b_modT[l], writes=[s_l])
        P.dma(g1, g1T[l], writes=[s_l])
        P.dma(g2, g2T[l], writes=[s_l])
        for w_i, c0 in enumerate((2048, 5120)):
            P.dma(brow[:, w_i, :], b_mod[l:l + 1, c0:c0 + 1024].broadcast_to([128, 1024]), writes=[s_l])
        ACT(scv, cv, AF.Silu, [s_l], [s_l])
        for k in range(8):
            for j in range(2):
                TS("dve", screp[:, k, j, :], onesf, scv[:, k, j:j + 1], None, ALU.mult, None, [s_l, s_const], [s_l])
        pmod, s_pmod = psacc()
        for g in range(6):
            b = g % 2
            P.dma(wm[b], w_mod[l, :, g * 1024:(g + 1) * 1024].rearrange("(k p) n -> p k n", p=128), writes=[s_wm[b]])
            for m in range(8):
                for k in range(8):
                    MM(pmod[:, (g * 8 + m) * 2:(g * 8 + m) * 2 + 2], wm[b][:, k, m * 128:(m + 1) * 128], scv[:, k, :], k == 0, k == 7,
                       [s_wm[b], s_l], [s_pmod])
            if g in (2, 5):
                w_i = 0 if g == 2 else 1
                for j in range(2):
                    for hf in range(2):
                        pg, s_pg = ps()
                        for k in range(8):
                            MM(pg, screp[:, k, j, :], wm[b][:, k, hf * 512:(hf + 1) * 512], k == 0, k == 7, [s_wm[b], s_l], [s_pg])
                        TT("dve", gst[:, j, hf * 512:(hf + 1) * 512], pg, brow[:, w_i, hf * 512:(hf + 1) * 512], ALU.add,
                           [s_pg, s_l], [s_gst[j]])
                    P.dma(gtd[l, w_i, j:j + 1, :], gst[0:1, j, :], reads=[s_gst[j]])
        TT("dve", modv, pmod[:, 0:96].rearrange("p (m j) -> p m j", j=2), bm.unsqueeze(2).broadcast_to([128, 48, 2]), ALU.add,
           [s_pmod, s_l], [s_l])
        for w_i, (sh0, sc0, g) in enumerate(((0, 8, g1), (24, 32, g2))):
            TS("dve", modA[:, l, w_i, :, :], modv[:, sc0:sc0 + 8, :], 1.0, None, ALU.add, None, [s_l], [s_mod])
            TT("dve", modA[:, l, w_i, :, :], modA[:, l, w_i, :, :], g.unsqueeze(2).broadcast_to([128, 8, 2]), ALU.mult, [s_l, s_mod], [s_mod])
            CP("dve", modB[:, l, w_i, :, :], modv[:, sh0:sh0 + 8, :], [s_l], [s_mod])

    def norm_mod_T(xt, s_x, ntile, l, which, jfun, hTb, s_hT, tmp, s_tmp, hTf=None, tok0=0):
        ms = tmp["ms"]
        for j in range(ntile):
            ACT(tmp["junk"], xt[:, j, :], AF.Square, [s_x], [s_tmp], scale=1.0 / 32.0, accum_out=ms[:, j:j + 1])
        TS("dve", ms[:, 0:ntile], ms[:, 0:ntile], EPS, None, ALU.add, None, [s_tmp], [s_tmp])
        ACT(ms[:, 0:ntile], ms[:, 0:ntile], AF.Ln, [s_tmp], [s_tmp])
        ACT(ms[:, 0:ntile], ms[:, 0:ntile], AF.Exp, [s_tmp], [s_tmp], scale=-0.5)
        for j in range(ntile):
            jj = jfun(j)
            if hTf is None:
                xn = tmp["xnb"]
                TS("dve", xn, xt[:, j, :], ms[:, j:j + 1], None, ALU.mult, None, [s_x, s_tmp], [s_tmp])
                pt, s_pt = ps()
                ptb = pt.bitcast(BF16)
                for k in range(8):
                    TR(ptb[:, k * 128:(k + 1) * 128], xn[:, k * 128:(k + 1) * 128], identb, [s_tmp, s_const], [s_pt])
                t2 = tmp["t2"]
                TT("dve", t2, ptb.rearrange("p (k t) -> p k t", t=128),
                   modA[:, l, which, :, jj:jj + 1].broadcast_to([128, 8, 128]), ALU.mult, [s_pt, s_mod], [s_tmp])
                TT("pool", hTb[:, :, tok0 + j * 128:tok0 + (j + 1) * 128], t2,
                   modB[:, l, which, :, jj:jj + 1].broadcast_to([128, 8, 128]), ALU.add, [s_tmp, s_mod], [s_hT])
            else:
                xn = tmp["xnf"]
                TS("dve", xn, xt[:, j, :], ms[:, j:j + 1], None, ALU.mult, None, [s_x, s_tmp], [s_tmp])
                for hf in range(2):
                    pt, s_pt = ps()
                    for k in range(4):
                        kk = hf * 4 + k
                        TR(pt[:, k * 128:(k + 1) * 128], xn[:, kk * 128:(kk + 1) * 128], ident, [s_tmp, s_const], [s_pt])
                    t2 = tmp["t2f"]
                    TT("dve", t2, pt.rearrange("p (k t) -> p k t", t=128),
                       modA[:, l, which, hf * 4:hf * 4 + 4, jj:jj + 1].broadcast_to([128, 4, 128]), ALU.mult, [s_pt, s_mod], [s_tmp])
                    TT("pool", hTf[:, hf * 4:hf * 4 + 4, j * 128:(j + 1) * 128], t2,
                       modB[:, l, which, hf * 4:hf * 4 + 4, jj:jj + 1].broadcast_to([128, 4, 128]), ALU.add, [s_tmp, s_mod], [s_hT])
                CP("act", hTb[:, :, tok0 + j * 128:tok0 + (j + 1) * 128], hTf[:, :, j * 128:(j + 1) * 128], [s_hT], [s_hT])

    def blocks(include_ctx=True, bs=512):
        bl = []
        if include_ctx:
            bl.append((0, L, True))
        t = L
        while t < T:
            n = min(bs, T - t)
            bl.append((t, n, False))
            t += n
        return bl


    NTM = O_GL

    def phase_A(l, xsrc):
        AR.reset(persist_mark)
        ST = "pool"
        wsb = AR.alloc([8, NTM], BF16)
        wuq = AR.alloc([2, 768], BF16)
        wukv = AR.alloc([1024], BF16)
        gq = AR.alloc([2], F32)
        gkv = AR.alloc([1], F32)
        s_wsb = P.slot("wsb")
        for k in range(8):
            P.dma(wsb[:, k, :], wb_in[l, k * 128:(k + 1) * 128, 0:NTM], writes=[s_wsb])
        P.dma(wuq, wb_uq[l].rearrange("(k p) n -> p k n", p=128), writes=[s_wsb])
        P.dma(wukv, wb_ukv[l], writes=[s_wsb])
        P.dma(gq, gqT[l], writes=[s_wsb])
        P.dma(gkv, gkvT[l], writes=[s_wsb])

        def dbl(shape, dt):
            return [AR.alloc(shape, dt) for _ in range(2)]
        wg = dbl([8, 512], BF16)
        s_wg = P.slots(2, "wg")
        xt = dbl([1, 1024], F32)
        s_x = P.slots(2, "xt")
        hT = dbl([8, 512], BF16)
        s_hT = P.slots(2, "hT")
        rA = dbl([4, 64], F32)
        rC = dbl([4, 32], F32)
        s_r = P.slots(2, "rope")
        tmp = dict(ms=AR.alloc([8], F32), junk=AR.alloc([1024], F32), xnb=AR.alloc([1024], BF16), t2=AR.alloc([8, 128], BF16))
        tmp_q = AR.alloc([768], F32)
        s_tq = P.slot("tq")
        s_tmp = P.slot("tmpA")
        qa_tm = dbl([512], BF16); ka_tm = dbl([128], BF16)
        ropet = dbl([512], F32); ropeu = dbl([512], F32)
        va_s = dbl([128], BF16)
        ib_s = dbl([512], BF16); zf_s = dbl([512], BF16); zb_s = dbl([512], BF16); sg_s = dbl([512], BF16)
        cst = dbl([8], F32)
        cqn = dbl([256], BF16); ckvn = dbl([128], BF16); kr_tm = dbl([32], BF16)
        qc_tm = dbl([768], BF16)
        vc_s = dbl([512], BF16)
        qaT_s = AR.alloc([4, 512], BF16); kaT_s = AR.alloc([512], BF16); qbT_s = AR.alloc([4, 512], BF16)
        cqnT = AR.alloc([2, 512], BF16); ckvnT = AR.alloc([512], BF16); krT_s = AR.alloc([512], BF16)
        qcT_s = AR.alloc([8, 512], BF16); kcT_s = AR.alloc([4, 512], BF16)
        gT_s = dbl([4, 512], BF16)
        s_ev = {}

        def sl(name, par=0):
            key = (name, par)
            if key not in s_ev:
                s_ev[key] = P.slot(name + str(par))
            return s_ev[key]

        tix = 0
        for bi_, (t0, n, isctx) in enumerate(blocks()):
            bp = bi_ % 2
            nt = n // 128
            jj = 1 if isctx else 0
            rd = [s_hT[bp], s_wsb]
            for j in range(nt):
                par = tix % 2
                tix += 1
                tt0 = t0 + j * 128
                P.dma(xt[par][:, 0, :], xsrc[tt0:tt0 + 128, :], writes=[s_x[par]], q=LD)
                P.dma(rA[par], ropeA[tt0:tt0 + 128], writes=[s_r[par]], q=LD)
                P.dma(rC[par], ropeC[tt0:tt0 + 128], writes=[s_r[par]], q=LD)
                norm_mod_T(xt[par], s_x[par], 1, l, 0, lambda j_: jj, hT[bp], s_hT[bp], tmp, s_tmp, tok0=j * 128)

                def tm_group(c0, ncols):
                    pg, s_pg = ps()
                    for k in range(8):
                        MM(pg[:, 0:ncols], hT[bp][:, k, j * 128:(j + 1) * 128], wsb[:, k, c0:c0 + ncols], k == 0, k == 7, rd, [s_pg])
                    return pg, s_pg

                def rope(dst, src, s_src, nh, hd, tab, a0, dsl, eng2="pool"):
                    half = hd // 2
                    s3 = src.rearrange("p (h d) -> p h d", d=hd)
                    t3 = ropet[par][:, 0:nh * hd].rearrange("p (h d) -> p h d", d=hd)
                    u3 = ropeu[par][:, 0:nh * hd].rearrange("p (h d) -> p h d", d=hd)
                    s_t = sl("ropet", par)
                    TT("dve", t3, s3, tab[:, a0, :].unsqueeze(1).broadcast_to([128, nh, hd]), ALU.mult, [s_src, s_r[par]], [s_t])
                    TT("dve", u3[:, :, 0:half], s3[:, :, half:hd], tab[:, a0 + 1, 0:half].unsqueeze(1).broadcast_to([128, nh, half]),
                       ALU.mult, [s_src, s_r[par]], [s_t])
                    TT("dve", u3[:, :, half:hd], s3[:, :, 0:half], tab[:, a0 + 1, half:hd].unsqueeze(1).broadcast_to([128, nh, half]),
                       ALU.mult, [s_src, s_r[par]], [s_t])
                    TT(eng2, dst, ropet[par][:, 0:nh * hd], ropeu[par][:, 0:nh * hd], ALU.add, [s_t], [dsl])

                tsl = slice(tt0, tt0 + 128)
                import os as _os
                KA = int(_os.environ.get("KA", "9"))
                if KA < 2:
                    continue
                KB = int(_os.environ.get("KB", "15"))
                if KB & 1:
                    pg, s_pg = tm_group(O_AQ, 512)
                    rope(qa_tm[par], pg, s_pg, 8, 64, rA[par], 0, sl("qa_tm", par))
                if KB & 2:
                    pt, s_pt = ps()
                    ptb = pt.bitcast(BF16)
                    for c in range(4):
                        TR(ptb[:, c * 128:(c + 1) * 128], qa_tm[par][:, c * 128:(c + 1) * 128], identb, [sl("qa_tm", par), s_const], [s_pt])
                    CP("act", qaT_s[:, :, j * 128:(j + 1) * 128], ptb[:, 0:512].rearrange("p (c t) -> p c t", t=128), [s_pt], [sl("qaT_s")])
                if KB & 4:
                    pg, s_pg = tm_group(O_AK, 256)
                    rope(ka_tm[par], pg[:, 0:128], s_pg, 2, 64, rA[par], 2, sl("ka_tm", par))
                    KC = int(_os.environ.get("KC", "3"))
                    if KC >= 2:
                        CP("act", va_s[par], pg[:, 128:256], [s_pg], [sl("va_s", par)])
                    if KC >= 3:
                        P.dma(va[tsl, :], va_s[par], reads=[sl("va_s", par)], q=ST)
                if KB & 8:
                    pt, s_pt = ps()
                    ptb = pt.bitcast(BF16)
                    TR(ptb[:, 0:128], ka_tm[par], identb, [sl("ka_tm", par), s_const], [s_pt])
                    CP("act", kaT_s[:, j * 128:(j + 1) * 128], ptb[:, 0:128], [s_pt], [sl("kaT_s")])
                if KA < 3:
                    continue
                for c0, dst, nm, dd in ((O_BI, ib_s, "ib_s", ib), (O_BZF, zf_s, "zf_s", zf), (O_BZB, zb_s, "zb_s", zb)):
                    pg, s_pg = tm_group(c0, 512)
                    CP("dve" if nm != "zb_s" else "act", dst[par], pg, [s_pg], [sl(nm, par)])
                    P.dma(dd[tsl, :], dst[par], reads=[sl(nm, par)], q=ST)
                pg, s_pg = tm_group(O_BG, 512)
                ACT(sg_s[par], pg, AF.Silu, [s_pg], [sl("sg_s", par)])
                P.dma(sgb[tsl, :], sg_s[par], reads=[sl("sg_s", par)], q=ST)
                if KA < 4:
                    continue
                pg, s_pg = tm_group(O_CQ, 416)
                cs = cst[par]
                s_cs = sl("cst", par)
                ACT(tmp["junk"][:, 0:256], pg[:, 0:256], AF.Square, [s_pg], [s_tmp, s_cs], scale=1.0 / 16.0, accum_out=cs[:, 0:1])
                ACT(tmp["junk"][:, 0:128], pg[:, 256:384], AF.Square, [s_pg], [s_tmp, s_cs], scale=float(128 ** -0.5), accum_out=cs[:, 1:2])
                TS("dve", cs[:, 0:2], cs[:, 0:2], EPS, None, ALU.add, None, [s_cs], [s_cs])
                ACT(cs[:, 0:2], cs[:, 0:2], AF.Ln, [s_cs], [s_cs])
                ACT(cs[:, 0:2], cs[:, 0:2], AF.Exp, [s_cs], [s_cs], scale=-0.5)
                TS("dve", cqn[par], pg[:, 0:256], cs[:, 0:1], None, ALU.mult, None, [s_pg, s_cs], [sl("cqn", par)])
                TS("dve", ckvn[par], pg[:, 256:384], cs[:, 1:2], None, ALU.mult, None, [s_pg, s_cs], [sl("ckvn", par)])
                rope(kr_tm[par], pg[:, 384:416], s_pg, 1, 32, rC[par], 2, sl("kr_tm", par))
                pt, s_pt = ps()
                ptb = pt.bitcast(BF16)
                TR(ptb[:, 0:128], cqn[par][:, 0:128], identb, [sl("cqn", par), s_const], [s_pt])
                TR(ptb[:, 128:256], cqn[par][:, 128:256], identb, [sl("cqn", par), s_const], [s_pt])
                TR(ptb[:, 256:384], ckvn[par], identb, [sl("ckvn", par), s_const], [s_pt])
                TR(ptb[0:32, 384:512], kr_tm[par], identb, [sl("kr_tm", par), s_const], [s_pt])
                for c in range(2):
                    ACT(cqnT[:, c, j * 128:(j + 1) * 128], ptb[:, c * 128:(c + 1) * 128], AF.Identity, [s_pt, s_wsb], [sl("cqnT")],
                        scale=gq[:, c:c + 1])
                ACT(ckvnT[:, j * 128:(j + 1) * 128], ptb[:, 256:384], AF.Identity, [s_pt, s_wsb], [sl("ckvnT")], scale=gkv[:, 0:1])
                CP("dve", krT_s[0:32, j * 128:(j + 1) * 128], ptb[0:32, 384:512], [s_pt], [sl("krT_s")])
                if KA < 5:
                    continue
                pq0, s_pq0 = ps()
                pq1, s_pq1 = ps()
                for c in range(2):
                    MM(pq0, cqnT[:, c, j * 128:(j + 1) * 128], wuq[:, c, 0:512], c == 0, c == 1, [sl("cqnT"), s_wsb], [s_pq0])
                for c in range(2):
                    MM(pq1[:, 0:256], cqnT[:, c, j * 128:(j + 1) * 128], wuq[:, c, 512:768], c == 0, c == 1, [sl("cqnT"), s_wsb], [s_pq1])
                qscale = float(96 ** -0.5)
                q3 = qc_tm[par].rearrange("p (h d) -> p h d", d=96)
                s_qc = sl("qc_tm", par)
                CP("act", tmp_q[:, 0:512], pq0, [s_pq0], [s_tq])
                CP("act", tmp_q[:, 512:768], pq1[:, 0:256], [s_pq1], [s_tq])
                tq3 = tmp_q.rearrange("p (h d) -> p h d", d=96)
                TS("dve", q3[:, :, 0:64], tq3[:, :, 0:64], qscale, None, ALU.mult, None, [s_tq], [s_qc])
                t3 = ropet[par][:, 0:256].rearrange("p (h d) -> p h d", d=32)
                u3 = ropeu[par][:, 0:256].rearrange("p (h d) -> p h d", d=32)
                s_t = sl("ropet", par)
                TT("dve", t3, tq3[:, :, 64:96], rC[par][:, 0, :].unsqueeze(1).broadcast_to([128, 8, 32]), ALU.mult, [s_tq, s_r[par]], [s_t])
                TT("dve", u3[:, :, 0:16], tq3[:, :, 80:96], rC[par][:, 1, 0:16].unsqueeze(1).broadcast_to([128, 8, 16]), ALU.mult,
                   [s_tq, s_r[par]], [s_t])
                TT("dve", u3[:, :, 16:32], tq3[:, :, 64:80], rC[par][:, 1, 16:32].unsqueeze(1).broadcast_to([128, 8, 16]), ALU.mult,
                   [s_tq, s_r[par]], [s_t])
                TT("pool", q3[:, :, 64:96], t3, u3, ALU.add, [s_t], [s_qc])
                pt, s_pt = ps()
                ptb = pt.bitcast(BF16)
                for hh in range(8):
                    TR(ptb[0:96, hh * 128:(hh + 1) * 128], qc_tm[par][:, hh * 96:(hh + 1) * 96], identb, [s_qc, s_const], [s_pt])
                CP("act", qcT_s[0:96, :, j * 128:(j + 1) * 128], ptb[0:96, :].rearrange("p (h t) -> p h t", t=128), [s_pt], [sl("qcT_s")])
                pv, s_pv = ps()
                MM(pv, ckvnT[:, j * 128:(j + 1) * 128], wukv[:, 512:1024], True, True, [sl("ckvnT"), s_wsb], [s_pv])
                CP("dve", vc_s[par], pv, [s_pv], [sl("vc_s", par)])
                P.dma(vc[tsl, :], vc_s[par], reads=[sl("vc_s", par)], q=ST)
            bsl = slice(t0, t0 + n)
            if KA < 6:
                continue
            for c in range(4):
                pg, s_pg = ps()
                for k in range(8):
                    MM(pg[:, 0:n], wsb[:, k, O_BQ + c * 128:O_BQ + (c + 1) * 128], hT[bp][:, k, 0:n], k == 0, k == 7, rd, [s_pg])
                CP("act" if c % 2 else "dve", qbT_s[:, c, 0:n], pg[:, 0:n], [s_pg], [sl("qbT_s")])
            for c in range(4):
                pg, s_pg = ps()
                MM(pg[:, 0:n], wukv[:, c * 128:(c + 1) * 128], ckvnT[:, 0:n], True, True, [sl("ckvnT"), s_wsb], [s_pg])
                CP("dve", kcT_s[:, c, 0:n], pg[:, 0:n], [s_pg], [sl("kcT_s")])
            P.dma(qaT[:, bsl].rearrange("(c p) t -> p c t", p=128), qaT_s[:, :, 0:n], reads=[sl("qaT_s")], q=ST)
            P.dma(kaT[:, bsl], kaT_s[:, 0:n], reads=[sl("kaT_s")], q=ST)
            P.dma(qbT[:, bsl].rearrange("(c p) t -> p c t", p=128), qbT_s[:, :, 0:n], reads=[sl("qbT_s")], q=ST)
            P.dma(qcT[:, :, bsl].rearrange("h d t -> d h t"), qcT_s[0:96, :, 0:n], reads=[sl("qcT_s")], q=ST)
            P.dma(kcT[:, bsl].rearrange("(c p) t -> p c t", p=128), kcT_s[:, :, 0:n], reads=[sl("kcT_s")], q=ST)
            P.dma(krT[:, bsl], krT_s[0:32, 0:n], reads=[sl("krT_s")], q=ST)
            for g in range(6):
                gp = g % 2
                P.dma(wg[gp], wb_in[l, :, O_GL + g * 512:O_GL + (g + 1) * 512].rearrange("(k p) n -> p k n", p=128), writes=[s_wg[gp]], q=LD)
                for c in range(4):
                    pg, s_pg = ps()
                    for k in range(8):
                        MM(pg[:, 0:n], wg[gp][:, k, c * 128:(c + 1) * 128], hT[bp][:, k, 0:n], k == 0, k == 7, [s_hT[bp], s_wg[gp]], [s_pg])
                    ACT(gT_s[gp][:, c, 0:n], pg[:, 0:n], AF.Sigmoid, [s_pg], [sl("gT_s", gp)])
                P.dma(gT[g * 512:(g + 1) * 512, bsl].rearrange("(c p) t -> p c t", p=128), gT_s[gp][:, :, 0:n], reads=[sl("gT_s", gp)], q=ST)

    def att_finalize(po, s_po, n, sink_ap, dst_dram, tmpo, rdt, s_fin, eng_dma=ST, nh=1):
        if sink_ap is not None:
            TT("dve", rdt[64:65, 0:n].rearrange("p (g t) -> p g t", g=nh), po[64:65, 0:n].rearrange("p (g t) -> p g t", g=nh),
               sink_ap, ALU.add, [s_po, s_const], [s_fin])
            P.dve(lambda e: e.reciprocal(out=rdt[64:65, 0:n], in_=rdt[64:65, 0:n]), [s_fin], [s_fin])
        else:
            P.dve(lambda e: e.reciprocal(out=rdt[64:65, 0:n], in_=po[64:65, 0:n]), [s_po], [s_fin])
        pb, s_pb = ps()
        MM(pb[0:64, 0:n], onesf[64:65, 0:64], rdt[64:65, 0:n], True, True, [s_fin, s_const], [s_pb])
        CP("act", tmpo[0:64, 0:n], po[0:64, 0:n], [s_po], [s_fin])
        o16 = tmpo[0:64, 512:1024].bitcast(BF16)[:, 0:n]
        TT("dve", o16, tmpo[0:64, 0:n], pb[0:64, 0:n], ALU.mult, [s_fin, s_pb], [s_fin])
        P.dma(dst_dram, o16 if nh == 1 else o16.rearrange("p (g t) -> p g t", g=nh), reads=[s_fin], q=eng_dma)

    def phase_attA(l, with_ctx):
        AR.reset(persist_mark)
        kT = AR.alloc([2, T], BF16)
        vt = AR.alloc([NT, 2, 65], BF16)
        msk = AR.alloc([2, 128], BF16)
        mskf = AR.alloc([2, 128], F32)
        s_kv = P.slot("attA_kv")
        for kvh in range(2):
            P.dma(kT[0:64, kvh, :], kaT[kvh * 64:(kvh + 1) * 64, :], writes=[s_kv])
        h.memset("pool", vt, 1.0, [s_kv])
        for kvh in range(2):
            P.dma(vt[:, :, kvh, 0:64], va[:, kvh * 64:(kvh + 1) * 64].rearrange("(j p) d -> p j d", p=128), writes=[s_kv])
        P.dma(mskf, amask, writes=[s_kv])
        CP("dve", msk, mskf, [s_kv], [s_kv])
        qt = [AR.alloc([8, 128], BF16) for _ in range(2)]
        s_q = P.slots(2, "attA_q")
        pT = [AR.alloc([512], BF16) for _ in range(4)]
        s_pT = P.slots(4, "attA_pT")
        pcnt_ = [0]
        tmpo = [AR.alloc([1024], F32) for _ in range(2)]
        rdt = [AR.alloc([512], F32) for _ in range(2)]
        s_fin = P.slots(2, "attA_fin")
        nlat = S // 128
        qtiles = ([0, 1] if with_ctx else []) + list(range(2, NT))
        cnt = 0
        pcnt = 0
        for qi_, gi in enumerate(qtiles):
            qp = qi_ % 2
            P.dma(qt[qp][0:64], qaT[:, gi * 128:(gi + 1) * 128].rearrange("(h d) t -> d h t", d=64), writes=[s_q[qp]], q=LD)
            if gi < 2:
                keys = [(0, None), (1, None)]
            else:
                nq = gi - 2
                keys = [(0, None), (1, None)]
                if nq >= 1:
                    keys.append((gi - 1, 0))
                keys.append((gi, None))
                if nq + 1 < nlat:
                    keys.append((gi + 1, 1))
            for kvh in range(2):
                po, s_po = psacc()
                LA = 2
                ppl = {}

                def qk(ki, kvh=kvh, qp=qp, keys=keys, ppl=ppl):
                    kt, mk = keys[ki]
                    pss, s_pss = ps()
                    MM(pss, kT[0:64, kvh, kt * 128:(kt + 1) * 128], qt[qp][0:64, kvh * 4:(kvh + 1) * 4, :], True, True,
                       [s_kv, s_q[qp]], [s_pss])
                    pp = pcnt_[0] % 4
                    pcnt_[0] += 1
                    ACT(pT[pp], pss, AF.Exp, [s_pss], [s_pT[pp]])
                    if mk is not None:
                        p3 = pT[pp].rearrange("p (g t) -> p g t", g=4)
                        TT("pool", p3, p3, msk[:, mk, :].unsqueeze(1).broadcast_to([128, 4, 128]), ALU.mult, [s_pT[pp], s_kv], [s_pT[pp]])
                    ppl[ki] = pp
                for ki in range(min(LA, len(keys))):
                    qk(ki)
                for ki, (kt, mk) in enumerate(keys):
                    if ki + LA < len(keys):
                        qk(ki + LA)
                    pp = ppl.pop(ki)
                    MM(po[0:65, :], vt[:, kt, kvh, :], pT[pp], ki == 0, ki == len(keys) - 1, [s_kv, s_pT[pp]], [s_po])
                fp = cnt % 2
                cnt += 1
                att_finalize(po, s_po, 512, sexp[64:65, l, kvh * 4:(kvh + 1) * 4].unsqueeze(2).broadcast_to([1, 4, 128]),
                             oT[0, kvh * 256:(kvh + 1) * 256, gi * 128:(gi + 1) * 128].rearrange("(g d) t -> d g t", d=64),
                             tmpo[fp], rdt[fp], s_fin[fp], nh=4)

    def phase_attC(l, with_ctx):
        AR.reset(persist_mark)
        kT = [AR.alloc([T], BF16) for _ in range(2)]
        vt = [AR.alloc([NT, 65], BF16) for _ in range(2)]
        s_kv = P.slots(2, "attC_kv")
        qt = [AR.alloc([512], BF16) for _ in range(2)]
        s_q = P.slots(2, "attC_q")
        pT = [AR.alloc([512], BF16) for _ in range(5)]
        s_pT = P.slots(5, "attC_pT")
        pcnt_ = [0]
        tmpo = [AR.alloc([1024], F32) for _ in range(2)]
        rdt = [AR.alloc([512], F32) for _ in range(2)]
        s_fin = P.slots(2, "attC_fin")
        qblocks = blocks(include_ctx=with_ctx)
        cnt = 0
        pcnt = 0
        for hh in range(8):
            hp = hh % 2
            P.dma(kT[hp][0:64, :], kcT[hh * 64:(hh + 1) * 64, :], writes=[s_kv[hp]], q=LD)
            P.dma(kT[hp][64:96, :], krT, writes=[s_kv[hp]], q=LD)
            h.memset("pool", vt[hp], 1.0, [s_kv[hp]])
            P.dma(vt[hp][:, :, 0:64], vc[:, hh * 64:(hh + 1) * 64].rearrange("(j p) d -> p j d", p=128), writes=[s_kv[hp]], q=LD)
            for (t0, n, isctx) in qblocks:
                qp = cnt % 2
                cnt += 1
                P.dma(qt[qp][0:96, 0:n], qcT[hh, :, t0:t0 + n], writes=[s_q[qp]], q=LD)
                keys = [0, 1] if isctx else list(range(NT))
                po, s_po = psacc()
                LA = 3
                ppl = {}

                def qk(ki, n=n, hp=hp, qp=qp, keys=keys, ppl=ppl):
                    kt = keys[ki]
                    pss, s_pss = ps()
                    MM(pss[:, 0:n], kT[hp][0:96, kt * 128:(kt + 1) * 128], qt[qp][0:96, 0:n], True, True, [s_kv[hp], s_q[qp]], [s_pss])
                    pp = pcnt_[0] % 5
                    pcnt_[0] += 1
                    ACT(pT[pp][:, 0:n], pss[:, 0:n], AF.Exp, [s_pss], [s_pT[pp]])
                    ppl[ki] = pp
                for ki in range(min(LA, len(keys))):
                    qk(ki)
                for ki, kt in enumerate(keys):
                    if ki + LA < len(keys):
                        qk(ki + LA)
                    pp = ppl.pop(ki)
                    MM(po[0:65, 0:n], vt[hp][:, kt, :], pT[pp][:, 0:n], ki == 0, ki == len(keys) - 1, [s_kv[hp], s_pT[pp]], [s_po])
                att_finalize(po, s_po, n, None, oT[2, hh * 64:(hh + 1) * 64, t0:t0 + n], tmpo[qp], rdt[qp], s_fin[qp])

    def phase_B(l):
        AR.reset(persist_mark)
        ST = "pool"
        C = 32
        R2 = 2 * C
        NCH = T // C
        GS = 4
        bm = AR.alloc([4, R2], F32)
        bmk = AR.alloc([8, R2], F32)
        rmk = AR.alloc([2], F32)
        lbt = AR.alloc([512], F32)
        c1t = AR.alloc([512], F32)
        l0 = AR.alloc([512], F32)
        s_c = P.slot("B_const")
        P.dma(bm[0:R2], bmats, writes=[s_c])
        P.dma(bmk[0:R2], bmask, writes=[s_c])
        P.dma(rmk[0:R2], brm, writes=[s_c])
        if l == 0:
            h.memset("pool", lbt, 0.0, [s_c])
            h.memset("pool", c1t, 1.0, [s_c])
        else:
            for d_ in range(2):
                P.dma(l0[d_ * C:(d_ + 1) * C, :], lb_logits[0, d_:d_ + 1, :].broadcast_to([C, 512]), writes=[s_c])
                P.dma(lbt[d_ * C:(d_ + 1) * C, :], lb_logits[1, d_:d_ + 1, :].broadcast_to([C, 512]), writes=[s_c])
            TT("dve", lbt[0:R2], lbt[0:R2], l0[0:R2], ALU.subtract, [s_c], [s_c])
            ACT(lbt[0:R2], lbt[0:R2], AF.Sigmoid, [s_c], [s_c])
            TS("dve", c1t[0:R2], lbt[0:R2], -1.0, 1.0, ALU.mult, ALU.add, [s_c], [s_c])
        Sst = AR.alloc([2, 8, 64], F32)
        Sb = AR.alloc([2, 8, 64], BF16)
        s_S = P.slot("B_S")
        s_Sb = P.slot("B_Sb")
        h.memset("pool", Sst, 0.0, [s_S])
        h.memset("pool", Sb, 0.0, [s_Sb])

        def dbl(shape, dt, n=2):
            return [AR.alloc(shape, dt) for _ in range(n)]
        z2 = dbl([GS, 512], BF16); v2 = dbl([GS, 512], BF16); q2 = dbl([8, GS, R2], BF16)
        s_z = P.slots(2, "B_z"); s_v = P.slots(2, "B_v"); s_q2 = P.slots(2, "B_q")
        sig = AR.alloc([GS, 512], F32); logf = dbl([GS, 512], F32); kk = dbl([GS, 512], F32)
        s_sig = P.slot("B_sig"); s_logf = P.slots(2, "B_logf"); s_kk = P.slots(2, "B_kk")
        ek = dbl([512], F32); e2 = dbl([512], F32); ktl = dbl([512], BF16); kh = dbl([2, 512], BF16)
        s_ek = P.slots(2, "B_ek"); s_e2 = P.slots(2, "B_e2"); s_kt = P.slots(2, "B_kt"); s_kh = P.slots(2, "B_kh")
        ktT = dbl([8, R2], BF16); s_ktT = P.slots(2, "B_ktT")
        eq = dbl([8, R2], F32); eqm = dbl([8, R2], F32); s_eq = P.slots(2, "B_eq"); s_eqm = P.slots(2, "B_eqm")
        qebf = dbl([8, R2], BF16); qebb = dbl([8, R2], BF16); qtl = dbl([8, R2], BF16)
        s_qe = P.slots(2, "B_qe"); s_qt = P.slots(2, "B_qt")
        attT = dbl([8, R2], BF16); s_att = P.slots(2, "B_att")
        o_s = dbl([GS, 512], BF16); s_os = P.slots(2, "B_os")
        for b_ in range(2):
            h.memset("pool", qebf[b_], 0.0, [s_qe[b_]])
            h.memset("pool", qebb[b_], 0.0, [s_qe[b_]])
        nctx = L // C
        gctx = nctx // GS
        NG = NCH // GS

        def cb0_of(g):
            return (nctx - GS * (g + 1)) if g < gctx else NCH - GS * (g - gctx + 1)

        def prep_group(g):
            gp = g % 2
            cf0 = g * GS
            cb0 = cb0_of(g)
            fsl = slice(cf0 * C, (cf0 + GS) * C)
            P.dma(z2[gp][0:C], zf[fsl, :].rearrange("(s p) f -> p s f", p=C), writes=[s_z[gp]], q=LD)
            P.dma(v2[gp][0:C], ib[fsl, :].rearrange("(s p) f -> p s f", p=C), writes=[s_v[gp]], q=LD)
            for s_ in range(GS):
                cb = cb0 + GS - 1 - s_
                cf = cf0 + s_
                P.dma(z2[gp][C:R2, s_, :], zb[cb * C:(cb + 1) * C, :], writes=[s_z[gp]], q=LD)
                P.dma(v2[gp][C:R2, s_, :], ib[cb * C:(cb + 1) * C, :], writes=[s_v[gp]], q=LD)
                P.dma(q2[gp][0:64, :, s_, 0:C], qbT[:, cf * C:(cf + 1) * C].rearrange("(h d) t -> d h t", d=64), writes=[s_q2[gp]], q=LD)
                P.dma(q2[gp][0:64, :, s_, C:R2], qbT[:, cb * C:(cb + 1) * C].rearrange("(h d) t -> d h t", d=64), writes=[s_q2[gp]], q=LD)
            z2f = z2[gp][0:R2].rearrange("p s f -> p (s f)")
            sigf = sig[0:R2].rearrange("p s f -> p (s f)")
            ACT(sigf, z2f, AF.Sigmoid, [s_z[gp]], [s_sig])
            TT("dve", sig[0:R2], sig[0:R2], c1t[0:R2].unsqueeze(1).broadcast_to([R2, GS, 512]), ALU.mult, [s_sig, s_c], [s_sig])
            TT("dve", sig[0:R2], sig[0:R2], lbt[0:R2].unsqueeze(1).broadcast_to([R2, GS, 512]), ALU.add, [s_sig, s_c], [s_sig])
            ACT(logf[gp][0:R2].rearrange("p s f -> p (s f)"), sigf, AF.Ln, [s_sig], [s_logf[gp]])
            TS("pool", kk[gp][0:R2].rearrange("p s f -> p (s f)"), sigf, -1.0, 1.0, ALU.mult, ALU.add, [s_sig], [s_kk[gp]])

        def stage1(step):
            g, s_ = step // GS, step % GS
            gp = g % 2
            sp = step % 2
            if s_ == 0:
                prep_group(g)
            lf = logf[gp][0:R2, s_, :]
            pe1, s_pe1 = ps()
            MM(pe1[0:R2], bm[0:R2, 0, :], lf, True, True, [s_c, s_logf[gp]], [s_pe1])
            ACT(ek[sp][0:R2], pe1[0:R2], AF.Exp, [s_pe1], [s_ek[sp]])
            TT("dve", ktl[sp][0:R2], kk[gp][0:R2, s_, :], ek[sp][0:R2], ALU.mult, [s_kk[gp], s_ek[sp]], [s_kt[sp]])
            pe2, s_pe2 = ps()
            MM(pe2[0:R2], bm[0:R2, 1, :], lf, True, True, [s_c, s_logf[gp]], [s_pe2])
            ACT(e2[sp][0:R2], pe2[0:R2], AF.Exp, [s_pe2], [s_e2[sp]])
            TT("dve", e2[sp][0:R2], kk[gp][0:R2, s_, :], e2[sp][0:R2], ALU.mult, [s_kk[gp], s_e2[sp]], [s_e2[sp]])
            for d_ in range(2):
                ACT(kh[sp][0:R2, d_, :], e2[sp][0:R2], AF.Identity, [s_e2[sp], s_c], [s_kh[sp]], scale=rmk[0:R2, d_:d_ + 1])
            pt, s_pt = ps()
            ptb = pt.bitcast(BF16)
            for hh in range(8):
                TR(ptb[0:64, hh * R2:(hh + 1) * R2], ktl[sp][0:R2, hh * 64:(hh + 1) * 64], identb[0:R2, 0:R2], [s_kt[sp], s_const], [s_pt])
            CP("act", ktT[sp][0:64], ptb[0:64, 0:8 * R2].rearrange("p (c t) -> p c t", t=R2), [s_pt], [s_ktT[sp]])
            pbT, s_pbT = ps()
            for hh in range(8):
                MM(pbT[0:64, hh * R2:(hh + 1) * R2], lf[:, hh * 64:(hh + 1) * 64], bm[0:R2, 2, :], True, True, [s_c, s_logf[gp]], [s_pbT])
            ACT(eq[sp][0:64], pbT[0:64, 0:8 * R2].rearrange("p (c t) -> p c t", t=R2), AF.Exp, [s_pbT], [s_eq[sp]])
            pbm, s_pbm = ps()
            for hh in range(8):
                MM(pbm[0:64, hh * R2:(hh + 1) * R2], lf[:, hh * 64:(hh + 1) * 64], bm[0:R2, 3, :], True, True, [s_c, s_logf[gp]], [s_pbm])
            ACT(eqm[sp][0:64], pbm[0:64, 0:8 * R2].rearrange("p (c t) -> p c t", t=R2), AF.Exp, [s_pbm], [s_eqm[sp]])
            qs = q2[gp][0:64, :, s_, :]
            TT("dve", qebf[sp][0:64, :, 0:C], qs[:, :, 0:C], eq[sp][0:64, :, 0:C], ALU.mult, [s_q2[gp], s_eq[sp]], [s_qe[sp]])
            TT("dve", qebb[sp][0:64, :, C:R2], qs[:, :, C:R2], eq[sp][0:64, :, C:R2], ALU.mult, [s_q2[gp], s_eq[sp]], [s_qe[sp]])
            TT("pool", qtl[sp][0:64], qs, eqm[sp][0:64], ALU.mult, [s_q2[gp], s_eqm[sp]], [s_qt[sp]])

        def stage2(step):
            g, s_ = step // GS, step % GS
            gp = g % 2
            sp = step % 2
            pa, s_pa = ps()
            for hh in range(8):
                MM(pa[0:R2, hh * R2:(hh + 1) * R2], ktT[sp][0:64, hh, :], qtl[sp][0:64, hh, :], True, True, [s_ktT[sp], s_qt[sp]], [s_pa])
            TT("dve", attT[sp][0:R2], pa[0:R2, 0:8 * R2].rearrange("p (h t) -> p h t", t=R2), bmk[0:R2], ALU.mult,
               [s_pa, s_c], [s_att[sp]])
            po, s_po = psacc()
            for hh in range(8):
                osl = po[0:R2, hh * 64:(hh + 1) * 64]
                MM(osl, attT[sp][0:R2, hh, :], v2[gp][0:R2, s_, hh * 64:(hh + 1) * 64], True, False, [s_att[sp], s_v[gp]], [s_po])
                MM(osl, qebf[sp][0:64, hh, :], Sb[0:64, 0, hh, :], False, False, [s_qe[sp], s_Sb], [s_po])
                MM(osl, qebb[sp][0:64, hh, :], Sb[0:64, 1, hh, :], False, True, [s_qe[sp], s_Sb], [s_po])
            CP("act", o_s[gp][0:R2, s_, :], po[0:R2], [s_po], [s_os[gp]])
            for d_ in range(2):
                pS, s_pS = ps()
                for hh in range(8):
                    MM(pS[0:64, hh * 64:(hh + 1) * 64], kh[sp][0:R2, d_, hh * 64:(hh + 1) * 64], v2[gp][0:R2, s_, hh * 64:(hh + 1) * 64], True, True,
                       [s_kh[sp], s_v[gp]], [s_pS])
                Sv = Sst[0:64, d_]
                TT("dve", Sv, Sv, eq[sp][0:64, :, C - 1 + d_:C + d_].broadcast_to([64, 8, 64]), ALU.mult, [s_S, s_eq[sp]], [s_S])
                TT("dve", Sv, Sv, pS[0:64].rearrange("p (h x) -> p h x", x=64), ALU.add, [s_S, s_pS], [s_S])
            CP("act", Sb[0:64], Sst[0:64], [s_S], [s_Sb])
            if s_ == GS - 1:
                cf0 = g * GS
                cb0 = cb0_of(g)
                fsl = slice(cf0 * C, (cf0 + GS) * C)
                P.dma(ofb[0, fsl, :].rearrange("(s p) f -> p s f", p=C), o_s[gp][0:C], reads=[s_os[gp]], q=ST)
                for s2 in range(GS):
                    cb = cb0 + GS - 1 - s2
                    P.dma(ofb[1, cb * C:(cb + 1) * C, :], o_s[gp][C:R2, s2, :], reads=[s_os[gp]], q=ST)

        nsteps = NG * GS
        stage1(0)
        for st_ in range(nsteps):
            if st_ + 1 < nsteps:
                stage1(st_ + 1)
            stage2(st_)
        P.barrier()
        gon = AR.alloc([512], F32)
        s_g = P.slot("B_gon")
        P.dma(gon, b_onorm[l:l + 1, :].broadcast_to([128, 512]), writes=[s_g])
        of_ = dbl([512], BF16); ob_ = dbl([512], BF16); sg_ = dbl([512], BF16)
        s_in = P.slots(2, "Bf_in")
        osum = dbl([512], F32); osq = dbl([512], F32); st = dbl([8], F32); on_ = dbl([512], BF16); oTs = dbl([4, 128], BF16)
        s_w2 = P.slots(2, "Bf_w"); s_oTs = P.slots(2, "Bf_oT")
        for j in range(NT):
            p_ = j % 2
            tsl = slice(j * 128, (j + 1) * 128)
            P.dma(of_[p_], ofb[0, tsl, :], writes=[s_in[p_]], q=LD)
            P.dma(ob_[p_], ofb[1, tsl, :], writes=[s_in[p_]], q=LD)
            P.dma(sg_[p_], sgb[tsl, :], writes=[s_in[p_]], q=LD)
            TT("dve", osum[p_], of_[p_], ob_[p_], ALU.add, [s_in[p_]], [s_w2[p_]])
            ACT(osq[p_], osum[p_], AF.Square, [s_w2[p_]], [s_w2[p_]], scale=0.125)
            h.reduce(st[p_], osq[p_].rearrange("p (h d) -> p h d", d=64), ALU.add, [s_w2[p_]], [s_w2[p_]])
            TS("dve", st[p_], st[p_], EPS, None, ALU.add, None, [s_w2[p_]], [s_w2[p_]])
            ACT(st[p_], st[p_], AF.Ln, [s_w2[p_]], [s_w2[p_]])
            ACT(st[p_], st[p_], AF.Exp, [s_w2[p_]], [s_w2[p_]], scale=-0.5)
            o3 = osum[p_].rearrange("p (h d) -> p h d", d=64)
            TT("dve", o3, o3, st[p_].unsqueeze(2).broadcast_to([128, 8, 64]), ALU.mult, [s_w2[p_]], [s_w2[p_]])
            TT("pool", osum[p_], osum[p_], gon, ALU.mult, [s_w2[p_], s_g], [s_w2[p_]])
            TT("pool", on_[p_], osum[p_], sg_[p_], ALU.mult, [s_w2[p_], s_in[p_]], [s_w2[p_]])
            pt, s_pt = ps()
            ptb = pt.bitcast(BF16)
            for c in range(4):
                TR(ptb[:, c * 128:(c + 1) * 128], on_[p_][:, c * 128:(c + 1) * 128], identb, [s_w2[p_], s_const], [s_pt])
            CP("act", oTs[p_], ptb[:, 0:512].rearrange("p (c t) -> p c t", t=128), [s_pt], [s_oTs[p_]])
            P.dma(oT[1, :, tsl].rearrange("(c p) t -> p c t", p=128), oTs[p_], reads=[s_oTs[p_]], q=ST)

    def phase_merge(l, xsrc, with_ctx):
        AR.reset(persist_mark)
        ST = "pool"
        wbr = AR.alloc([3, 4, 1024], BF16)
        wout = AR.alloc([8, 1024], BF16)
        wr = AR.alloc([8, 36], F32)
        gt1 = AR.alloc([2, 1024], F32)
        s_wm = P.slot("M_w")
        for n_ in range(3):
            P.dma(wbr[:, n_], wb_br[l, n_].rearrange("(k p) n -> p k n", p=128), writes=[s_wm])
        P.dma(wout, wb_out[l].rearrange("(k p) n -> p k n", p=128), writes=[s_wm])
        P.dma(wr, w_r[l].rearrange("(k p) n -> p k n", p=128), writes=[s_wm])
        for j_ in range(2):
            P.dma(gt1[:, j_, :], gtd[l, 0, j_:j_ + 1, :].broadcast_to([128, 1024]), writes=[s_wm])
        oT3 = [AR.alloc([3, 4, 512], BF16) for _ in range(2)]
        gT3 = [AR.alloc([24, 512], BF16) for _ in range(2)]
        s_in = P.slots(2, "M_in")
        yT = AR.alloc([8, 512], BF16)
        s_yT = P.slot("M_yT")
        acc = [AR.alloc([512], F32) for _ in range(2)]
        tm1 = [AR.alloc([512], F32) for _ in range(2)]
        s_acc = P.slots(2, "M_acc")
        xt = [AR.alloc([1, 1024], F32) for _ in range(2)]
        xnew = [AR.alloc([1, 1024], F32) for _ in range(2)]
        s_x = P.slots(2, "M_x")
        s_xn = P.slots(2, "M_xn")
        h2f = AR.alloc([8, 128], F32)
        h2b = [AR.alloc([8, 512], BF16) for _ in range(2)]
        _sh2 = P.slot("M_h2")
        s_h2 = [_sh2, _sh2]
        s_h2f = P.slot("M_h2f")
        tmp = dict(ms=AR.alloc([8], F32), junk=AR.alloc([1024], F32), xnf=AR.alloc([1024], F32), t2f=AR.alloc([4, 128], F32))
        s_tmp = P.slot("M_tmp")
        R = {k: AR.alloc([n_], F32) for k, n_ in dict(lg=36, oh=4, ge=4, esel=8, es2=8, eq1=8, eq2=8, sc=8, csel=8).items()}
        cmb = [AR.alloc([32], F32) for _ in range(2)]
        s_R = P.slot("M_R")
        s_cmb = P.slots(2, "M_cmb")
        tix = 0
        for bi_, (t0, n, isctx) in enumerate(blocks(include_ctx=with_ctx)):
            bp = bi_ % 2
            nt = n // 128
            jj = 1 if isctx else 0
            bsl = slice(t0, t0 + n)
            for br in range(3):
                P.dma(oT3[bp][:, br, :, 0:n], oT[br, :, bsl].rearrange("(k p) t -> p k t", p=128), writes=[s_in[bp]], q=LD)
            P.dma(gT3[bp][:, :, 0:n], gT[:, bsl].rearrange("(c p) t -> p c t", p=128), writes=[s_in[bp]], q=LD)
            for m in range(8):
                pbs = []
                for br in range(3):
                    pb, s_pb = ps()
                    for k in range(4):
                        MM(pb[:, 0:n], wbr[:, br, k, m * 128:(m + 1) * 128], oT3[bp][:, br, k, 0:n], k == 0, k == 3, [s_wm, s_in[bp]], [s_pb])
                    pbs.append((pb, s_pb))
                ap_ = m % 2
                TT("dve", acc[ap_][:, 0:n], pbs[0][0][:, 0:n], gT3[bp][:, m, 0:n], ALU.mult, [pbs[0][1], s_in[bp]], [s_acc[ap_]])
                TT("dve", tm1[ap_][:, 0:n], pbs[1][0][:, 0:n], gT3[bp][:, 8 + m, 0:n], ALU.mult, [pbs[1][1], s_in[bp]], [s_acc[ap_]])
                TT("pool", acc[ap_][:, 0:n], acc[ap_][:, 0:n], tm1[ap_][:, 0:n], ALU.add, [s_acc[ap_]], [s_acc[ap_]])
                TT("dve", tm1[ap_][:, 0:n], pbs[2][0][:, 0:n], gT3[bp][:, 16 + m, 0:n], ALU.mult, [pbs[2][1], s_in[bp], s_acc[ap_]], [s_acc[ap_]])
                TT("pool", yT[:, m, 0:n], acc[ap_][:, 0:n], tm1[ap_][:, 0:n], ALU.add, [s_acc[ap_]], [s_yT])
            for j in range(nt):
                par = tix % 2
                tix += 1
                tsl = slice(t0 + j * 128, t0 + (j + 1) * 128)
                P.dma(xt[par][:, 0, :], xsrc[tsl, :], writes=[s_x[par]], q=LD)
                for hf in range(2):
                    pz, s_pz = ps()
                    for k in range(8):
                        MM(pz, yT[:, k, j * 128:(j + 1) * 128], wout[:, k, hf * 512:(hf + 1) * 512], k == 0, k == 7, [s_yT, s_wm], [s_pz])
                    TT("dve", xnew[par][:, 0, hf * 512:(hf + 1) * 512], pz, gt1[:, jj, hf * 512:(hf + 1) * 512], ALU.mult, [s_pz, s_wm], [s_xn[par]])
                TT("pool", xnew[par][:, 0, :], xnew[par][:, 0, :], xt[par][:, 0, :], ALU.add, [s_xn[par], s_x[par]], [s_xn[par]])
                P.dma(xs2[tsl, :], xnew[par][:, 0, :], reads=[s_xn[par]], q=ST)
                norm_mod_T(xnew[par], s_xn[par], 1, l, 1, lambda j_: jj, h2b[bp], s_h2[bp], tmp, s_tmp, hTf=h2f, tok0=j * 128)
                pr_, s_pr = ps()
                for k in range(8):
                    MM(pr_[:, 0:36], h2f[:, k, :], wr[:, k, :], k == 0, k == 7, [s_h2[bp], s_wm], [s_pr])
                lg = R["lg"]
                CP("dve", lg, pr_[:, 0:36], [s_pr], [s_R])
                sc_ = R["sc"]
                rs = [s_R]
                h.reduce(sc_[:, 0:1], lg[:, 0:4], ALU.max, rs, rs)
                TS("dve", R["oh"], lg[:, 0:4], sc_[:, 0:1], None, ALU.is_equal, None, rs, rs)
                TS("dve", sc_[:, 1:2], sc_[:, 0:1], -1.0, None, ALU.mult, None, rs, rs)
                ACT(R["ge"], lg[:, 0:4], AF.Exp, rs, rs, bias=sc_[:, 1:2], accum_out=sc_[:, 2:3])
                P.dve(lambda e, o_=sc_[:, 2:3]: e.reciprocal(out=o_, in_=o_), rs, rs)
                el = lg[:, 4:36].rearrange("p (g e) -> p g e", e=8)
                TS("dve", R["esel"], el[:, 0, :], R["oh"][:, 0:1], None, ALU.mult, None, rs, rs)
                for g_ in range(1, 4):
                    h.stt("dve", R["esel"], el[:, g_, :], R["oh"][:, g_:g_ + 1], R["esel"], ALU.mult, ALU.add, rs, rs)
                h.reduce(sc_[:, 3:4], R["esel"], ALU.max, rs, rs)
                TS("dve", R["eq1"], R["esel"], sc_[:, 3:4], None, ALU.is_equal, None, rs, rs)
                h.stt("dve", R["es2"], R["eq1"], -1e30, R["esel"], ALU.mult, ALU.add, rs, rs)
                h.reduce(sc_[:, 4:5], R["es2"], ALU.max, rs, rs)
                TS("dve", R["eq2"], R["es2"], sc_[:, 4:5], None, ALU.is_equal, None, rs, rs)
                TS("dve", sc_[:, 5:6], sc_[:, 3:4], -1.0, None, ALU.mult, None, rs, rs)
                ACT(sc_[:, 6:7], sc_[:, 4:5], AF.Exp, rs, rs, bias=sc_[:, 5:6])
                TS("dve", sc_[:, 7:8], sc_[:, 6:7], 1.0, None, ALU.add, None, rs, rs)
                P.dve(lambda e, o_=sc_[:, 7:8]: e.reciprocal(out=o_, in_=o_), rs, rs)
                TT("dve", sc_[:, 7:8], sc_[:, 7:8], sc_[:, 2:3], ALU.mult, rs, rs)
                TT("dve", sc_[:, 6:7], sc_[:, 6:7], sc_[:, 7:8], ALU.mult, rs, rs)
                TS("dve", R["csel"], R["eq1"], sc_[:, 7:8], None, ALU.mult, None, rs, rs)
                h.stt("dve", R["csel"], R["eq2"], sc_[:, 6:7], R["csel"], ALU.mult, ALU.add, rs, rs)
                c3 = cmb[par].rearrange("p (g e) -> p g e", e=8)
                for g_ in range(4):
                    TS("dve", c3[:, g_, :], R["csel"], R["oh"][:, g_:g_ + 1], None, ALU.mult, None, rs, [s_cmb[par]])
                P.dma(cmbd[tsl, :], cmb[par], reads=[s_cmb[par]], q=ST)
            P.dma(h2T[:, bsl].rearrange("(k p) t -> p k t", p=128), h2b[bp][:, :, 0:n], reads=[s_h2[bp]], q=ST)

    def phase_moe(l, with_ctx, last):
        AR.reset(persist_mark)
        BS = 2048
        gt2 = AR.alloc([2, 1024], F32)
        gfin = AR.alloc([1024], F32)
        s_c = P.slot("E_c")
        for j_ in range(2):
            P.dma(gt2[:, j_, :], gtd[l, 1, j_:j_ + 1, :].broadcast_to([128, 1024]), writes=[s_c])
        P.dma(gfin, g_final.unsqueeze(0).broadcast_to([128, 1024]), writes=[s_c])
        hb = AR.alloc([8, BS], BF16)
        cm = AR.alloc([BS // 128, 32], F32)
        yacc = AR.alloc([BS // 128, 1024], F32)
        s_hb = P.slot("E_hb"); s_y = P.slots(BS // 128, "E_y")
        w1e = [AR.alloc([8, 256], BF16) for _ in range(2)]
        w3e = [AR.alloc([8, 256], BF16) for _ in range(2)]
        w2e = [AR.alloc([2, 1024], BF16) for _ in range(2)]
        s_we = P.slots(2, "E_w")
        su = [AR.alloc([512], F32) for _ in range(2)]
        aT = [AR.alloc([2, 512], BF16) for _ in range(2)]
        s_su = P.slots(2, "E_su"); s_aT = P.slots(2, "E_aT")
        xt = [AR.alloc([1024], F32) for _ in range(2)]
        xo = [AR.alloc([1024], F32) for _ in range(2)]
        st = [AR.alloc([2], F32) for _ in range(2)]
        junk = AR.alloc([1024], F32)
        s_x = P.slots(2, "E_x"); s_xo = P.slots(2, "E_xo")
        for (t0, n, isctx) in blocks(include_ctx=with_ctx, bs=BS):
            nt = n // 128
            jj = 1 if isctx else 0
            bsl = slice(t0, t0 + n)
            P.dma(hb[:, :, 0:n], h2T[:, bsl].rearrange("(k p) t -> p k t", p=128), writes=[s_hb], q=LD)
            P.dma(cm[:, 0:nt, :], cmbd[bsl, :].rearrange("(j p) e -> p j e", p=128), writes=[s_hb], q=LD)
            for j in range(nt):
                h.memset("pool", yacc[:, j, :], 0.0, [s_y[j]])
            items = [(e_, sb0, min(512, n - sb0)) for e_ in range(NEXP) for sb0 in range(0, n, 512)]
            loaded = set()

            def uv(ii):
                e_, sb0, nn = items[ii]
                ep = e_ % 2
                ap_ = ii % 2
                if e_ not in loaded:
                    loaded.add(e_)
                    P.dma(w1e[ep], wb1[l, e_].rearrange("(k p) n -> p k n", p=128), writes=[s_we[ep]], q=LD)
                    P.dma(w3e[ep], wb3[l, e_].rearrange("(k p) n -> p k n", p=128), writes=[s_we[ep]], q=LD)
                    P.dma(w2e[ep], wb2[l, e_].rearrange("(k p) n -> p k n", p=128), writes=[s_we[ep]], q=LD)
                for cc in range(2):
                    pu, s_pu = ps()
                    for k in range(8):
                        MM(pu[:, 0:nn], w1e[ep][:, k, cc * 128:(cc + 1) * 128], hb[:, k, sb0:sb0 + nn], k == 0, k == 7, [s_we[ep], s_hb], [s_pu])
                    pv, s_pv = ps()
                    for k in range(8):
                        MM(pv[:, 0:nn], w3e[ep][:, k, cc * 128:(cc + 1) * 128], hb[:, k, sb0:sb0 + nn], k == 0, k == 7, [s_we[ep], s_hb], [s_pv])
                    ACT(su[cc][:, 0:nn], pu[:, 0:nn], AF.Silu, [s_pu], [s_su[cc]])
                    TT("dve", aT[ap_][:, cc, 0:nn], su[cc][:, 0:nn], pv[:, 0:nn], ALU.mult, [s_su[cc], s_pv], [s_aT[ap_]])

            def yy(ii):
                e_, sb0, nn = items[ii]
                ep = e_ % 2
                ap_ = ii % 2
                for j in range(nn // 128):
                    tj = sb0 // 128 + j
                    for hf in range(2):
                        py, s_py = ps()
                        for cc in range(2):
                            MM(py, aT[ap_][:, cc, j * 128:(j + 1) * 128], w2e[ep][:, cc, hf * 512:(hf + 1) * 512], cc == 0, cc == 1,
                               [s_aT[ap_], s_we[ep]], [s_py])
                        ysl = yacc[:, tj, hf * 512:(hf + 1) * 512]
                        h.stt("dve", ysl, py, cm[:, tj, e_:e_ + 1], ysl, ALU.mult, ALU.add, [s_py, s_hb, s_y[tj]], [s_y[tj]])

            uv(0)
            for ii in range(len(items)):
                if ii + 1 < len(items):
                    uv(ii + 1)
                yy(ii)
            for j in range(nt):
                p_ = j % 2
                tsl = slice(t0 + j * 128, t0 + (j + 1) * 128)
                P.dma(xt[p_], xs2[tsl, :], writes=[s_x[p_]], q=LD)
                TT("dve", xo[p_], yacc[:, j, :], gt2[:, jj, :], ALU.mult, [s_y[j], s_c], [s_xo[p_]])
                TT("pool", xo[p_], xo[p_], xt[p_], ALU.add, [s_xo[p_], s_x[p_]], [s_xo[p_]])
                if not last:
                    P.dma(xs[tsl, :], xo[p_], reads=[s_xo[p_]], q=ST)
                elif not isctx:
                    ACT(junk, xo[p_], AF.Square, [s_xo[p_]], [s_xo[p_]], scale=1.0 / 32.0, accum_out=st[p_][:, 0:1])
                    TS("dve", st[p_][:, 0:1], st[p_][:, 0:1], EPS, None, ALU.add, None, [s_xo[p_]], [s_xo[p_]])
                    ACT(st[p_][:, 0:1], st[p_][:, 0:1], AF.Ln, [s_xo[p_]], [s_xo[p_]])
                    ACT(st[p_][:, 0:1], st[p_][:, 0:1], AF.Exp, [s_xo[p_]], [s_xo[p_]], scale=-0.5)
                    h.stt("dve", xo[p_], xo[p_], st[p_][:, 0:1], gfin, ALU.mult, ALU.mult, [s_xo[p_], s_c], [s_xo[p_]])
                    final_ops.append(P.dma(out[t0 - L + j * 128:t0 - L + (j + 1) * 128, :], xo[p_], reads=[s_xo[p_]], q=ST))

    final_ops = []
    P.barrier()

    def on(name):
        return phases is None or name in phases

    for l in range(layers):
        last = (l == layers - 1)
        if on("mod"):
            phase_mod(l)
            P.barrier()
        if on("A"):
            phase_A(l, xin if l == 0 else xs)
            P.barrier()
        if on("attA"):
            phase_attA(l, not last)
            P.barrier()
        if on("attC"):
            phase_attC(l, not last)
            P.barrier()
        if on("B"):
            phase_B(l)
            P.barrier()
        if on("merge"):
            phase_merge(l, xin if l == 0 else xs, not last)
            P.barrier()
        if on("moe"):
            phase_moe(l, not last, last)
            P.barrier()
    if not final_ops:
        final_ops = [P.barrier()]
    P.emit(final_wait_ops=final_ops)
    return nc, dbg_names


def _rope_tables(S, rot_dim, qscale):
    rows = S // 64
    row = np.repeat(np.arange(rows, dtype=np.float32), 64)
    col = np.tile(np.arange(64, dtype=np.float32), rows)
    n_freq = rot_dim // 4
    inv = (10000.0 ** (-np.arange(n_freq, dtype=np.float32) / n_freq)).astype(np.float32)
    ang = np.concatenate([row[:, None] * inv, col[:, None] * inv], axis=-1).astype(np.float32)
    c, s_ = np.cos(ang).astype(np.float32), np.sin(ang).astype(np.float32)
    T = L + S
    tab = np.zeros((T, 4, rot_dim), np.float32)
    c2 = np.concatenate([c, c], -1)
    s2 = np.concatenate([-s_, s_], -1)
    tab[L:, 0] = c2 * qscale
    tab[L:, 1] = s2 * qscale
    tab[L:, 2] = c2
    tab[L:, 3] = s2
    tab[:L, 0] = qscale
    tab[:L, 2] = 1.0
    return tab


def _consts(S):
    c = {}
    c["ident"] = np.eye(128, dtype=np.float32)
    c["ropeA"] = _rope_tables(S, 64, 64 ** -0.5)
    c["ropeC"] = _rope_tables(S, 32, 96 ** -0.5)
    j = np.arange(128)[:, None]
    i = np.arange(128)[None, :]
    am = np.zeros((128, 2, 128), np.float32)
    am[:, 0] = (j >= i)
    am[:, 1] = (j <= i)
    c["amask"] = am
    CC = 32
    mid = CC // 2 - 1
    s_ = np.arange(CC)[:, None]
    t_ = np.arange(CC)[None, :]
    bm = np.zeros((2 * CC, 4, 2 * CC), np.float32)
    cmk_f = (s_ <= mid).astype(np.float32) - (s_ <= t_)
    cmk_b = (s_ >= CC - 1 - mid).astype(np.float32) - (s_ >= t_)
    bm[:CC, 0, :CC] = cmk_f; bm[CC:, 0, CC:] = cmk_b
    bm[:CC, 1, :CC] = (s_ > t_); bm[CC:, 1, CC:] = (s_ < t_)
    bm[:CC, 2, :CC] = (s_ <= t_); bm[CC:, 2, CC:] = (s_ >= t_)
    bm[:CC, 3, :CC] = -cmk_f; bm[CC:, 3, CC:] = -cmk_b
    c["bmats"] = bm
    mk = np.zeros((2 * CC, 8, 2 * CC), np.float32)
    mk[:CC, :, :CC] = (s_ <= t_)[:, None, :]
    mk[CC:, :, CC:] = (s_ >= t_)[:, None, :]
    c["bmask"] = mk
    rm = np.zeros((2 * CC, 2), np.float32)
    rm[:CC, 0] = 1.0
    rm[CC:, 1] = 1.0
    c["brm"] = rm
    return c


def _fm(v, k):
    return np.ascontiguousarray(np.swapaxes(v.reshape(v.shape[:-1] + (k, 128)), -1, -2))


def make_in_map(inp, b, S):
    f = lambda a: np.ascontiguousarray(np.asarray(a, dtype=np.float32))
    m = {}
    m["xin"] = np.concatenate([f(inp["ctx"])[b], f(inp["x"])[b, :S]], axis=0)
    m["cvec"] = np.ascontiguousarray(np.stack([_fm(f(inp["c"])[b], 8), _fm(f(inp["c_ctx"]), 8)], axis=-1))
    m["w_mod"] = f(inp["w_mod"])
    m["b_modT"] = _fm(f(inp["b_mod"]), 48)
    m["b_mod"] = f(inp["b_mod"])
    m["g1T"] = _fm(f(inp["g_norm1"]), 8)
    m["g2T"] = _fm(f(inp["g_norm2"]), 8)
    m["w_in"] = f(inp["w_in"])
    m["a_sink"] = f(inp["a_sink"])
    m["lb_logits"] = f(inp["b_lb_logits"])
    m["b_onorm"] = f(inp["b_onorm"])
    m["gqT"] = _fm(f(inp["c_qnorm"]), 2)
    m["gkvT"] = _fm(f(inp["c_kvnorm"]), 1)
    m["w_uq"] = f(inp["w_uq"])
    wk = f(inp["w_ukv"]).reshape(2, 128, 8, 128)
    m["w_ukv"] = np.ascontiguousarray(np.concatenate([wk[..., :64].reshape(2, 128, 512), wk[..., 64:].reshape(2, 128, 512)], axis=-1))
    m["w_br"] = f(inp["w_br"])
    m["w_out"] = f(inp["w_out"])
    m["w_r"] = np.ascontiguousarray(np.concatenate([f(inp["w_rg"]), f(inp["w_re"])], axis=-1))
    m["w1"] = f(inp["w1"]); m["w3"] = f(inp["w3"]); m["w2"] = f(inp["w2"])
    m["g_final"] = f(inp["g_final"])
    return m


_CACHE = {}


def kernel(**inputs):
    x = np.asarray(inputs["x"])
    B, S, _ = x.shape
    if S not in _CACHE:
        _CACHE[S] = (build_program(S)[0], _consts(S))
    nc, consts = _CACHE[S]
    in_maps = []
    for b in range(B):
        m = make_in_map(inputs, b, S)
        m.update(consts)
        in_maps.append(m)
    res = run_bass_kernel_spmd(nc, in_maps, core_ids=list(range(B)))
    return np.stack([np.asarray(r["out"], dtype=np.float32) for r in res.results], axis=0)
```
